# Optimizing a Trainium2 kernel written in Bass

```python
import jax, jax.numpy as jnp
from jax import lax
import numpy as np

D_MODEL = 1024
BATCH = 8
SEQ = 4096
DEPTH = 1

HEAD_DIM = 128
FOX_HEADS = 4
DSA_HEADS = 4
IDX_HEADS = 8
IDX_DIM = 64
ROT_FRAC_DEN = 4
ROPE_THETA = 500000.0
TOPK_MAX = 256
Q_BLOCK = 128
D_FF = -(-8 * D_MODEL // (3 * 256)) * 256
RMS_EPS = 1e-6
N_BRANCHES = 2

FOX_W = FOX_HEADS * HEAD_DIM
DSA_W = DSA_HEADS * HEAD_DIM
IN_WIDTHS = (FOX_W, FOX_W, FOX_W, FOX_HEADS, DSA_W, HEAD_DIM, HEAD_DIM,
             IDX_HEADS * IDX_DIM, IDX_DIM, IDX_HEADS, D_MODEL, D_MODEL)
D_IN = sum(IN_WIDTHS)
IN_SPLITS = tuple(int(v) for v in np.cumsum(IN_WIDTHS)[:-1])

kernel_name = "fox_dsa_gated_hybrid_block"


def rmsnorm(x, g):
    xf = x.astype(jnp.float32)
    y = xf * lax.rsqrt(jnp.mean(xf * xf, axis=-1, keepdims=True) + RMS_EPS)
    return (y * g.astype(jnp.float32)).astype(x.dtype)


def rope_partial(x, pos, rot):
    half = rot // 2
    inv_freq = jnp.float32(ROPE_THETA) ** (-jnp.arange(half, dtype=jnp.float32) * 2.0 / rot)
    ang = pos.astype(jnp.float32)[:, None] * inv_freq[None, :]
    if x.ndim == 4:
        ang = ang[:, None, :]
    cos = jnp.cos(ang).astype(x.dtype)
    sin = jnp.sin(ang).astype(x.dtype)
    x1, x2, rest = x[..., :half], x[..., half:rot], x[..., rot:]
    return jnp.concatenate([x1 * cos - x2 * sin, x2 * cos + x1 * sin, rest], axis=-1)


def fox_attention(q, k, v, log_f):
    B, S, H, dh = q.shape
    scale = dh ** -0.5
    c = jnp.cumsum(log_f, axis=1).transpose(0, 2, 1)
    kpos = jnp.arange(S)

    def block(i):
        start = i * Q_BLOCK
        q_b = lax.dynamic_slice_in_dim(q, start, Q_BLOCK, axis=1)
        c_b = lax.dynamic_slice_in_dim(c, start, Q_BLOCK, axis=2)
        s = jnp.einsum('bqhd,bkhd->bhqk', q_b, k).astype(jnp.float32) * scale
        bias = c_b[..., :, None] - c[..., None, :]
        qpos = start + jnp.arange(Q_BLOCK)
        causal = kpos[None, :] <= qpos[:, None]
        s = jnp.where(causal, s + bias, -jnp.inf)
        p = jax.nn.softmax(s, axis=-1)
        return jnp.einsum('bhqk,bkhd->bqhd', p.astype(v.dtype), v)

    out = lax.map(block, jnp.arange(S // Q_BLOCK))
    return out.transpose(1, 0, 2, 3, 4).reshape(B, S, H, dh)


def dsa_attention(q, k, v, q_idx, k_idx, w_idx, topk):
    B, S, H, dh = q.shape
    scale = dh ** -0.5
    idx_scale = (IDX_DIM ** -0.5) * (IDX_HEADS ** -0.5)
    kpos = jnp.arange(S)
    gather = jax.vmap(lambda arr, ii: arr[ii])

    def block(i):
        start = i * Q_BLOCK
        qpos = start + jnp.arange(Q_BLOCK)
        q_b = lax.dynamic_slice_in_dim(q, start, Q_BLOCK, axis=1)
        qi_b = lax.dynamic_slice_in_dim(q_idx, start, Q_BLOCK, axis=1)
        wi_b = lax.dynamic_slice_in_dim(w_idx, start, Q_BLOCK, axis=1)
        dots = jnp.einsum('bqhd,bkd->bqhk', qi_b, k_idx).astype(jnp.float32)
        score = jnp.einsum('bqhk,bqh->bqk', jax.nn.relu(dots), wi_b.astype(jnp.float32)) * idx_scale
        causal = kpos[None, :] <= qpos[:, None]
        score = jnp.where(causal[None], score, -jnp.inf)
        _, sel = lax.top_k(score, topk)
        valid = sel <= qpos[None, :, None]
        k_g = gather(k, sel)
        v_g = gather(v, sel)
        s = jnp.einsum('bqhd,bqkd->bhqk', q_b, k_g).astype(jnp.float32) * scale
        s = jnp.where(valid[:, None], s, -jnp.inf)
        p = jax.nn.softmax(s, axis=-1)
        return jnp.einsum('bhqk,bqkd->bqhd', p.astype(v.dtype), v_g)

    out = lax.map(block, jnp.arange(S // Q_BLOCK))
    return out.transpose(1, 0, 2, 3, 4).reshape(B, S, H, dh)


def setup_inputs(seed: int = 0) -> dict:
    key = jax.random.key(seed)
    ks = jax.random.split(key, 16)
    f32 = jnp.float32
    nrm = lambda k, shape, fan: jax.random.normal(k, shape, f32) * (fan ** -0.5)
    gain = lambda k: 1.0 + 0.1 * jax.random.normal(k, (DEPTH, D_MODEL), f32)
    return {
        "x": jax.random.normal(ks[0], (BATCH, SEQ, D_MODEL), f32),
        "norm_mix_pre": gain(ks[1]),
        "w_in": nrm(ks[2], (DEPTH, D_MODEL, D_IN), D_MODEL),
        "b_forget": 2.0 + 0.1 * jax.random.normal(ks[3], (DEPTH, FOX_HEADS), f32),
        "b_gate": 0.1 * jax.random.normal(ks[4], (DEPTH, N_BRANCHES, D_MODEL), f32),
        "w_branch_fox": nrm(ks[5], (DEPTH, FOX_W, D_MODEL), FOX_W),
        "w_branch_dsa": nrm(ks[6], (DEPTH, DSA_W, D_MODEL), DSA_W),
        "w_out": nrm(ks[7], (DEPTH, D_MODEL, D_MODEL), D_MODEL),
        "norm_mix_post": gain(ks[8]),
        "norm_ffn_pre": gain(ks[9]),
        "w_ffn_gate": nrm(ks[10], (DEPTH, D_MODEL, D_FF), D_MODEL),
        "w_ffn_up": nrm(ks[11], (DEPTH, D_MODEL, D_FF), D_MODEL),
        "w_ffn_down": nrm(ks[12], (DEPTH, D_FF, D_MODEL), D_FF),
        "norm_ffn_post": gain(ks[13]),
    }


def reference(x, norm_mix_pre, w_in, b_forget, b_gate, w_branch_fox, w_branch_dsa, w_out,
              norm_mix_post, norm_ffn_pre, w_ffn_gate, w_ffn_up, w_ffn_down, norm_ffn_post):
    B, S, _ = x.shape
    pos = jnp.arange(S)
    topk = min(TOPK_MAX, S // 4)
    rot_h = HEAD_DIM // ROT_FRAC_DEN
    rot_i = IDX_DIM // ROT_FRAC_DEN
    h = x
    for l in range(DEPTH):
        u = rmsnorm(h, norm_mix_pre[l])
        proj = u @ w_in[l]
        (q_f, k_f, v_f, f_logit, q_d, k_d, v_d,
         q_i, k_i, w_i, g_f, g_d) = jnp.split(proj, IN_SPLITS, axis=-1)

        log_f = jax.nn.log_sigmoid(f_logit.astype(jnp.float32) + b_forget[l].astype(jnp.float32))
        o_f = fox_attention(q_f.reshape(B, S, FOX_HEADS, HEAD_DIM),
                            k_f.reshape(B, S, FOX_HEADS, HEAD_DIM),
                            v_f.reshape(B, S, FOX_HEADS, HEAD_DIM), log_f).reshape(B, S, FOX_W)

        q_d = rope_partial(q_d.reshape(B, S, DSA_HEADS, HEAD_DIM), pos, rot_h)
        k_d = rope_partial(k_d, pos, rot_h)
        q_i = rope_partial(q_i.reshape(B, S, IDX_HEADS, IDX_DIM), pos, rot_i)
        k_i = rope_partial(k_i, pos, rot_i)
        o_d = dsa_attention(q_d, k_d, v_d, q_i, k_i, w_i, topk).reshape(B, S, DSA_W)

        mixed = (jax.nn.sigmoid(g_f + b_gate[l, 0]) * (o_f @ w_branch_fox[l])
                 + jax.nn.sigmoid(g_d + b_gate[l, 1]) * (o_d @ w_branch_dsa[l]))
        h = h + rmsnorm(mixed @ w_out[l], norm_mix_post[l])

        v_in = rmsnorm(h, norm_ffn_pre[l])
        ff = (jax.nn.silu(v_in @ w_ffn_gate[l]) * (v_in @ w_ffn_up[l])) @ w_ffn_down[l]
        h = h + rmsnorm(ff, norm_ffn_post[l])
    return h
```

```python
import numpy as np
from contextlib import ExitStack
import concourse.bass as bass
import concourse.mybir as mybir
from concourse.bass_utils import run_bass_kernel_spmd

F32 = mybir.dt.float32
BF16 = mybir.dt.bfloat16
ALU = mybir.AluOpType
AF = mybir.ActivationFunctionType
AX = mybir.AxisListType

ENGS = ("sync", "scalar", "tensor", "vector", "gpsimd")

D = 1024
KC = 8
DFF = 2816
NFT = DFF // 128
HD = 128
NIT = 16
ARENA_BYTES = 200 * 1024
EPS = 1e-6
NEG = -30000.0


class Prog:
    NDMA_SEM = 6

    def __init__(self):
        self.ops = {e: [] for e in ENGS}
        self.last_w = {}
        self.readers = {}
        self.last_compute = {e: None for e in ENGS}
        self.last_dmas = {e: [] for e in ENGS}

    def add(self, eng, fn, r=(), w=(), dma=False, extra=()):
        idx = len(self.ops[eng])
        deps = set(extra)
        px = [k for k in r if isinstance(k, str) and k.startswith("ps")]
        if px:
            r = [k for k in r if k not in px]
            w = list(w) + px
        for k in r:
            if k in self.last_w:
                deps.add(self.last_w[k])
        for k in w:
            if k in self.last_w:
                deps.add(self.last_w[k])
            for rd in self.readers.get(k, ()):
                deps.add(rd)
        me = (eng, idx)
        deps.discard(me)
        self.ops[eng].append(dict(fn=fn, deps=deps, dma=dma, signal=False, sig=None))
        for k in r:
            self.readers.setdefault(k, []).append(me)
        for k in w:
            self.last_w[k] = me
            self.readers[k] = []
        if fn is not None:
            if dma:
                self.last_dmas[eng] = (self.last_dmas[eng] + [me])[-self.NDMA_SEM:]
            else:
                self.last_compute[eng] = me
        return me

    def barrier(self):
        deps = []
        for e in ENGS:
            if self.last_compute[e] is not None:
                deps.append(self.last_compute[e])
            deps.extend(self.last_dmas[e])
        for e in ENGS:
            self.add(e, None, extra=deps)
        self.last_w = {}
        self.readers = {}

    def emit(self, block, sems):
        ops = self.ops
        for e in ENGS:
            for op in ops[e]:
                for (e2, j) in op["deps"]:
                    p = ops[e2][j]
                    if e2 == e and e == "tensor" and not p["dma"]:
                        continue
                    p["signal"] = True
        for e in ENGS:
            cnt = 0
            ndma = 0
            for op in ops[e]:
                if op["fn"] is None:
                    continue
                if op["dma"]:
                    s = sems["dma"][e][ndma % self.NDMA_SEM]
                    v = 16 * (ndma // self.NDMA_SEM + 1)
                    op["sig"] = (s, v)
                    op["prev"] = (s, v - 16)
                    ndma += 1
                elif op["signal"]:
                    cnt += 1
                    op["sig"] = (sems["cnt"][e], cnt)

        def make(e):
            def body(eng):
                waited = {}
                for op in ops[e]:
                    need = {}
                    for (e2, j) in op["deps"]:
                        p = ops[e2][j]
                        if p["sig"] is None:
                            continue
                        if e2 == e and e == "tensor" and not p["dma"]:
                            continue
                        s, v = p["sig"]
                        if need.get(id(s), (None, 0))[1] < v:
                            need[id(s)] = (s, v)
                    if op["dma"] and op["fn"] is not None:
                        s, v = op["prev"]
                        if v > 0 and need.get(id(s), (None, 0))[1] < v:
                            need[id(s)] = (s, v)
                    for sid, (s, v) in need.items():
                        if waited.get(sid, 0) < v:
                            eng.wait_ge(s, v)
                            waited[sid] = v
                    if op["fn"] is None:
                        continue
                    ins = op["fn"](eng)
                    if op["dma"]:
                        ins.then_inc(op["sig"][0], 16)
                    elif op["signal"]:
                        ins.then_inc(op["sig"][0], 1)
            return body

        block.sync(make("sync"))
        block.scalar(make("scalar"))
        block.tensor(make("tensor"))
        block.vector(make("vector"))
        block.gpsimd(make("gpsimd"))


class Arena:
    def __init__(self, t, nbytes):
        self.t = t
        self.cap = nbytes // 2
        self.off = 0
        self.peak = 0

    def alloc(self, shape, dtype):
        n = int(np.prod(shape))
        size = 4 if dtype == F32 else 2
        nel = n * size // 2
        nel = (nel + 15) // 16 * 16
        assert self.off + nel <= self.cap, f"arena overflow {self.off + nel} > {self.cap}"
        ap = self.t[:, self.off:self.off + n * size // 2]
        self.off += nel
        self.peak = max(self.peak, self.off)
        if dtype != BF16:
            ap = ap.bitcast(dtype)
        if len(shape) == 2:
            ap = ap.rearrange("p (a b) -> p a b", a=shape[0], b=shape[1])
        elif len(shape) == 3:
            ap = ap.rearrange("p (a b c) -> p a b c", a=shape[0], b=shape[1], c=shape[2])
        return ap

    def mark(self):
        return self.off

    def reset(self, m):
        self.off = m


def host_consts(S, TOPK):
    T = S // 128
    i = np.arange(128)
    c = {}
    c["ident"] = np.eye(128, dtype=np.float32)
    c["ident4"] = np.tile(np.eye(128, dtype=np.float32), (1, 4))
    c["cmT"] = np.where(i[:, None] > i[None, :], NEG, 0.0).astype(np.float32)
    c["cmQ"] = np.where(i[None, :] > i[:, None], NEG, 0.0).astype(np.float32)
    c["negm"] = np.where(i[None, :] > i[:, None], -1e30, 0.0).astype(np.float32)
    c["posm"] = np.where(i[None, :] > i[:, None], 1e30, 0.0).astype(np.float32)
    c["tri"] = (i[:, None] <= i[None, :]).astype(np.float32)
    c["ones"] = np.ones((128, 128), np.float32)
    sel0 = np.zeros((128, 128), np.float32)
    sel0[0, :] = 1.0
    c["sel0"] = sel0
    pos = np.arange(S, dtype=np.float32)

    def tab(rot):
        half = rot // 2
        inv = np.float32(500000.0) ** (-np.arange(half, dtype=np.float32) * np.float32(2.0) / np.float32(rot))
        ang = (pos[:, None] * inv[None, :]).astype(np.float32)
        cs = np.cos(ang).astype(np.float32).reshape(T, 128, half).transpose(1, 0, 2)
        sn = np.sin(ang).astype(np.float32).reshape(T, 128, half).transpose(1, 0, 2)
        return np.ascontiguousarray(cs), np.ascontiguousarray(sn)

    c["cosh"], c["sinh"] = tab(32)
    c["cosi"], c["sini"] = tab(16)
    c["cvec"] = np.tile((2.0 ** -(np.arange(NIT) + 1.0)).astype(np.float32)[None, :], (128, 1))
    return c


CONST_SHAPES = lambda S: {
    "ident": [128, 128], "ident4": [128, 512], "cmT": [128, 128], "cmQ": [128, 128],
    "negm": [128, 128], "posm": [128, 128], "tri": [128, 128], "ones": [128, 128],
    "sel0": [128, 128], "cosh": [128, S // 128, 16], "sinh": [128, S // 128, 16],
    "cosi": [128, S // 128, 8], "sini": [128, S // 128, 8], "cvec": [128, NIT],
}


def build(S, TOPK, passes="FDM2", dbg=None):
    T = S // 128
    NCH = S // 512
    KT0 = TOPK // 128
    nc = bass.Bass("TRN2", target_bir_lowering=False)
    dr = lambda n, s: nc.dram_tensor(n, s, F32, kind="ExternalInput").ap()
    x = dr("x", [S, D])
    g_pre = dr("norm_mix_pre", [D])
    w_in = dr("w_in", [D, 4940])
    b_forget = dr("b_forget", [4])
    b_gate = dr("b_gate", [2, D])
    w_bf = dr("w_branch_fox", [512, D])
    w_bd = dr("w_branch_dsa", [512, D])
    w_out = dr("w_out", [D, D])
    g_post = dr("norm_mix_post", [D])
    g_fpre = dr("norm_ffn_pre", [D])
    w_fg = dr("w_ffn_gate", [D, DFF])
    w_fu = dr("w_ffn_up", [D, DFF])
    w_fd = dr("w_ffn_down", [DFF, D])
    g_fpost = dr("norm_ffn_post", [D])
    cst = {k: dr(k, s) for k, s in CONST_SHAPES(S).items()}
    out = nc.dram_tensor("out", [S, D], F32, kind="ExternalOutput").ap()
    dbg_out = nc.dram_tensor("dbg", [128, 8 * S], F32, kind="ExternalOutput").ap() if dbg else None
    dbg_in = nc.dram_tensor("dbg_in", [128, 8 * S], F32, kind="ExternalInput").ap() if dbg in ("Mi", "2i", "Mif", "Mid") else None

    P = Prog()
    scale = float(HD ** -0.5)
    idx_scale = float((64 ** -0.5) * (8 ** -0.5))

    with ExitStack() as es:
        arena_t = es.enter_context(nc.sbuf_tensor("arena", [128, ARENA_BYTES // 2], BF16))
        A = Arena(arena_t, ARENA_BYTES)
        psf = [es.enter_context(nc.psum_tensor(f"ps{i}", [128, 512], F32)) for i in range(7)]
        pst = es.enter_context(nc.psum_tensor("pst", [128, 1024], BF16))
        sems = {"cnt": {e: es.enter_context(nc.semaphore("c_" + e)) for e in ENGS},
                "dma": {e: [es.enter_context(nc.semaphore(f"d_{e}{i}")) for i in range(Prog.NDMA_SEM)]
                        for e in ("sync", "gpsimd")}}
        sems["dma"]["scalar"] = []
        block = es.enter_context(nc.Block())

        def MM(out_, lhsT, rhs, start, stop, r, w):
            P.add("tensor", lambda e: e.matmul(out=out_, lhsT=lhsT, rhs=rhs, start=start, stop=stop), r=r, w=w)

        def TR(out_, in_, idn, r, w):
            P.add("tensor", lambda e: e.transpose(out=out_, in_=in_, identity=idn), r=r, w=w)

        def ACT(out_, in_, func, r, w, bias=0.0, scale_=1.0, accum=None):
            if accum is None:
                P.add("scalar", lambda e: e.activation(out=out_, in_=in_, func=func, bias=bias, scale=scale_), r=r, w=w)
            else:
                P.add("scalar", lambda e: e.activation(out=out_, in_=in_, func=func, bias=bias, scale=scale_,
                                                       accum_out=accum), r=r, w=w)

        def TS(eng, out_, in0, s1, s2, op0, op1, r, w, accum=None):
            if accum is not None:
                P.add(eng, lambda e: e.tensor_scalar(out=out_, in0=in0, scalar1=s1, scalar2=s2, op0=op0, op1=op1,
                                                     accum_out=accum), r=r, w=w)
            elif op1 is None:
                P.add(eng, lambda e: e.tensor_scalar(out=out_, in0=in0, scalar1=s1, scalar2=None, op0=op0), r=r, w=w)
            else:
                P.add(eng, lambda e: e.tensor_scalar(out=out_, in0=in0, scalar1=s1, scalar2=s2, op0=op0, op1=op1),
                      r=r, w=w)

        def TT(eng, out_, in0, in1, op, r, w):
            P.add(eng, lambda e: e.tensor_tensor(out=out_, in0=in0, in1=in1, op=op), r=r, w=w)

        def STT(out_, in0, sc, in1, op0, op1, r, w):
            P.add("vector", lambda e: e.scalar_tensor_tensor(out=out_, in0=in0, scalar=sc, in1=in1, op0=op0, op1=op1),
                  r=r, w=w)

        def CP(eng, out_, in_, r, w):
            if eng == "scalar":
                P.add(eng, lambda e: e.copy(out=out_, in_=in_), r=r, w=w)
            else:
                P.add(eng, lambda e: e.tensor_copy(out=out_, in_=in_), r=r, w=w)

        def RCP(out_, in_, r, w):
            P.add("vector", lambda e: e.reciprocal(out=out_, in_=in_), r=r, w=w)

        def RED(out_, in_, op, r, w):
            P.add("vector", lambda e: e.tensor_reduce(out=out_, in_=in_, axis=AX.X, op=op), r=r, w=w)

        def MS(eng, ap, val, w):
            P.add(eng, lambda e: e.memset(ap, val), w=w)

        def DMA(eng, out_, in_, r, w):
            P.add(eng, lambda e: e.dma_start(out=out_, in_=in_), r=r, w=w, dma=True)

        rot_state = {"i": 0}

        def rot(nb=3):
            i = rot_state["i"] % nb
            rot_state["i"] += 1
            return psf[i], f"ps{i}"

        identb = A.alloc([128], BF16)
        ident4b = A.alloc([512], BF16)
        cmTb = A.alloc([128], BF16)
        cmQb = A.alloc([128], BF16)
        onesb = A.alloc([128], BF16)
        negm = A.alloc([128], F32)
        posm = A.alloc([128], F32)
        cosh = A.alloc([T, 16], F32)
        sinh = A.alloc([T, 16], F32)
        cosi = A.alloc([T, 8], F32)
        sini = A.alloc([T, 8], F32)
        cvec = A.alloc([NIT], F32)
        nhalf = A.alloc([1], F32)
        gpre = A.alloc([D], F32)
        ofT = A.alloc([4, S], BF16)
        odT = A.alloc([4, S], BF16)
        for ap_, nm in ((identb, "ident"), (ident4b, "ident4"), (cmTb, "cmT"), (cmQb, "cmQ"), (onesb, "ones")):
            DMA("gpsimd", ap_, cst[nm], r=[], w=["c_" + nm + "b"])
        for ap_, nm in ((negm, "negm"), (posm, "posm"), (cosh, "cosh"), (sinh, "sinh"), (cosi, "cosi"), (sini, "sini"),
                        (cvec, "cvec")):
            DMA("sync", ap_, cst[nm], r=[], w=["c_" + nm])
        DMA("sync", gpre, g_pre.partition_broadcast(128), r=[], w=["gpre"])
        MS("gpsimd", nhalf, -0.5, w=["nhalf"])
        persist_mark = A.mark()

        def u_stage(t, j, bufs, gain, gain_key, src_key=None, src_ap=None):
            s = t % 2
            xs = t % len(bufs["xt"])
            xt, ssq, rstd, ub, uT = bufs["xt"][xs], bufs["ss"][s], bufs["rstd"][s], bufs["ub"][s], bufs["uT"]
            if src_ap is None:
                DMA("sync", xt, x[t * 128:(t + 1) * 128, :], r=[], w=[("xt", xs)])
                src_ap, src_key = xt, ("xt", xs)
            ACT(ub, src_ap, AF.Square, r=[src_key], w=[("ss", s), ("ub", s)], accum=ssq)
            TS("gpsimd", rstd, ssq, 1.0 / D, EPS, ALU.mult, ALU.add, r=[("ss", s)], w=[("rstd", s)])
            TT("gpsimd", rstd, rstd, nhalf, ALU.pow, r=[("rstd", s), "nhalf"], w=[("rstd", s)])
            STT(ub, src_ap, rstd, gain, ALU.mult, ALU.mult, r=[src_key, ("rstd", s), gain_key], w=[("ub", s)])
            for kc in range(KC):
                TR(pst[:, kc * 128:(kc + 1) * 128], ub[:, kc * 128:(kc + 1) * 128], identb,
                   r=[("ub", s), "c_identb"], w=["pst"])
            CP("vector", uT[:, :, j * 128:(j + 1) * 128], pst[:].rearrange("p (a b) -> p a b", a=KC, b=128),
               r=["pst"], w=[("uT", j)])

        def u_bufs(nx=2):
            return dict(xt=[A.alloc([D], F32) for _ in range(nx)], ss=[A.alloc([1], F32) for _ in range(2)],
                        rstd=[A.alloc([1], F32) for _ in range(2)], ub=[A.alloc([D], BF16) for _ in range(2)],
                        uT=A.alloc([KC, 512], BF16))

        w_in_r = w_in.rearrange("(kc p) c -> p kc c", p=128)

        if "F" in passes:
            tri = A.alloc([128], F32)
            onesf = A.alloc([128], F32)
            sel0 = A.alloc([128], F32)
            for ap_, nm in ((tri, "tri"), (onesf, "ones"), (sel0, "sel0")):
                DMA("sync", ap_, cst[nm], r=[], w=["c_" + nm])
            WF = A.alloc([KC, 1540], BF16)
            KfT = A.alloc([4, S], BF16)
            Vf = A.alloc([T, 512], BF16)
            negc = A.alloc([T, 4], F32)
            biasc = A.alloc([T, 4], F32)
            bfb = A.alloc([4, 4], F32)
            carry = A.alloc([4], F32)
            refb = A.alloc([4], F32)
            v3 = lambda a_: a_.rearrange("p (a b) -> p a b", a=4, b=4)
            ztf = A.alloc([16], F32)
            etf = A.alloc([16], F32)
            ltf = A.alloc([16], F32)
            Lsf = A.alloc([16], F32)
            zt, et, lt, Ls = v3(ztf), v3(etf), v3(ltf), v3(Lsf)
            ltot = A.alloc([4], F32)
            QfT = A.alloc([4, 512], BF16)
            pT = [A.alloc([512], BF16) for _ in range(2)]
            rden = A.alloc([512], F32)
            ub_ = u_bufs()
            for half in range(2):
                DMA("gpsimd", WF[:, half * 4:(half + 1) * 4, :], w_in_r[:, half * 4:(half + 1) * 4, 0:1540],
                    r=[], w=[("WF", half)])
            WFK = [("WF", 0), ("WF", 1)]
            for jj in range(4):
                DMA("sync", bfb[:, jj, :], b_forget.partition_broadcast(128), r=[], w=[("bfb", jj)])
            MS("gpsimd", carry, 0.0, w=["carry"])
            MS("gpsimd", Ls[:, 0, :], 0.0, w=["Ls0"])
            oset = 0
            for c in range(NCH):
                for j in range(4):
                    u_stage(4 * c + j, j, ub_, gpre, "gpre")
                uT = ub_["uT"]
                uTk = [("uT", j) for j in range(4)]
                for g in range(8):
                    bank, bk = rot()
                    for kc in range(KC):
                        MM(bank[:], WF[:, kc, g * 128:(g + 1) * 128], uT[:, kc, :], kc == 0, kc == KC - 1,
                           r=WFK + uTk, w=[bk])
                    if g < 4:
                        CP("vector", QfT[:, g, :], bank[:], r=[bk], w=[("QfT", g)])
                    else:
                        CP("vector", KfT[:, g - 4, c * 512:(c + 1) * 512], bank[:], r=[bk], w=[("KfT", g - 4, c)])
                fbank, fbk = psf[3], "ps3"
                for j in range(4):
                    t = 4 * c + j
                    bank, bk = rot()
                    for kc in range(KC):
                        MM(bank[:], uT[:, kc, j * 128:(j + 1) * 128], WF[:, kc, 1024:1536], kc == 0, kc == KC - 1,
                           r=WFK + [("uT", j)], w=[bk])
                    CP("vector", Vf[:, t, :], bank[:], r=[bk], w=[("Vf", t)])
                    for kc in range(KC):
                        MM(fbank[:, j * 4:(j + 1) * 4], uT[:, kc, j * 128:(j + 1) * 128], WF[:, kc, 1536:1540],
                           kc == 0, kc == KC - 1, r=WFK + [("uT", j)], w=[fbk])
                TT("vector", zt, fbank[:, 0:16].rearrange("p (a b) -> p a b", a=4, b=4), bfb, ALU.add,
                   r=[fbk] + [("bfb", jj) for jj in range(4)], w=["zt"])
                ACT(et, zt, AF.Exp, r=["zt"], w=["et"], scale_=-1.0)
                ACT(lt, et, AF.Ln, r=["et"], w=["lt"], bias=1.0)
                CP("gpsimd", Ls[:, 1, :], lt[:, 0, :], r=["lt"], w=["Ls1"])
                TT("gpsimd", Ls[:, 2, :], Ls[:, 1, :], lt[:, 1, :], ALU.add, r=["lt", "Ls1"], w=["Ls2"])
                TT("gpsimd", Ls[:, 3, :], Ls[:, 2, :], lt[:, 2, :], ALU.add, r=["lt", "Ls2"], w=["Ls3"])
                TT("gpsimd", ltot, Ls[:, 3, :], lt[:, 3, :], ALU.add, r=["lt", "Ls3"], w=["ltot"])
                cb, cbk = psf[4], "ps4"
                MM(cb[:, 0:16], tri, ltf, True, False, r=["c_tri", "lt"], w=[cbk])
                MM(cb[:, 0:16], onesf, Lsf, False, True, r=["c_ones", "Ls0", "Ls1", "Ls2", "Ls3"], w=[cbk])
                MM(cb[:, 16:20], onesf, ltot, True, True, r=["c_ones", "ltot"], w=[cbk])
                TT("vector", negc[:, 4 * c:4 * c + 4, :], cb[:, 0:16].rearrange("p (a b) -> p a b", a=4, b=4),
                   carry.unsqueeze(1).broadcast_to([128, 4, 4]), ALU.add, r=[cbk, "carry"], w=[("negc", c)])
                TT("vector", carry, cb[:, 16:20], carry, ALU.add, r=[cbk, "carry"], w=["carry"])
                rb, rbk = psf[5], "ps5"
                MM(rb[:, 0:4], sel0, negc[:, 4 * c + 2, :], True, True, r=["c_sel0", ("negc", c)], w=[rbk])
                CP("vector", refb, rb[:, 0:4], r=[rbk], w=["refb"])
                nkt = 4 * c + 4
                for h in range(4):
                    TS("vector", biasc[:, 0:nkt, h], negc[:, 0:nkt, h], refb[:, h:h + 1], None, ALU.subtract, None,
                       r=[("negc", cc) for cc in range(c + 1)] + ["refb"], w=[("biasc", h)])
                for h in range(4):
                    ob, obk, db, dbk = (psf[3], "ps3", psf[4], "ps4") if oset == 0 else (psf[5], "ps5", psf[6], "ps6")
                    oset ^= 1
                    for kt in range(nkt):
                        off = max(0, kt - 4 * c) * 128
                        diag = kt >= 4 * c
                        bank, bk = rot()
                        MM(bank[:, off:512], KfT[:, h, kt * 128:(kt + 1) * 128], QfT[:, h, off:512], True, not diag,
                           r=[("KfT", h, kt // 4), ("QfT", h)], w=[bk])
                        if diag:
                            MM(bank[:, off:off + 128], identb, cmTb, False, True, r=["c_identb", "c_cmTb"], w=[bk])
                        ps_ = kt % 2
                        ACT(pT[ps_][:, off:512], bank[:, off:512], AF.Exp, r=[bk, ("biasc", h)], w=[("pT", ps_)],
                            bias=biasc[:, kt, h:h + 1], scale_=scale)
                        MM(ob[:, off:512], Vf[:, kt, h * 128:(h + 1) * 128], pT[ps_][:, off:512], kt == 0,
                           kt == nkt - 1, r=[("Vf", kt), ("pT", ps_)], w=[obk])
                        MM(db[:, off:512], onesb, pT[ps_][:, off:512], kt == 0, kt == nkt - 1,
                           r=["c_onesb", ("pT", ps_)], w=[dbk])
                    RCP(rden, db[:], r=[dbk], w=["rden"])
                    TT("vector", ofT[:, h, c * 512:(c + 1) * 512], ob[:], rden, ALU.mult, r=[obk, "rden"],
                       w=[("ofT", h, c)])
            P.barrier()
            A.reset(persist_mark)
            if dbg == "F":
                DMA("gpsimd", dbg_out[:, 0:4 * S], ofT.rearrange("p a b -> p (a b)"), r=[], w=["dbgF"])
                P.add("sync", None, r=["dbgF"])
                P.barrier()

        if "D" in passes:
            import os as _os
            if _os.environ.get("K_DPAD"):
                A.alloc([int(_os.environ["K_DPAD"])], BF16)
            WD_ = A.alloc([KC, 1352], BF16)
            KdT = A.alloc([S], BF16)
            Vd = A.alloc([T, 128], BF16)
            kiT = A.alloc([S], BF16)
            QQ = [A.alloc([1024], BF16) for _ in range(2)]
            sgnD = [A.alloc([8, 128], BF16) for _ in range(1)]
            sc = A.alloc([S], F32)
            Mneg = [A.alloc([S], BF16) for _ in range(2)]
            R = [A.alloc([8, 512], BF16) for _ in range(1)]
            qd_f = A.alloc([4, 128], F32)
            qi_f = A.alloc([8, 64], F32)
            g4_f = A.alloc([328], F32)
            qd_b = A.alloc([4, 128], BF16)
            qi_b = A.alloc([8, 64], BF16)
            kk_b = A.alloc([256], BF16)
            rt = [A.alloc([8, 16], F32) for _ in range(4)]
            aw = A.alloc([8], F32)
            sg01 = A.alloc([8], F32)
            sgn = A.alloc([8], F32)
            tmpd = A.alloc([128], F32)
            st = A.alloc([8], F32)
            Wc = A.alloc([NIT], F32)
            mids = A.alloc([NIT + 1], F32)
            cnts = A.alloc([NIT], F32)
            sgs = A.alloc([NIT], F32)
            pT = [A.alloc([512], BF16) for _ in range(2)]
            rden = A.alloc([512], F32)
            ub_ = u_bufs(1)
            for (d0, s0, n_) in ((0, 1540, 512), (512, 2308, 512), (1024, 2052, 256), (1280, 2820, 72))[:int(_os.environ.get("K_DNDMA", "4"))]:
                DMA("gpsimd", WD_[:, :, d0:d0 + n_], w_in_r[:, :, s0:s0 + n_], r=[], w=[("WD", d0)])
            WDK = [("WD", 0), ("WD", 512), ("WD", 1024), ("WD", 1280)]

            def rope(src3, dst3, nh, half, cos_t, sin_t, key_src, key_dst):
                x1 = src3[:, :, 0:half]
                x2 = src3[:, :, half:2 * half]
                cb_ = cos_t.unsqueeze(1).broadcast_to([128, nh, half])
                sb_ = sin_t.unsqueeze(1).broadcast_to([128, nh, half])
                ta, tb, tc, td = [r_[:, 0:nh, 0:half] for r_ in rt]
                TT("gpsimd", ta, x1, cb_, ALU.mult, r=[key_src], w=["rt0"])
                TT("gpsimd", tb, x2, sb_, ALU.mult, r=[key_src], w=["rt1"])
                TT("gpsimd", tc, x2, cb_, ALU.mult, r=[key_src], w=["rt2"])
                TT("gpsimd", td, x1, sb_, ALU.mult, r=[key_src], w=["rt3"])
                TT("gpsimd", dst3[:, :, 0:half], ta, tb, ALU.subtract, r=["rt0", "rt1"], w=[key_dst])
                TT("gpsimd", dst3[:, :, half:2 * half], tc, td, ALU.add, r=["rt2", "rt3"], w=[key_dst])

            import os as _os
            _dstop = int(_os.environ.get("K_DSTOP", "0"))
            for t in range(T if _dstop != 1 else 0):
                c, j = t // 4, t % 4
                qs = t % 2
                if j == 0:
                    for jj in range(4):
                        u_stage(4 * c + jj, jj, ub_, gpre, "gpre")
                uT = ub_["uT"]
                n = (t + 1) * 128
                for (dst, c0, wn, key) in ((qd_f, 0, 512, "qd_f"), (qi_f, 512, 512, "qi_f"), (g4_f, 1024, 328, "g4_f")):
                    bank, bk = rot()
                    for kc in range(KC):
                        MM(bank[:, 0:wn], uT[:, kc, j * 128:(j + 1) * 128], WD_[:, kc, c0:c0 + wn], kc == 0,
                           kc == KC - 1, r=WDK + [("uT", j)], w=[bk])
                    dflat = dst if key == "g4_f" else dst.rearrange("p a b -> p (a b)")
                    CP("scalar", dflat, bank[:, 0:wn], r=[bk], w=[key])
                rope(qd_f, qd_b, 4, 16, cosh[:, t, :], sinh[:, t, :], "qd_f", "qd_b")
                CP("gpsimd", qd_b[:, :, 32:128], qd_f[:, :, 32:128], r=["qd_f"], w=["qd_b"])
                TS("gpsimd", sg01, g4_f[:, 320:328], 0.0, None, ALU.is_ge, None, r=["g4_f"], w=["sg01"])
                TS("gpsimd", sgn, sg01, 2.0, -1.0, ALU.mult, ALU.add, r=["sg01"], w=["sgn"])
                TS("gpsimd", aw, sg01, 2.0 * idx_scale, -idx_scale, ALU.mult, ALU.add, r=["sg01"], w=["aw"])
                TT("gpsimd", aw, aw, g4_f[:, 320:328], ALU.mult, r=["aw", "g4_f"], w=["aw"])
                rope(qi_f, qi_f, 8, 8, cosi[:, t, :], sini[:, t, :], "qi_f", "qi_f")
                TT("gpsimd", qi_b, qi_f, aw.unsqueeze(2).broadcast_to([128, 8, 64]), ALU.mult, r=["qi_f", "aw"],
                   w=["qi_b"])
                kd3 = g4_f[:, 0:128].rearrange("p (a b) -> p a b", a=1, b=128)
                kdb3 = kk_b[:, 0:128].rearrange("p (a b) -> p a b", a=1, b=128)
                rope(kd3, kdb3, 1, 16, cosh[:, t, :], sinh[:, t, :], "g4_f", "kk_b")
                CP("gpsimd", kk_b[:, 32:128], g4_f[:, 32:128], r=["g4_f"], w=["kk_b"])
                ki3 = g4_f[:, 256:320].rearrange("p (a b) -> p a b", a=1, b=64)
                kib3 = kk_b[:, 128:192].rearrange("p (a b) -> p a b", a=1, b=64)
                rope(ki3, kib3, 1, 8, cosi[:, t, :], sini[:, t, :], "g4_f", "kk_b")
                CP("gpsimd", kk_b[:, 144:192], g4_f[:, 272:320], r=["g4_f"], w=["kk_b"])
                CP("gpsimd", kk_b[:, 192:256], kk_b[:, 128:192], r=["kk_b"], w=["kk_b2"])
                CP("gpsimd", Vd[:, t, :], g4_f[:, 128:256], r=["g4_f"], w=[("Vd", t)])
                TT("gpsimd", sgnD[0], identb.unsqueeze(1).broadcast_to([128, 8, 128]),
                   sgn.unsqueeze(2).broadcast_to([128, 8, 128]), ALU.mult, r=["c_identb", "sgn"], w=[("sgnD", 0)])
                for i_ in range(2):
                    TR(pst[:, i_ * 128:(i_ + 1) * 128], kk_b[:, i_ * 128:(i_ + 1) * 128], identb,
                       r=["kk_b", "kk_b2", "c_identb"], w=["pst"])
                CP("scalar", KdT[:, t * 128:(t + 1) * 128], pst[:, 0:128], r=["pst"], w=[("KdT", t)])
                CP("scalar", kiT[:, t * 128:(t + 1) * 128], pst[:, 128:256], r=["pst"], w=[("kiT", t)])
                qdb2 = qd_b.rearrange("p a b -> p (a b)")
                qib2 = qi_b.rearrange("p a b -> p (a b)")
                for i_ in range(4):
                    TR(pst[:, i_ * 128:(i_ + 1) * 128], qdb2[:, i_ * 128:(i_ + 1) * 128], identb,
                       r=["qd_b", "c_identb"], w=["pst"])
                for i_ in range(4):
                    TR(pst[:, 512 + i_ * 128:512 + (i_ + 1) * 128], qib2[:, i_ * 128:(i_ + 1) * 128], identb,
                       r=["qi_b", "c_identb"], w=["pst"])
                CP("scalar", QQ[qs], pst[:], r=["pst"], w=[("QQ", qs)])
                ms_ = t % 2
                if t >= KT0:
                    for k5 in range((n + 511) // 512):
                        c0 = k5 * 512
                        wn = min(512, n - c0)
                        rs = 0
                        kik = [("kiT", tt) for tt in range(c0 // 128, (c0 + wn) // 128)]
                        for hd in range(8):
                            hp, hf = hd // 2, hd % 2
                            bank, bk = rot()
                            MM(bank[:, 0:wn], QQ[qs][64 * hf:64 * hf + 64, 512 + hp * 128:512 + (hp + 1) * 128],
                               kiT[64 * hf:64 * hf + 64, c0:c0 + wn], True, True, r=[("QQ", qs)] + kik, w=[bk])
                            ACT(R[rs][:, hd, 0:wn], bank[:, 0:wn], AF.Relu, r=[bk], w=[("R", rs, hd)])
                        sb_, sbk = psf[3], "ps3"
                        for hd in range(8):
                            MM(sb_[:, 0:wn], sgnD[0][:, hd, :], R[rs][:, hd, 0:wn], hd == 0, hd == 7,
                               r=[("sgnD", 0), ("R", rs, hd)], w=[sbk])
                        CP("scalar", sc[:, c0:c0 + wn], sb_[:, 0:wn], r=[sbk], w=[("sc", k5)])
                    nk5 = (n + 511) // 512
                    sck = [("sc", k5) for k5 in range(nk5)]
                    TT("gpsimd", tmpd, sc[:, n - 128:n], posm, ALU.add, r=sck + ["c_posm"], w=["tmpd"])
                    TT("gpsimd", sc[:, n - 128:n], sc[:, n - 128:n], negm, ALU.add, r=sck + ["c_negm", "tmpd"],
                       w=[("sc", nk5 - 1)])
                    RED(st[:, 0:1], sc[:, 0:n], ALU.max, r=sck, w=["hi"])
                    RED(st[:, 1:2], tmpd, ALU.min, r=["tmpd"], w=["m1"])
                    RED(st[:, 2:3], sc[:, 0:n - 128], ALU.min, r=sck, w=["m2"])
                    TT("vector", st[:, 3:4], st[:, 1:2], st[:, 2:3], ALU.min, r=["m1", "m2"], w=["lo"])
                    TT("vector", st[:, 4:5], st[:, 0:1], st[:, 3:4], ALU.subtract, r=["hi", "lo"], w=["Wd"])
                    TS("vector", Wc, cvec, st[:, 4:5], None, ALU.mult, None, r=["c_cvec", "Wd"], w=["Wc"])
                    TT("vector", mids[:, 0:1], st[:, 3:4], Wc[:, 0:1], ALU.add, r=["lo", "Wc"], w=[("mid", 0)])
                    for it in range(NIT):
                        TS("vector", Mneg[ms_][:, 0:n], sc[:, 0:n], mids[:, it:it + 1], None, ALU.is_ge, ALU.add,
                           r=sck + [("mid", it)], w=[("cnt", it), ("Mneg", ms_)], accum=cnts[:, it:it + 1])
                        TS("vector", sgs[:, it:it + 1], cnts[:, it:it + 1], TOPK - 0.5, 0.5, ALU.is_ge, ALU.subtract,
                           r=[("cnt", it)], w=[("sg", it)])
                        STT(mids[:, it + 1:it + 2], sgs[:, it:it + 1], Wc[:, it:it + 1], mids[:, it:it + 1], ALU.mult,
                            ALU.add, r=[("sg", it), "Wc", ("mid", it)], w=[("mid", it + 1)])
                    STT(st[:, 5:6], st[:, 4:5], -(2.0 ** -(NIT + 1)), mids[:, NIT:NIT + 1], ALU.mult, ALU.add,
                        r=["Wd", ("mid", NIT)], w=["tau"])
                    TS("vector", Mneg[ms_][:, 0:n], sc[:, 0:n], st[:, 5:6], NEG, ALU.is_lt, ALU.mult,
                       r=sck + ["tau"], w=[("Mneg", ms_)])
                else:
                    if n > 128:
                        MS("gpsimd", Mneg[ms_][:, 0:n - 128], 0.0, w=[("Mneg", ms_)])
                    CP("gpsimd", Mneg[ms_][:, n - 128:n], cmQb, r=["c_cmQb"], w=[("Mneg", ms_)])
                ob, obk, db, dbk = psf[4], "ps4", psf[5], "ps5"
                for kt in range(t + 1):
                    bank, bk = rot()
                    MM(bank[:], KdT[:, kt * 128:(kt + 1) * 128], QQ[qs][:, 0:512], True, False,
                       r=[("KdT", kt), ("QQ", qs)], w=[bk])
                    MM(bank[:], Mneg[ms_][:, kt * 128:(kt + 1) * 128], ident4b, False, True,
                       r=[("Mneg", ms_), "c_ident4b"], w=[bk])
                    ps_ = kt % 2
                    ACT(pT[ps_], bank[:], AF.Exp, r=[bk], w=[("pT", ps_)], scale_=scale)
                    MM(ob[:], Vd[:, kt, :], pT[ps_], kt == 0, kt == t, r=[("Vd", kt), ("pT", ps_)], w=[obk])
                    MM(db[:], onesb, pT[ps_], kt == 0, kt == t, r=["c_onesb", ("pT", ps_)], w=[dbk])
                RCP(rden, db[:], r=[dbk], w=["rden"])
                TT("vector", odT[:, :, t * 128:(t + 1) * 128], ob[:].rearrange("p (a b) -> p a b", a=4, b=128),
                   rden.rearrange("p (a b) -> p a b", a=4, b=128), ALU.mult, r=[obk, "rden"], w=[("odT", t)])
            P.barrier()
            A.reset(persist_mark)
            if dbg in ("D", "Y"):
                if dbg == "D":
                    DMA("gpsimd", dbg_out[:, 4 * S:8 * S], odT.rearrange("p a b -> p (a b)"), r=[], w=["dbgD"])
                if dbg == "Y":
                    DMA("gpsimd", dbg_out[:, 0:4 * S], ofT.rearrange("p a b -> p (a b)"), r=[], w=["dbgD"])
                P.add("sync", None, r=["dbgD"])
                P.barrier()

        if dbg in ("Mi", "2i", "Mif", "Mid"):
            if dbg != "Mif":
                DMA("gpsimd", ofT.rearrange("p a b -> p (a b)"), dbg_in[:, 0:4 * S], r=[], w=["dbgi"])
            if dbg != "Mid":
                DMA("gpsimd", odT.rearrange("p a b -> p (a b)"), dbg_in[:, 4 * S:8 * S], r=[], w=["dbgi2"])
            P.barrier()
        if "M" in passes:
            Wg = A.alloc([KC, 2048], BF16)
            Wbf = A.alloc([4, D], BF16)
            Wbd = A.alloc([4, D], BF16)
            identf = A.alloc([128], F32)
            DMA("sync", identf, cst["ident"], r=[], w=["c_ident"])
            bg16 = A.alloc([128], F32)
            bgT = A.alloc([16], F32)
            sigf = A.alloc([512], F32)
            sigd = A.alloc([512], F32)
            t1 = A.alloc([512], F32)
            t2 = A.alloc([512], F32)
            mixt = A.alloc([8, 512], BF16)
            ub_ = u_bufs()
            for q4 in range(4):
                DMA("gpsimd", Wg[:, q4 * 2:(q4 + 1) * 2, :], w_in_r[:, q4 * 2:(q4 + 1) * 2, 2892:4940], r=[],
                    w=[("Wg", q4)])
            WGK = [("Wg", q4) for q4 in range(4)]
            DMA("gpsimd", Wbf, w_bf.rearrange("(h p) c -> p h c", p=128), r=[], w=["Wbf"])
            DMA("gpsimd", Wbd, w_bd.rearrange("(h p) c -> p h c", p=128), r=[], w=["Wbd"])
            DMA("sync", bg16[0:16, :], b_gate.rearrange("b (kc p) -> (b kc) p", p=128), r=[], w=["bg16"])
            tb_, tbk = psf[6], "ps6"
            TR(tb_[:, 0:16], bg16[0:16, :], identf[0:16, 0:16], r=["bg16", "c_ident"], w=[tbk])
            CP("vector", bgT, tb_[:, 0:16], r=[tbk], w=["bgT"])
            for c in range(NCH):
                for j in range(4):
                    u_stage(4 * c + j, j, ub_, gpre, "gpre")
                uT = ub_["uT"]
                uTk = [("uT", j) for j in range(4)]
                cs = slice(c * 512, (c + 1) * 512)
                for cc in range(8):
                    bA, kA = rot(7)
                    for kc in range(KC):
                        MM(bA[:], Wg[:, kc, cc * 128:(cc + 1) * 128], uT[:, kc, :], kc == 0, kc == KC - 1,
                           r=WGK + uTk, w=[kA])
                    bB, kB = rot(7)
                    for kc in range(KC):
                        MM(bB[:], Wg[:, kc, 1024 + cc * 128:1024 + (cc + 1) * 128], uT[:, kc, :], kc == 0,
                           kc == KC - 1, r=WGK + uTk, w=[kB])
                    bC, kCk = rot(7)
                    for h in range(4):
                        MM(bC[:], Wbf[:, h, cc * 128:(cc + 1) * 128], ofT[:, h, cs], h == 0, h == 3,
                           r=["Wbf", ("ofT", h, c)], w=[kCk])
                    bD, kDk = rot(7)
                    for h in range(4):
                        MM(bD[:], Wbd[:, h, cc * 128:(cc + 1) * 128], odT[:, h, cs], h == 0, h == 3,
                           r=["Wbd", ("odT", h, c)], w=[kDk])
                    ACT(sigf, bA[:], AF.Sigmoid, r=[kA, "bgT"], w=["sigf"], bias=bgT[:, cc:cc + 1])
                    ACT(sigd, bB[:], AF.Sigmoid, r=[kB, "bgT"], w=["sigd"], bias=bgT[:, 8 + cc:9 + cc])
                    TT("vector", t1, sigf, bC[:], ALU.mult, r=["sigf", kCk], w=["t1"])
                    TT("vector", t2, sigd, bD[:], ALU.mult, r=["sigd", kDk], w=["t2"])
                    TT("vector", mixt[:, cc, :], t1, t2, ALU.add, r=["t1", "t2"], w=[("mixt", cc)])
                CP("gpsimd", ofT[:, :, cs], mixt[:, 0:4, :], r=[("mixt", cc) for cc in range(4)],
                   w=[("ofT", h, c) for h in range(4)])
                CP("gpsimd", odT[:, :, cs], mixt[:, 4:8, :], r=[("mixt", cc) for cc in range(4, 8)],
                   w=[("odT", h, c) for h in range(4)])
            P.barrier()
            A.reset(persist_mark)
            if dbg in ("M", "Mi", "Mif", "Mid"):
                DMA("gpsimd", dbg_out[:, 0:4 * S], ofT.rearrange("p a b -> p (a b)"), r=[], w=["dbgF"])
                DMA("gpsimd", dbg_out[:, 4 * S:8 * S], odT.rearrange("p a b -> p (a b)"), r=[], w=["dbgD"])
                P.add("sync", None, r=["dbgF", "dbgD"])
                P.barrier()

        if "2" in passes:
            Wo = A.alloc([KC, D], BF16)
            gpost = A.alloc([D], F32)
            g3 = A.alloc([D], F32)
            g4 = A.alloc([D], F32)
            hbuf = A.alloc([4, D], F32)
            ffb = A.alloc([4, D], F32)
            tmpy = A.alloc([512], F32)
            vb = A.alloc([D], BF16)
            vT = A.alloc([KC, 512], BF16)
            WGU = [A.alloc([KC, 512], BF16) for _ in range(2)]
            WDp = [A.alloc([2, 512], BF16) for _ in range(2)]
            sgb = [A.alloc([512], F32) for _ in range(2)]
            actT = A.alloc([NFT, 512], BF16)
            junk = A.alloc([D], BF16)
            ssy = A.alloc([4, 2], F32)
            ssh = A.alloc([4], F32)
            ssf = A.alloc([4, 2], F32)
            rs1 = A.alloc([4], F32)
            rs2 = A.alloc([4], F32)
            rs3 = A.alloc([4], F32)
            DMA("gpsimd", Wo, w_out.rearrange("(kc p) c -> p kc c", p=128), r=[], w=["Wo"])
            DMA("sync", gpost, g_post.partition_broadcast(128), r=[], w=["gpost"])
            DMA("sync", g3, g_fpre.partition_broadcast(128), r=[], w=["g3"])
            DMA("sync", g4, g_fpost.partition_broadcast(128), r=[], w=["g4"])
            w_fg_r = w_fg.rearrange("(kc p) f -> p kc f", p=128)
            w_fu_r = w_fu.rearrange("(kc p) f -> p kc f", p=128)
            w_fd_r = w_fd.rearrange("(ft p) c -> p ft c", p=128)
            npiece = NFT // 2
            wgu_n = 0
            wd_n = 0

            def mixT(kc, sl):
                return ofT[:, kc, sl] if kc < 4 else odT[:, kc - 4, sl]

            import os as _os
            _p2 = int(_os.environ.get("K_P2", "9"))
            for c in range(NCH):
                for j in range(4):
                    t = 4 * c + j
                    ts_ = slice(t * 128, (t + 1) * 128)
                    DMA("sync", hbuf[:, j, :], x[ts_, :], r=[], w=[("h", j)])
                    banks = []
                    for hf in range(2):
                        bank, bk = rot(7)
                        banks.append((bank, bk))
                        for kc in range(KC):
                            MM(bank[:], mixT(kc, ts_), Wo[:, kc, hf * 512:(hf + 1) * 512], kc == 0, kc == KC - 1,
                               r=["Wo"], w=[bk])
                        ACT(junk[:, 0:512], bank[:], AF.Square, r=[bk], w=[("ssy", j, hf)], accum=ssy[:, j, hf:hf + 1])
                    TT("gpsimd", rs1[:, j:j + 1], ssy[:, j, 0:1], ssy[:, j, 1:2], ALU.add,
                       r=[("ssy", j, 0), ("ssy", j, 1)], w=[("rs1", j)])
                    TS("gpsimd", rs1[:, j:j + 1], rs1[:, j:j + 1], 1.0 / D, EPS, ALU.mult, ALU.add, r=[("rs1", j)],
                       w=[("rs1", j)])
                    TT("gpsimd", rs1[:, j:j + 1], rs1[:, j:j + 1], nhalf, ALU.pow, r=[("rs1", j), "nhalf"],
                       w=[("rs1", j)])
                    for hf in range(2):
                        bank, bk = banks[hf]
                        hs = slice(hf * 512, (hf + 1) * 512)
                        STT(tmpy, bank[:], rs1[:, j:j + 1], gpost[:, hs], ALU.mult, ALU.mult,
                            r=[bk, ("rs1", j), "gpost"], w=["tmpy"])
                        TT("vector", hbuf[:, j, hs], hbuf[:, j, hs], tmpy, ALU.add, r=["tmpy", ("h", j)],
                           w=[("h", j)])
                    ACT(junk, hbuf[:, j, :], AF.Square, r=[("h", j)], w=[("ssh", j)], accum=ssh[:, j:j + 1])
                    TS("gpsimd", rs2[:, j:j + 1], ssh[:, j:j + 1], 1.0 / D, EPS, ALU.mult, ALU.add, r=[("ssh", j)],
                       w=[("rs2", j)])
                    TT("gpsimd", rs2[:, j:j + 1], rs2[:, j:j + 1], nhalf, ALU.pow, r=[("rs2", j), "nhalf"],
                       w=[("rs2", j)])
                    STT(vb, hbuf[:, j, :], rs2[:, j:j + 1], g3, ALU.mult, ALU.mult,
                        r=[("h", j), ("rs2", j), "g3"], w=["vb"])
                    for kc in range(KC):
                        TR(pst[:, kc * 128:(kc + 1) * 128], vb[:, kc * 128:(kc + 1) * 128], identb,
                           r=["vb", "c_identb"], w=["pst"])
                    CP("vector", vT[:, :, j * 128:(j + 1) * 128], pst[:].rearrange("p (a b) -> p a b", a=KC, b=128),
                       r=["pst"], w=[("vT", j)])
                vTk = [("vT", j) for j in range(4)]
                if _p2 < 1:
                    for j in range(4):
                        DMA("sync", out[(4 * c + j) * 128:(4 * c + j + 1) * 128, :], hbuf[:, j, :], r=[("h", j)], w=[("out", 4 * c + j)])
                    continue
                for p_ in range(npiece):
                    sl = wgu_n % 2
                    wgu_n += 1
                    f0 = p_ * 256
                    DMA("gpsimd", WGU[sl][:, :, 0:256], w_fg_r[:, :, f0:f0 + 256], r=[], w=[("WGUg", sl)])
                    DMA("gpsimd", WGU[sl][:, :, 256:512], w_fu_r[:, :, f0:f0 + 256], r=[], w=[("WGUu", sl)])
                    for f2 in range(2):
                        ft = 2 * p_ + f2
                        bG, kG = rot(7)
                        for kc in range(KC):
                            MM(bG[:], WGU[sl][:, kc, f2 * 128:(f2 + 1) * 128], vT[:, kc, :], kc == 0, kc == KC - 1,
                               r=[("WGUg", sl)] + vTk, w=[kG])
                        bU, kU = rot(7)
                        for kc in range(KC):
                            MM(bU[:], WGU[sl][:, kc, 256 + f2 * 128:256 + (f2 + 1) * 128], vT[:, kc, :], kc == 0,
                               kc == KC - 1, r=[("WGUu", sl)] + vTk, w=[kU])
                        ss_ = ft % 2
                        ACT(sgb[ss_], bG[:], AF.Silu, r=[kG], w=[("sgb", ss_)])
                        TT("vector", actT[:, ft, :], sgb[ss_], bU[:], ALU.mult, r=[("sgb", ss_), kU], w=[("actT", ft)])
                if _p2 < 2:
                    for j in range(4):
                        DMA("sync", out[(4 * c + j) * 128:(4 * c + j + 1) * 128, :], hbuf[:, j, :], r=[("h", j)], w=[("out", 4 * c + j)])
                    continue
                for hf in range(2):
                    hs = slice(hf * 512, (hf + 1) * 512)
                    accs = [(psf[3 + j], f"ps{3 + j}") for j in range(4)]
                    for p_ in range(npiece):
                        sl = wd_n % 2
                        wd_n += 1
                        DMA("gpsimd", WDp[sl], w_fd_r[:, 2 * p_:2 * p_ + 2, hs], r=[], w=[("WDp", sl)])
                        for f2 in range(2):
                            ft = 2 * p_ + f2
                            for j in range(4):
                                MM(accs[j][0][:], actT[:, ft, j * 128:(j + 1) * 128], WDp[sl][:, f2, :], ft == 0,
                                   ft == NFT - 1, r=[("actT", ft), ("WDp", sl)], w=[accs[j][1]])
                    for j in range(4):
                        ACT(junk[:, 0:512], accs[j][0][:], AF.Square, r=[accs[j][1]], w=[("ssf", j, hf)],
                            accum=ssf[:, j, hf:hf + 1])
                        CP("vector", ffb[:, j, hs], accs[j][0][:], r=[accs[j][1]], w=[("ffb", j)])
                for j in range(4):
                    t = 4 * c + j
                    TT("gpsimd", rs3[:, j:j + 1], ssf[:, j, 0:1], ssf[:, j, 1:2], ALU.add,
                       r=[("ssf", j, 0), ("ssf", j, 1)], w=[("rs3", j)])
                    TS("gpsimd", rs3[:, j:j + 1], rs3[:, j:j + 1], 1.0 / D, EPS, ALU.mult, ALU.add, r=[("rs3", j)],
                       w=[("rs3", j)])
                    TT("gpsimd", rs3[:, j:j + 1], rs3[:, j:j + 1], nhalf, ALU.pow, r=[("rs3", j), "nhalf"],
                       w=[("rs3", j)])
                    STT(ffb[:, j, :], ffb[:, j, :], rs3[:, j:j + 1], g4, ALU.mult, ALU.mult,
                        r=[("ffb", j), ("rs3", j), "g4"], w=[("ffb", j)])
                    TT("vector", ffb[:, j, :], ffb[:, j, :], hbuf[:, j, :], ALU.add, r=[("ffb", j), ("h", j)],
                       w=[("ffb", j)])
                    DMA("sync", out[t * 128:(t + 1) * 128, :], ffb[:, j, :], r=[("ffb", j)], w=[("out", t)])
            P.add("sync", None, r=[("out", t) for t in range(T)])
        P.barrier()
        print("arena peak bytes", A.peak * 2, "ops", {e: len(P.ops[e]) for e in ENGS})
        P.emit(block, sems)
    return nc


_CACHE = {}


def kernel(**inputs):
    S = 4096
    TOPK = 256
    B = 8
    x = np.asarray(inputs["x"], dtype=np.float32)
    consts = host_consts(S, TOPK)
    shared = {}
    for k in ("norm_mix_pre", "w_in", "b_forget", "b_gate", "w_branch_fox", "w_branch_dsa", "w_out",
              "norm_mix_post", "norm_ffn_pre", "w_ffn_gate", "w_ffn_up", "w_ffn_down", "norm_ffn_post"):
        shared[k] = np.ascontiguousarray(np.asarray(inputs[k], dtype=np.float32)[0])
    shared.update(consts)
    if "nc" not in _CACHE:
        _CACHE["nc"] = build(S, TOPK)
    nc = _CACHE["nc"]
    in_maps = []
    for b in range(B):
        m = dict(shared)
        m["x"] = np.ascontiguousarray(x[b])
        in_maps.append(m)
    res = run_bass_kernel_spmd(nc, in_maps, core_ids=list(range(B)))
    return np.stack([np.asarray(r["out"], dtype=np.float32) for r in res.results], axis=0)
```

```python
import numpy as np
from contextlib import ExitStack
import concourse.bass as bass
import concourse.mybir as mybir
from concourse.bass_utils import run_bass_kernel_spmd

F32 = mybir.dt.float32
BF16 = mybir.dt.bfloat16
ALU = mybir.AluOpType
AF = mybir.ActivationFunctionType
AX = mybir.AxisListType

ENGS = ("sync", "scalar", "tensor", "vector", "gpsimd")

D = 1024
KC = 8
DFF = 2816
NFT = DFF // 128
HD = 128
NIT = 16
ARENA_BYTES = 200 * 1024
EPS = 1e-6
NEG = -30000.0


class Prog:
    NDMA_SEM = 6

    def __init__(self):
        self.ops = {e: [] for e in ENGS}
        self.last_w = {}
        self.readers = {}
        self.last_compute = {e: None for e in ENGS}
        self.last_dmas = {e: [] for e in ENGS}

    def add(self, eng, fn, r=(), w=(), dma=False, extra=()):
        idx = len(self.ops[eng])
        deps = set(extra)
        px = [k for k in r if isinstance(k, str) and k.startswith("ps")]
        if px:
            r = [k for k in r if k not in px]
            w = list(w) + px
        for k in r:
            if k in self.last_w:
                deps.add(self.last_w[k])
        for k in w:
            if k in self.last_w:
                deps.add(self.last_w[k])
            for rd in self.readers.get(k, ()):
                deps.add(rd)
        me = (eng, idx)
        deps.discard(me)
        self.ops[eng].append(dict(fn=fn, deps=deps, dma=dma, signal=False, sig=None))
        for k in r:
            self.readers.setdefault(k, []).append(me)
        for k in w:
            self.last_w[k] = me
            self.readers[k] = []
        if fn is not None:
            if dma:
                self.last_dmas[eng] = (self.last_dmas[eng] + [me])[-self.NDMA_SEM:]
            else:
                self.last_compute[eng] = me
        return me

    def barrier(self):
        deps = []
        for e in ENGS:
            if self.last_compute[e] is not None:
                deps.append(self.last_compute[e])
            deps.extend(self.last_dmas[e])
        for e in ENGS:
            self.add(e, None, extra=deps)
        self.last_w = {}
        self.readers = {}

    def emit(self, block, sems):
        ops = self.ops
        for e in ENGS:
            for op in ops[e]:
                for (e2, j) in op["deps"]:
                    p = ops[e2][j]
                    if e2 == e and e == "tensor" and not p["dma"]:
                        continue
                    p["signal"] = True
        for e in ENGS:
            cnt = 0
            ndma = 0
            for op in ops[e]:
                if op["fn"] is None:
                    continue
                if op["dma"]:
                    s = sems["dma"][e][ndma % self.NDMA_SEM]
                    v = 16 * (ndma // self.NDMA_SEM + 1)
                    op["sig"] = (s, v)
                    op["prev"] = (s, v - 16)
                    ndma += 1
                elif op["signal"]:
                    cnt += 1
                    op["sig"] = (sems["cnt"][e], cnt)

        def make(e):
            def body(eng):
                waited = {}
                for op in ops[e]:
                    need = {}
                    for (e2, j) in op["deps"]:
                        p = ops[e2][j]
                        if p["sig"] is None:
                            continue
                        if e2 == e and e == "tensor" and not p["dma"]:
                            continue
                        s, v = p["sig"]
                        if need.get(id(s), (None, 0))[1] < v:
                            need[id(s)] = (s, v)
                    if op["dma"] and op["fn"] is not None:
                        s, v = op["prev"]
                        if v > 0 and need.get(id(s), (None, 0))[1] < v:
                            need[id(s)] = (s, v)
                    for sid, (s, v) in need.items():
                        if waited.get(sid, 0) < v:
                            eng.wait_ge(s, v)
                            waited[sid] = v
                    if op["fn"] is None:
                        continue
                    ins = op["fn"](eng)
                    if op["dma"]:
                        ins.then_inc(op["sig"][0], 16)
                    elif op["signal"]:
                        ins.then_inc(op["sig"][0], 1)
            return body

        block.sync(make("sync"))
        block.scalar(make("scalar"))
        block.tensor(make("tensor"))
        block.vector(make("vector"))
        block.gpsimd(make("gpsimd"))


class Arena:
    def __init__(self, t, nbytes):
        self.t = t
        self.cap = nbytes // 2
        self.off = 0
        self.peak = 0

    def alloc(self, shape, dtype):
        n = int(np.prod(shape))
        size = 4 if dtype == F32 else 2
        nel = n * size // 2
        nel = (nel + 15) // 16 * 16
        assert self.off + nel <= self.cap, f"arena overflow {self.off + nel} > {self.cap}"
        ap = self.t[:, self.off:self.off + n * size // 2]
        self.off += nel
        self.peak = max(self.peak, self.off)
        if dtype != BF16:
            ap = ap.bitcast(dtype)
        if len(shape) == 2:
            ap = ap.rearrange("p (a b) -> p a b", a=shape[0], b=shape[1])
        elif len(shape) == 3:
            ap = ap.rearrange("p (a b c) -> p a b c", a=shape[0], b=shape[1], c=shape[2])
        return ap

    def mark(self):
        return self.off

    def reset(self, m):
        self.off = m


def host_consts(S, TOPK):
    T = S // 128
    i = np.arange(128)
    c = {}
    c["ident"] = np.eye(128, dtype=np.float32)
    c["ident4"] = np.tile(np.eye(128, dtype=np.float32), (1, 4))
    c["cmT"] = np.where(i[:, None] > i[None, :], NEG, 0.0).astype(np.float32)
    c["cmQ"] = np.where(i[None, :] > i[:, None], NEG, 0.0).astype(np.float32)
    c["negm"] = np.where(i[None, :] > i[:, None], -1e30, 0.0).astype(np.float32)
    c["posm"] = np.where(i[None, :] > i[:, None], 1e30, 0.0).astype(np.float32)
    c["tri"] = (i[:, None] <= i[None, :]).astype(np.float32)
    c["ones"] = np.ones((128, 128), np.float32)
    sel0 = np.zeros((128, 128), np.float32)
    sel0[0, :] = 1.0
    c["sel0"] = sel0
    pos = np.arange(S, dtype=np.float32)

    def tab(rot):
        half = rot // 2
        inv = np.float32(500000.0) ** (-np.arange(half, dtype=np.float32) * np.float32(2.0) / np.float32(rot))
        ang = (pos[:, None] * inv[None, :]).astype(np.float32)
        cs = np.cos(ang).astype(np.float32).reshape(T, 128, half).transpose(1, 0, 2)
        sn = np.sin(ang).astype(np.float32).reshape(T, 128, half).transpose(1, 0, 2)
        return np.ascontiguousarray(cs), np.ascontiguousarray(sn)

    c["cosh"], c["sinh"] = tab(32)
    c["cosi"], c["sini"] = tab(16)
    c["cvec"] = np.tile((2.0 ** -(np.arange(NIT) + 1.0)).astype(np.float32)[None, :], (128, 1))
    return c


CONST_SHAPES = lambda S: {
    "ident": [128, 128], "ident4": [128, 512], "cmT": [128, 128], "cmQ": [128, 128],
    "negm": [128, 128], "posm": [128, 128], "tri": [128, 128], "ones": [128, 128],
    "sel0": [128, 128], "cosh": [128, S // 128, 16], "sinh": [128, S // 128, 16],
    "cosi": [128, S // 128, 8], "sini": [128, S // 128, 8], "cvec": [128, NIT],
}


def build(S, TOPK, passes="FDM2", dbg=None):
    T = S // 128
    NCH = S // 512
    KT0 = TOPK // 128
    nc = bass.Bass("TRN2", target_bir_lowering=False)
    dr = lambda n, s: nc.dram_tensor(n, s, F32, kind="ExternalInput").ap()
    x = dr("x", [S, D])
    g_pre = dr("norm_mix_pre", [D])
    w_in = dr("w_in", [D, 4940])
    b_forget = dr("b_forget", [4])
    b_gate = dr("b_gate", [2, D])
    w_bf = dr("w_branch_fox", [512, D])
    w_bd = dr("w_branch_dsa", [512, D])
    w_out = dr("w_out", [D, D])
    g_post = dr("norm_mix_post", [D])
    g_fpre = dr("norm_ffn_pre", [D])
    w_fg = dr("w_ffn_gate", [D, DFF])
    w_fu = dr("w_ffn_up", [D, DFF])
    w_fd = dr("w_ffn_down", [DFF, D])
    g_fpost = dr("norm_ffn_post", [D])
    cst = {k: dr(k, s) for k, s in CONST_SHAPES(S).items()}
    out = nc.dram_tensor("out", [S, D], F32, kind="ExternalOutput").ap()
    dbg_out = nc.dram_tensor("dbg", [128, 8 * S], F32, kind="ExternalOutput").ap() if dbg else None
    dbg_in = nc.dram_tensor("dbg_in", [128, 8 * S], F32, kind="ExternalInput").ap() if dbg in ("Mi", "2i", "Mif", "Mid") else None

    P = Prog()
    scale = float(HD ** -0.5)
    idx_scale = float((64 ** -0.5) * (8 ** -0.5))

    with ExitStack() as es:
        arena_t = es.enter_context(nc.sbuf_tensor("arena", [128, ARENA_BYTES // 2], BF16))
        A = Arena(arena_t, ARENA_BYTES)
        psf = [es.enter_context(nc.psum_tensor(f"ps{i}", [128, 512], F32)) for i in range(7)]
        pst = es.enter_context(nc.psum_tensor("pst", [128, 1024], BF16))
        sems = {"cnt": {e: es.enter_context(nc.semaphore("c_" + e)) for e in ENGS},
                "dma": {e: [es.enter_context(nc.semaphore(f"d_{e}{i}")) for i in range(Prog.NDMA_SEM)]
                        for e in ("sync", "gpsimd")}}
        sems["dma"]["scalar"] = []
        block = es.enter_context(nc.Block())

        def MM(out_, lhsT, rhs, start, stop, r, w):
            P.add("tensor", lambda e: e.matmul(out=out_, lhsT=lhsT, rhs=rhs, start=start, stop=stop), r=r, w=w)

        def TR(out_, in_, idn, r, w):
            P.add("tensor", lambda e: e.transpose(out=out_, in_=in_, identity=idn), r=r, w=w)

        def ACT(out_, in_, func, r, w, bias=0.0, scale_=1.0, accum=None):
            if accum is None:
                P.add("scalar", lambda e: e.activation(out=out_, in_=in_, func=func, bias=bias, scale=scale_), r=r, w=w)
            else:
                P.add("scalar", lambda e: e.activation(out=out_, in_=in_, func=func, bias=bias, scale=scale_,
                                                       accum_out=accum), r=r, w=w)

        def TS(eng, out_, in0, s1, s2, op0, op1, r, w, accum=None):
            if accum is not None:
                P.add(eng, lambda e: e.tensor_scalar(out=out_, in0=in0, scalar1=s1, scalar2=s2, op0=op0, op1=op1,
                                                     accum_out=accum), r=r, w=w)
            elif op1 is None:
                P.add(eng, lambda e: e.tensor_scalar(out=out_, in0=in0, scalar1=s1, scalar2=None, op0=op0), r=r, w=w)
            else:
                P.add(eng, lambda e: e.tensor_scalar(out=out_, in0=in0, scalar1=s1, scalar2=s2, op0=op0, op1=op1),
                      r=r, w=w)

        def TT(eng, out_, in0, in1, op, r, w):
            P.add(eng, lambda e: e.tensor_tensor(out=out_, in0=in0, in1=in1, op=op), r=r, w=w)

        def STT(out_, in0, sc, in1, op0, op1, r, w):
            P.add("vector", lambda e: e.scalar_tensor_tensor(out=out_, in0=in0, scalar=sc, in1=in1, op0=op0, op1=op1),
                  r=r, w=w)

        def CP(eng, out_, in_, r, w):
            if eng == "scalar":
                P.add(eng, lambda e: e.copy(out=out_, in_=in_), r=r, w=w)
            else:
                P.add(eng, lambda e: e.tensor_copy(out=out_, in_=in_), r=r, w=w)

        def RCP(out_, in_, r, w):
            P.add("vector", lambda e: e.reciprocal(out=out_, in_=in_), r=r, w=w)

        def RED(out_, in_, op, r, w):
            P.add("vector", lambda e: e.tensor_reduce(out=out_, in_=in_, axis=AX.X, op=op), r=r, w=w)

        def MS(eng, ap, val, w):
            P.add(eng, lambda e: e.memset(ap, val), w=w)

        def DMA(eng, out_, in_, r, w):
            P.add(eng, lambda e: e.dma_start(out=out_, in_=in_), r=r, w=w, dma=True)

        rot_state = {"i": 0}

        def rot(nb=3):
            i = rot_state["i"] % nb
            rot_state["i"] += 1
            return psf[i], f"ps{i}"

        identb = A.alloc([128], BF16)
        ident4b = A.alloc([512], BF16)
        cmTb = A.alloc([128], BF16)
        cmQb = A.alloc([128], BF16)
        onesb = A.alloc([128], BF16)
        negm = A.alloc([128], F32)
        posm = A.alloc([128], F32)
        cosh = A.alloc([T, 16], F32)
        sinh = A.alloc([T, 16], F32)
        cosi = A.alloc([T, 8], F32)
        sini = A.alloc([T, 8], F32)
        cvec = A.alloc([NIT], F32)
        nhalf = A.alloc([1], F32)
        identf = A.alloc([128], F32)
        g8 = A.alloc([128], F32)
        gT = A.alloc([8], F32)
        odT = A.alloc([4, S], BF16)
        A.full_cap = A.cap
        ofT = arena_t[:, A.cap - 4 * S:A.cap].rearrange("p (a b) -> p a b", a=4, b=S)
        A.cap = A.full_cap - 4 * S
        for ap_, nm in ((identb, "ident"), (ident4b, "ident4"), (cmTb, "cmT"), (cmQb, "cmQ"), (onesb, "ones")):
            DMA("gpsimd", ap_, cst[nm], r=[], w=["c_" + nm + "b"])
        for ap_, nm in ((negm, "negm"), (posm, "posm"), (cosh, "cosh"), (sinh, "sinh"), (cosi, "cosi"), (sini, "sini"),
                        (cvec, "cvec")):
            DMA("sync", ap_, cst[nm], r=[], w=["c_" + nm])
        DMA("sync", identf, cst["ident"], r=[], w=["c_ident"])
        DMA("sync", g8[0:8, :], g_pre.rearrange("(kc p) -> kc p", p=128), r=[], w=["g8"])
        TR(psf[6][:, 0:8], g8[0:8, :], identf[0:8, 0:8], r=["g8", "c_ident"], w=["ps6"])
        CP("vector", gT, psf[6][:, 0:8], r=["ps6"], w=["gT"])
        MS("gpsimd", nhalf, -0.5, w=["nhalf"])
        persist_mark = A.mark()

        def u_stage(t, j, bufs):
            s = t % 2
            xs = t % len(bufs["xt"])
            xt, ssq, rstd, ub, uT = bufs["xt"][xs], bufs["ss"][s], bufs["rstd"][s], bufs["ub"][s], bufs["uT"]
            DMA("sync", xt, x[t * 128:(t + 1) * 128, :], r=[], w=[("xt", xs)])
            ACT(ub, xt, AF.Square, r=[("xt", xs)], w=[("ss", s), ("ub", s)], accum=ssq)
            TS("gpsimd", rstd, ssq, 1.0 / D, EPS, ALU.mult, ALU.add, r=[("ss", s)], w=[("rstd", s)])
            TT("gpsimd", rstd, rstd, nhalf, ALU.pow, r=[("rstd", s), "nhalf"], w=[("rstd", s)])
            ACT(ub, xt, AF.Copy, r=[("xt", xs), ("rstd", s)], w=[("ub", s)], scale_=rstd)
            for kc in range(KC):
                TR(pst[:, kc * 128:(kc + 1) * 128], ub[:, kc * 128:(kc + 1) * 128], identb,
                   r=[("ub", s), "c_identb"], w=["pst"])
            CP("scalar", uT[:, :, j * 128:(j + 1) * 128], pst[:].rearrange("p (a b) -> p a b", a=KC, b=128),
               r=["pst"], w=[("uT", j)])

        def fold_gain(W, keys):
            for kc in range(KC):
                TS("vector", W[:, kc, :], W[:, kc, :], gT[:, kc:kc + 1], None, ALU.mult, None, r=list(keys) + ["gT"],
                   w=list(keys))

        def u_bufs(nx=2):
            return dict(xt=[A.alloc([D], F32) for _ in range(nx)], ss=[A.alloc([1], F32) for _ in range(2)],
                        rstd=[A.alloc([1], F32) for _ in range(2)], ub=[A.alloc([D], BF16) for _ in range(2)],
                        uT=A.alloc([KC, 512], BF16))

        w_in_r = w_in.rearrange("(kc p) c -> p kc c", p=128)

        if "D" in passes:
            A.cap = A.full_cap
            WD_ = A.alloc([KC, 1352], BF16)
            KdT = A.alloc([S], BF16)
            Vd = A.alloc([T, 128], BF16)
            kiT = A.alloc([S], BF16)
            QQ = [A.alloc([1024], BF16) for _ in range(3)]
            sgnD = [A.alloc([8, 128], BF16) for _ in range(3)]
            scs = [A.alloc([S], F32) for _ in range(2)]
            Mneg = [A.alloc([S], BF16) for _ in range(2)]
            R = [A.alloc([8, 512], BF16) for _ in range(1)]
            qd_f = A.alloc([4, 128], F32)
            qi_f = A.alloc([8, 64], F32)
            g4_f = A.alloc([328], F32)
            qd_b = A.alloc([4, 128], BF16)
            qi_b = A.alloc([8, 64], BF16)
            kk_b = A.alloc([256], BF16)
            rt = [A.alloc([8, 16], F32) for _ in range(4)]
            aw = A.alloc([8], F32)
            sg01 = A.alloc([8], F32)
            sgn = A.alloc([8], F32)
            tmpds = [A.alloc([128], F32) for _ in range(2)]
            sts = [A.alloc([8], F32) for _ in range(2)]
            Wc = A.alloc([NIT], F32)
            mids = A.alloc([NIT + 1], F32)
            cnts = A.alloc([NIT], F32)
            sgs = A.alloc([NIT], F32)
            pT = [A.alloc([512], BF16) for _ in range(2)]
            oS = [A.alloc([512], F32) for _ in range(2)]
            dS = [A.alloc([512], F32) for _ in range(2)]
            rden = A.alloc([512], F32)
            ub_ = u_bufs(2)
            for (d0, s0, n_) in ((0, 1540, 512), (512, 2308, 512), (1024, 2052, 256), (1280, 2820, 72)):
                DMA("gpsimd", WD_[:, :, d0:d0 + n_], w_in_r[:, :, s0:s0 + n_], r=[], w=[("WD", d0)])
            WDK = [("WD", 0), ("WD", 512), ("WD", 1024), ("WD", 1280)]
            fold_gain(WD_, WDK)

            def rope(src3, dst3, nh, half, cos_t, sin_t, key_src, key_dst):
                x1 = src3[:, :, 0:half]
                x2 = src3[:, :, half:2 * half]
                cb_ = cos_t.unsqueeze(1).broadcast_to([128, nh, half])
                sb_ = sin_t.unsqueeze(1).broadcast_to([128, nh, half])
                ta, tb, tc, td = [r_[:, 0:nh, 0:half] for r_ in rt]
                TT("gpsimd", ta, x1, cb_, ALU.mult, r=[key_src], w=["rt0"])
                TT("gpsimd", tb, x2, sb_, ALU.mult, r=[key_src], w=["rt1"])
                TT("gpsimd", tc, x2, cb_, ALU.mult, r=[key_src], w=["rt2"])
                TT("gpsimd", td, x1, sb_, ALU.mult, r=[key_src], w=["rt3"])
                TT("gpsimd", dst3[:, :, 0:half], ta, tb, ALU.subtract, r=["rt0", "rt1"], w=[key_dst])
                TT("gpsimd", dst3[:, :, half:2 * half], tc, td, ALU.add, r=["rt2", "rt3"], w=[key_dst])

            def prepP(t):
                c, j = t // 4, t % 4
                qs = t % 3
                if j == 0:
                    for jj in range(4):
                        u_stage(4 * c + jj, jj, ub_)
                uT = ub_["uT"]
                for (dst, c0, wn, key) in ((qd_f, 0, 512, "qd_f"), (qi_f, 512, 512, "qi_f"), (g4_f, 1024, 328, "g4_f")):
                    bank, bk = rot()
                    for kc in range(KC):
                        MM(bank[:, 0:wn], uT[:, kc, j * 128:(j + 1) * 128], WD_[:, kc, c0:c0 + wn], kc == 0,
                           kc == KC - 1, r=WDK + [("uT", j)], w=[bk])
                    dflat = dst if key == "g4_f" else dst.rearrange("p a b -> p (a b)")
                    CP("scalar", dflat, bank[:, 0:wn], r=[bk], w=[key])
                rope(qd_f, qd_b, 4, 16, cosh[:, t, :], sinh[:, t, :], "qd_f", "qd_b")
                CP("gpsimd", qd_b[:, :, 32:128], qd_f[:, :, 32:128], r=["qd_f"], w=["qd_b"])
                TS("gpsimd", sg01, g4_f[:, 320:328], 0.0, None, ALU.is_ge, None, r=["g4_f"], w=["sg01"])
                TS("gpsimd", sgn, sg01, 2.0, -1.0, ALU.mult, ALU.add, r=["sg01"], w=["sgn"])
                TS("gpsimd", aw, sg01, 2.0 * idx_scale, -idx_scale, ALU.mult, ALU.add, r=["sg01"], w=["aw"])
                TT("gpsimd", aw, aw, g4_f[:, 320:328], ALU.mult, r=["aw", "g4_f"], w=["aw"])
                rope(qi_f, qi_f, 8, 8, cosi[:, t, :], sini[:, t, :], "qi_f", "qi_f")
                TT("gpsimd", qi_b, qi_f, aw.unsqueeze(2).broadcast_to([128, 8, 64]), ALU.mult, r=["qi_f", "aw"],
                   w=["qi_b"])
                kd3 = g4_f[:, 0:128].rearrange("p (a b) -> p a b", a=1, b=128)
                kdb3 = kk_b[:, 0:128].rearrange("p (a b) -> p a b", a=1, b=128)
                rope(kd3, kdb3, 1, 16, cosh[:, t, :], sinh[:, t, :], "g4_f", "kk_b")
                CP("gpsimd", kk_b[:, 32:128], g4_f[:, 32:128], r=["g4_f"], w=["kk_b"])
                ki3 = g4_f[:, 256:320].rearrange("p (a b) -> p a b", a=1, b=64)
                kib3 = kk_b[:, 128:192].rearrange("p (a b) -> p a b", a=1, b=64)
                rope(ki3, kib3, 1, 8, cosi[:, t, :], sini[:, t, :], "g4_f", "kk_b")
                CP("gpsimd", kk_b[:, 144:192], g4_f[:, 272:320], r=["g4_f"], w=["kk_b"])
                CP("gpsimd", kk_b[:, 192:256], kk_b[:, 128:192], r=["kk_b"], w=["kk_b2"])
                CP("gpsimd", Vd[:, t, :], g4_f[:, 128:256], r=["g4_f"], w=[("Vd", t)])
                TT("gpsimd", sgnD[qs], identb.unsqueeze(1).broadcast_to([128, 8, 128]),
                   sgn.unsqueeze(2).broadcast_to([128, 8, 128]), ALU.mult, r=["c_identb", "sgn"], w=[("sgnD", qs)])

            def prepT(t):
                qs = t % 3
                for i_ in range(2):
                    TR(pst[:, i_ * 128:(i_ + 1) * 128], kk_b[:, i_ * 128:(i_ + 1) * 128], identb,
                       r=["kk_b", "kk_b2", "c_identb"], w=["pst"])
                CP("scalar", KdT[:, t * 128:(t + 1) * 128], pst[:, 0:128], r=["pst"], w=[("KdT", t)])
                CP("scalar", kiT[:, t * 128:(t + 1) * 128], pst[:, 128:256], r=["pst"], w=[("kiT", t)])
                qdb2 = qd_b.rearrange("p a b -> p (a b)")
                qib2 = qi_b.rearrange("p a b -> p (a b)")
                for i_ in range(4):
                    TR(pst[:, i_ * 128:(i_ + 1) * 128], qdb2[:, i_ * 128:(i_ + 1) * 128], identb,
                       r=["qd_b", "c_identb"], w=["pst"])
                for i_ in range(4):
                    TR(pst[:, 512 + i_ * 128:512 + (i_ + 1) * 128], qib2[:, i_ * 128:(i_ + 1) * 128], identb,
                       r=["qi_b", "c_identb"], w=["pst"])
                CP("scalar", QQ[qs], pst[:], r=["pst"], w=[("QQ", qs)])

            def stageA(t):
                q3 = t % 3
                qs = t % 2
                n = (t + 1) * 128
                sc = scs[qs]
                tmpd = tmpds[qs]
                if t < KT0:
                    return
                nk5 = (n + 511) // 512
                for k5 in range(nk5):
                    c0 = k5 * 512
                    wn = min(512, n - c0)
                    rs = 0
                    kik = [("kiT", tt) for tt in range(c0 // 128, (c0 + wn) // 128)]
                    for hd in range(8):
                        hp, hf = hd // 2, hd % 2
                        bank, bk = rot()
                        MM(bank[:, 0:wn], QQ[q3][64 * hf:64 * hf + 64, 512 + hp * 128:512 + (hp + 1) * 128],
                           kiT[64 * hf:64 * hf + 64, c0:c0 + wn], True, True, r=[("QQ", q3)] + kik, w=[bk])
                        ACT(R[rs][:, hd, 0:wn], bank[:, 0:wn], AF.Relu, r=[bk], w=[("R", rs, hd)])
                    sb_, sbk = psf[3], "ps3"
                    for hd in range(8):
                        MM(sb_[:, 0:wn], sgnD[q3][:, hd, :], R[rs][:, hd, 0:wn], hd == 0, hd == 7,
                           r=[("sgnD", q3), ("R", rs, hd)], w=[sbk])
                    CP("scalar", sc[:, c0:c0 + wn], sb_[:, 0:wn], r=[sbk], w=[("sc", qs, k5)])
                sck = [("sc", qs, k5) for k5 in range(nk5)]
                TT("gpsimd", tmpd, sc[:, n - 128:n], posm, ALU.add, r=sck + ["c_posm"], w=[("tmpd", qs)])
                TT("gpsimd", sc[:, n - 128:n], sc[:, n - 128:n], negm, ALU.add, r=sck + ["c_negm", ("tmpd", qs)],
                   w=[("sc", qs, nk5 - 1)])

            def stageB(t):
                qs = t % 2
                ms_ = qs
                n = (t + 1) * 128
                sc = scs[qs]
                tmpd = tmpds[qs]
                st = sts[qs]
                if t < KT0:
                    if n > 128:
                        MS("gpsimd", Mneg[ms_][:, 0:n - 128], 0.0, w=[("Mneg", ms_)])
                    CP("gpsimd", Mneg[ms_][:, n - 128:n], cmQb, r=["c_cmQb"], w=[("Mneg", ms_)])
                    return
                nk5 = (n + 511) // 512
                sck = [("sc", qs, k5) for k5 in range(nk5)]
                RED(st[:, 0:1], sc[:, 0:n], ALU.max, r=sck, w=[("hi", qs)])
                RED(st[:, 1:2], tmpd, ALU.min, r=[("tmpd", qs)], w=[("m1", qs)])
                RED(st[:, 2:3], sc[:, 0:n - 128], ALU.min, r=sck, w=[("m2", qs)])
                TT("vector", st[:, 3:4], st[:, 1:2], st[:, 2:3], ALU.min, r=[("m1", qs), ("m2", qs)], w=[("lo", qs)])
                TT("vector", st[:, 4:5], st[:, 0:1], st[:, 3:4], ALU.subtract, r=[("hi", qs), ("lo", qs)],
                   w=[("Wd", qs)])
                TS("vector", Wc, cvec, st[:, 4:5], None, ALU.mult, None, r=["c_cvec", ("Wd", qs)], w=["Wc"])
                TT("vector", mids[:, 0:1], st[:, 3:4], Wc[:, 0:1], ALU.add, r=[("lo", qs), "Wc"], w=[("mid", 0)])
                for it in range(NIT):
                    TS("vector", Mneg[ms_][:, 0:n], sc[:, 0:n], mids[:, it:it + 1], None, ALU.is_ge, ALU.add,
                       r=sck + [("mid", it)], w=[("cnt", it), ("Mneg", ms_)], accum=cnts[:, it:it + 1])
                    TS("vector", sgs[:, it:it + 1], cnts[:, it:it + 1], TOPK - 0.5, 0.5, ALU.is_ge, ALU.subtract,
                       r=[("cnt", it)], w=[("sg", it)])
                    STT(mids[:, it + 1:it + 2], sgs[:, it:it + 1], Wc[:, it:it + 1], mids[:, it:it + 1], ALU.mult,
                        ALU.add, r=[("sg", it), "Wc", ("mid", it)], w=[("mid", it + 1)])
                STT(st[:, 5:6], st[:, 4:5], -(2.0 ** -(NIT + 1)), mids[:, NIT:NIT + 1], ALU.mult, ALU.add,
                    r=[("Wd", qs), ("mid", NIT)], w=[("tau", qs)])
                TS("vector", Mneg[ms_][:, 0:n], sc[:, 0:n], st[:, 5:6], NEG, ALU.is_lt, ALU.mult,
                   r=sck + [("tau", qs)], w=[("Mneg", ms_)])

            def stageC(t):
                qs = t % 2
                q3 = t % 3
                ms_ = qs
                ob, obk, db, dbk = psf[4], "ps4", psf[5], "ps5"
                def qk(kt):
                    bank, bk = rot()
                    MM(bank[:], KdT[:, kt * 128:(kt + 1) * 128], QQ[q3][:, 0:512], True, False,
                       r=[("KdT", kt), ("QQ", q3)], w=[bk])
                    MM(bank[:], Mneg[ms_][:, kt * 128:(kt + 1) * 128], ident4b, False, True,
                       r=[("Mneg", ms_), "c_ident4b"], w=[bk])
                    return bank, bk
                cur = qk(0)
                for kt in range(t + 1):
                    nxt = qk(kt + 1) if kt + 1 <= t else None
                    bank, bk = cur
                    ps_ = kt % 2
                    ACT(pT[ps_], bank[:], AF.Exp, r=[bk], w=[("pT", ps_)], scale_=scale)
                    MM(ob[:], Vd[:, kt, :], pT[ps_], kt == 0, kt == t, r=[("Vd", kt), ("pT", ps_)], w=[obk])
                    MM(db[:], onesb, pT[ps_], kt == 0, kt == t, r=["c_onesb", ("pT", ps_)], w=[dbk])
                    cur = nxt
                CP("scalar", oS[qs], ob[:], r=[obk], w=[("oS", qs)])
                CP("scalar", dS[qs], db[:], r=[dbk], w=[("dS", qs)])

            def stageN(t):
                qs = t % 2
                RCP(rden, dS[qs], r=[("dS", qs)], w=["rden"])
                TT("vector", odT[:, :, t * 128:(t + 1) * 128], oS[qs].rearrange("p (a b) -> p a b", a=4, b=128),
                   rden.rearrange("p (a b) -> p a b", a=4, b=128), ALU.mult, r=[("oS", qs), "rden"], w=[("odT", t)])

            prepP(0)
            prepT(0)
            stageA(0)
            if T > 1:
                prepP(1)
                prepT(1)
            for t in range(T):
                if t + 2 < T:
                    prepP(t + 2)
                if t + 1 < T:
                    stageA(t + 1)
                stageB(t)
                if t >= 1:
                    stageN(t - 1)
                if t + 2 < T:
                    prepT(t + 2)
                stageC(t)
            stageN(T - 1)
            P.barrier()
            A.reset(persist_mark)
            A.cap = A.full_cap - 4 * S
            if dbg in ("D", "Y"):
                if dbg == "D":
                    DMA("gpsimd", dbg_out[:, 4 * S:8 * S], odT.rearrange("p a b -> p (a b)"), r=[], w=["dbgD"])
                if dbg == "Y":
                    DMA("gpsimd", dbg_out[:, 0:4 * S], ofT.rearrange("p a b -> p (a b)"), r=[], w=["dbgD"])
                P.add("sync", None, r=["dbgD"])
                P.barrier()

        if "F" in passes:
            tri = A.alloc([128], F32)
            onesf = A.alloc([128], F32)
            sel0 = A.alloc([128], F32)
            for ap_, nm in ((tri, "tri"), (onesf, "ones"), (sel0, "sel0")):
                DMA("sync", ap_, cst[nm], r=[], w=["c_" + nm])
            WF = A.alloc([KC, 1540], BF16)
            KfT = A.alloc([4, S], BF16)
            Vf = A.alloc([T, 512], BF16)
            negc = A.alloc([T, 4], F32)
            biasc = A.alloc([T, 4], F32)
            bfb = A.alloc([4, 4], F32)
            carry = A.alloc([4], F32)
            refb = A.alloc([4], F32)
            v3 = lambda a_: a_.rearrange("p (a b) -> p a b", a=4, b=4)
            ztf = A.alloc([16], F32)
            etf = A.alloc([16], F32)
            ltf = A.alloc([16], F32)
            Lsf = A.alloc([16], F32)
            zt, et, lt, Ls = v3(ztf), v3(etf), v3(ltf), v3(Lsf)
            ltot = A.alloc([4], F32)
            QfT = A.alloc([4, 512], BF16)
            pT = [A.alloc([512], BF16) for _ in range(2)]
            rden = A.alloc([512], F32)
            ub_ = u_bufs()
            for half in range(2):
                DMA("gpsimd", WF[:, half * 4:(half + 1) * 4, :], w_in_r[:, half * 4:(half + 1) * 4, 0:1540],
                    r=[], w=[("WF", half)])
            WFK = [("WF", 0), ("WF", 1)]
            fold_gain(WF, WFK)
            for jj in range(4):
                DMA("sync", bfb[:, jj, :], b_forget.partition_broadcast(128), r=[], w=[("bfb", jj)])
            MS("gpsimd", carry, 0.0, w=["carry"])
            MS("gpsimd", Ls[:, 0, :], 0.0, w=["Ls0"])
            oset = 0
            for c in range(NCH):
                for j in range(4):
                    u_stage(4 * c + j, j, ub_)
                uT = ub_["uT"]
                uTk = [("uT", j) for j in range(4)]
                for g in range(8):
                    bank, bk = rot()
                    for kc in range(KC):
                        MM(bank[:], WF[:, kc, g * 128:(g + 1) * 128], uT[:, kc, :], kc == 0, kc == KC - 1,
                           r=WFK + uTk, w=[bk])
                    if g < 4:
                        CP("vector", QfT[:, g, :], bank[:], r=[bk], w=[("QfT", g)])
                    else:
                        CP("vector", KfT[:, g - 4, c * 512:(c + 1) * 512], bank[:], r=[bk], w=[("KfT", g - 4, c)])
                fbank, fbk = psf[3], "ps3"
                for j in range(4):
                    t = 4 * c + j
                    bank, bk = rot()
                    for kc in range(KC):
                        MM(bank[:], uT[:, kc, j * 128:(j + 1) * 128], WF[:, kc, 1024:1536], kc == 0, kc == KC - 1,
                           r=WFK + [("uT", j)], w=[bk])
                    CP("vector", Vf[:, t, :], bank[:], r=[bk], w=[("Vf", t)])
                    for kc in range(KC):
                        MM(fbank[:, j * 4:(j + 1) * 4], uT[:, kc, j * 128:(j + 1) * 128], WF[:, kc, 1536:1540],
                           kc == 0, kc == KC - 1, r=WFK + [("uT", j)], w=[fbk])
                TT("vector", zt, fbank[:, 0:16].rearrange("p (a b) -> p a b", a=4, b=4), bfb, ALU.add,
                   r=[fbk] + [("bfb", jj) for jj in range(4)], w=["zt"])
                ACT(et, zt, AF.Exp, r=["zt"], w=["et"], scale_=-1.0)
                ACT(lt, et, AF.Ln, r=["et"], w=["lt"], bias=1.0)
                CP("gpsimd", Ls[:, 1, :], lt[:, 0, :], r=["lt"], w=["Ls1"])
                TT("gpsimd", Ls[:, 2, :], Ls[:, 1, :], lt[:, 1, :], ALU.add, r=["lt", "Ls1"], w=["Ls2"])
                TT("gpsimd", Ls[:, 3, :], Ls[:, 2, :], lt[:, 2, :], ALU.add, r=["lt", "Ls2"], w=["Ls3"])
                TT("gpsimd", ltot, Ls[:, 3, :], lt[:, 3, :], ALU.add, r=["lt", "Ls3"], w=["ltot"])
                cb, cbk = psf[4], "ps4"
                MM(cb[:, 0:16], tri, ltf, True, False, r=["c_tri", "lt"], w=[cbk])
                MM(cb[:, 0:16], onesf, Lsf, False, True, r=["c_ones", "Ls0", "Ls1", "Ls2", "Ls3"], w=[cbk])
                MM(cb[:, 16:20], onesf, ltot, True, True, r=["c_ones", "ltot"], w=[cbk])
                TT("vector", negc[:, 4 * c:4 * c + 4, :], cb[:, 0:16].rearrange("p (a b) -> p a b", a=4, b=4),
                   carry.unsqueeze(1).broadcast_to([128, 4, 4]), ALU.add, r=[cbk, "carry"], w=[("negc", c)])
                TT("vector", carry, cb[:, 16:20], carry, ALU.add, r=[cbk, "carry"], w=["carry"])
                rb, rbk = psf[5], "ps5"
                MM(rb[:, 0:4], sel0, negc[:, 4 * c + 2, :], True, True, r=["c_sel0", ("negc", c)], w=[rbk])
                CP("vector", refb, rb[:, 0:4], r=[rbk], w=["refb"])
                nkt = 4 * c + 4
                for h in range(4):
                    TS("vector", biasc[:, 0:nkt, h], negc[:, 0:nkt, h], refb[:, h:h + 1], None, ALU.subtract, None,
                       r=[("negc", cc) for cc in range(c + 1)] + ["refb"], w=[("biasc", h)])
                for h in range(4):
                    ob, obk, db, dbk = (psf[3], "ps3", psf[4], "ps4") if oset == 0 else (psf[5], "ps5", psf[6], "ps6")
                    oset ^= 1
                    def qkf(kt):
                        off = max(0, kt - 4 * c) * 128
                        diag = kt >= 4 * c
                        bank, bk = rot()
                        MM(bank[:, off:512], KfT[:, h, kt * 128:(kt + 1) * 128], QfT[:, h, off:512], True, not diag,
                           r=[("KfT", h, kt // 4), ("QfT", h)], w=[bk])
                        if diag:
                            MM(bank[:, off:off + 128], identb, cmTb, False, True, r=["c_identb", "c_cmTb"], w=[bk])
                        return bank, bk, off
                    cur = qkf(0)
                    for kt in range(nkt):
                        nxt = qkf(kt + 1) if kt + 1 < nkt else None
                        bank, bk, off = cur
                        ps_ = kt % 2
                        ACT(pT[ps_][:, off:512], bank[:, off:512], AF.Exp, r=[bk, ("biasc", h)], w=[("pT", ps_)],
                            bias=biasc[:, kt, h:h + 1], scale_=scale)
                        MM(ob[:, off:512], Vf[:, kt, h * 128:(h + 1) * 128], pT[ps_][:, off:512], kt == 0,
                           kt == nkt - 1, r=[("Vf", kt), ("pT", ps_)], w=[obk])
                        MM(db[:, off:512], onesb, pT[ps_][:, off:512], kt == 0, kt == nkt - 1,
                           r=["c_onesb", ("pT", ps_)], w=[dbk])
                        cur = nxt
                    RCP(rden, db[:], r=[dbk], w=["rden"])
                    TT("vector", ofT[:, h, c * 512:(c + 1) * 512], ob[:], rden, ALU.mult, r=[obk, "rden"],
                       w=[("ofT", h, c)])
            P.barrier()
            A.reset(persist_mark)
            if dbg == "F":
                DMA("gpsimd", dbg_out[:, 0:4 * S], ofT.rearrange("p a b -> p (a b)"), r=[], w=["dbgF"])
                P.add("sync", None, r=["dbgF"])
                P.barrier()

        if dbg in ("Mi", "2i", "Mif", "Mid"):
            if dbg != "Mif":
                DMA("gpsimd", ofT.rearrange("p a b -> p (a b)"), dbg_in[:, 0:4 * S], r=[], w=["dbgi"])
            if dbg != "Mid":
                DMA("gpsimd", odT.rearrange("p a b -> p (a b)"), dbg_in[:, 4 * S:8 * S], r=[], w=["dbgi2"])
            P.barrier()
        if "M" in passes:
            Wg = A.alloc([KC, 2048], BF16)
            Wbf = A.alloc([4, D], BF16)
            Wbd = A.alloc([4, D], BF16)
            bg16 = A.alloc([128], F32)
            bgT = A.alloc([16], F32)
            sigf = A.alloc([512], F32)
            sigd = A.alloc([512], F32)
            t1 = A.alloc([512], F32)
            t2 = A.alloc([512], F32)
            mixt = A.alloc([8, 512], BF16)
            ub_ = u_bufs()
            for q4 in range(4):
                DMA("gpsimd", Wg[:, q4 * 2:(q4 + 1) * 2, :], w_in_r[:, q4 * 2:(q4 + 1) * 2, 2892:4940], r=[],
                    w=[("Wg", q4)])
            WGK = [("Wg", q4) for q4 in range(4)]
            fold_gain(Wg, WGK)
            DMA("gpsimd", Wbf, w_bf.rearrange("(h p) c -> p h c", p=128), r=[], w=["Wbf"])
            DMA("gpsimd", Wbd, w_bd.rearrange("(h p) c -> p h c", p=128), r=[], w=["Wbd"])
            DMA("sync", bg16[0:16, :], b_gate.rearrange("b (kc p) -> (b kc) p", p=128), r=[], w=["bg16"])
            tb_, tbk = psf[6], "ps6"
            TR(tb_[:, 0:16], bg16[0:16, :], identf[0:16, 0:16], r=["bg16", "c_ident"], w=[tbk])
            CP("vector", bgT, tb_[:, 0:16], r=[tbk], w=["bgT"])
            for c in range(NCH):
                for j in range(4):
                    u_stage(4 * c + j, j, ub_)
                uT = ub_["uT"]
                uTk = [("uT", j) for j in range(4)]
                cs = slice(c * 512, (c + 1) * 512)
                for cc in range(8):
                    bA, kA = rot(7)
                    for kc in range(KC):
                        MM(bA[:], Wg[:, kc, cc * 128:(cc + 1) * 128], uT[:, kc, :], kc == 0, kc == KC - 1,
                           r=WGK + uTk, w=[kA])
                    bB, kB = rot(7)
                    for kc in range(KC):
                        MM(bB[:], Wg[:, kc, 1024 + cc * 128:1024 + (cc + 1) * 128], uT[:, kc, :], kc == 0,
                           kc == KC - 1, r=WGK + uTk, w=[kB])
                    bC, kCk = rot(7)
                    for h in range(4):
                        MM(bC[:], Wbf[:, h, cc * 128:(cc + 1) * 128], ofT[:, h, cs], h == 0, h == 3,
                           r=["Wbf", ("ofT", h, c)], w=[kCk])
                    bD, kDk = rot(7)
                    for h in range(4):
                        MM(bD[:], Wbd[:, h, cc * 128:(cc + 1) * 128], odT[:, h, cs], h == 0, h == 3,
                           r=["Wbd", ("odT", h, c)], w=[kDk])
                    ACT(sigf, bA[:], AF.Sigmoid, r=[kA, "bgT"], w=["sigf"], bias=bgT[:, cc:cc + 1])
                    ACT(sigd, bB[:], AF.Sigmoid, r=[kB, "bgT"], w=["sigd"], bias=bgT[:, 8 + cc:9 + cc])
                    TT("vector", t1, sigf, bC[:], ALU.mult, r=["sigf", kCk], w=["t1"])
                    TT("vector", t2, sigd, bD[:], ALU.mult, r=["sigd", kDk], w=["t2"])
                    TT("vector", mixt[:, cc, :], t1, t2, ALU.add, r=["t1", "t2"], w=[("mixt", cc)])
                CP("gpsimd", ofT[:, :, cs], mixt[:, 0:4, :], r=[("mixt", cc) for cc in range(4)],
                   w=[("ofT", h, c) for h in range(4)])
                CP("gpsimd", odT[:, :, cs], mixt[:, 4:8, :], r=[("mixt", cc) for cc in range(4, 8)],
                   w=[("odT", h, c) for h in range(4)])
            P.barrier()
            A.reset(persist_mark)
            if dbg in ("M", "Mi", "Mif", "Mid"):
                DMA("gpsimd", dbg_out[:, 0:4 * S], ofT.rearrange("p a b -> p (a b)"), r=[], w=["dbgF"])
                DMA("gpsimd", dbg_out[:, 4 * S:8 * S], odT.rearrange("p a b -> p (a b)"), r=[], w=["dbgD"])
                P.add("sync", None, r=["dbgF", "dbgD"])
                P.barrier()

        if "2" in passes:
            Wo = A.alloc([KC, D], BF16)
            gpost = A.alloc([D], F32)
            g3 = A.alloc([D], F32)
            g4 = A.alloc([D], F32)
            hbuf = A.alloc([4, D], F32)
            ffb = A.alloc([4, D], F32)
            tmpy = A.alloc([512], F32)
            vb = A.alloc([D], BF16)
            vT = A.alloc([KC, 512], BF16)
            WGU = [A.alloc([KC, 512], BF16) for _ in range(2)]
            WDp = [A.alloc([2, 512], BF16) for _ in range(2)]
            sgb = [A.alloc([512], F32) for _ in range(2)]
            actT = A.alloc([NFT, 512], BF16)
            junk = A.alloc([D], BF16)
            ssy = A.alloc([4, 2], F32)
            ssh = A.alloc([4], F32)
            ssf = A.alloc([4, 2], F32)
            rs1 = A.alloc([4], F32)
            rs2 = A.alloc([4], F32)
            rs3 = A.alloc([4], F32)
            DMA("gpsimd", Wo, w_out.rearrange("(kc p) c -> p kc c", p=128), r=[], w=["Wo"])
            DMA("sync", gpost, g_post.partition_broadcast(128), r=[], w=["gpost"])
            DMA("sync", g3, g_fpre.partition_broadcast(128), r=[], w=["g3"])
            DMA("sync", g4, g_fpost.partition_broadcast(128), r=[], w=["g4"])
            w_fg_r = w_fg.rearrange("(kc p) f -> p kc f", p=128)
            w_fu_r = w_fu.rearrange("(kc p) f -> p kc f", p=128)
            w_fd_r = w_fd.rearrange("(ft p) c -> p ft c", p=128)
            npiece = NFT // 2
            wgu_n = 0
            wd_n = 0

            def mixT(kc, sl):
                return ofT[:, kc, sl] if kc < 4 else odT[:, kc - 4, sl]

            import os as _os
            _p2 = int(_os.environ.get("K_P2", "9"))
            for c in range(NCH):
                for j in range(4):
                    t = 4 * c + j
                    ts_ = slice(t * 128, (t + 1) * 128)
                    DMA("sync", hbuf[:, j, :], x[ts_, :], r=[], w=[("h", j)])
                    banks = []
                    for hf in range(2):
                        bank, bk = rot(7)
                        banks.append((bank, bk))
                        for kc in range(KC):
                            MM(bank[:], mixT(kc, ts_), Wo[:, kc, hf * 512:(hf + 1) * 512], kc == 0, kc == KC - 1,
                               r=["Wo"], w=[bk])
                        ACT(junk[:, 0:512], bank[:], AF.Square, r=[bk], w=[("ssy", j, hf)], accum=ssy[:, j, hf:hf + 1])
                    TT("gpsimd", rs1[:, j:j + 1], ssy[:, j, 0:1], ssy[:, j, 1:2], ALU.add,
                       r=[("ssy", j, 0), ("ssy", j, 1)], w=[("rs1", j)])
                    TS("gpsimd", rs1[:, j:j + 1], rs1[:, j:j + 1], 1.0 / D, EPS, ALU.mult, ALU.add, r=[("rs1", j)],
                       w=[("rs1", j)])
                    TT("gpsimd", rs1[:, j:j + 1], rs1[:, j:j + 1], nhalf, ALU.pow, r=[("rs1", j), "nhalf"],
                       w=[("rs1", j)])
                    for hf in range(2):
                        bank, bk = banks[hf]
                        hs = slice(hf * 512, (hf + 1) * 512)
                        STT(tmpy, bank[:], rs1[:, j:j + 1], gpost[:, hs], ALU.mult, ALU.mult,
                            r=[bk, ("rs1", j), "gpost"], w=["tmpy"])
                        TT("vector", hbuf[:, j, hs], hbuf[:, j, hs], tmpy, ALU.add, r=["tmpy", ("h", j)],
                           w=[("h", j)])
                    ACT(junk, hbuf[:, j, :], AF.Square, r=[("h", j)], w=[("ssh", j)], accum=ssh[:, j:j + 1])
                    TS("gpsimd", rs2[:, j:j + 1], ssh[:, j:j + 1], 1.0 / D, EPS, ALU.mult, ALU.add, r=[("ssh", j)],
                       w=[("rs2", j)])
                    TT("gpsimd", rs2[:, j:j + 1], rs2[:, j:j + 1], nhalf, ALU.pow, r=[("rs2", j), "nhalf"],
                       w=[("rs2", j)])
                    STT(vb, hbuf[:, j, :], rs2[:, j:j + 1], g3, ALU.mult, ALU.mult,
                        r=[("h", j), ("rs2", j), "g3"], w=["vb"])
                    for kc in range(KC):
                        TR(pst[:, kc * 128:(kc + 1) * 128], vb[:, kc * 128:(kc + 1) * 128], identb,
                           r=["vb", "c_identb"], w=["pst"])
                    CP("vector", vT[:, :, j * 128:(j + 1) * 128], pst[:].rearrange("p (a b) -> p a b", a=KC, b=128),
                       r=["pst"], w=[("vT", j)])
                vTk = [("vT", j) for j in range(4)]
                if _p2 < 1:
                    for j in range(4):
                        DMA("sync", out[(4 * c + j) * 128:(4 * c + j + 1) * 128, :], hbuf[:, j, :], r=[("h", j)], w=[("out", 4 * c + j)])
                    continue
                for p_ in range(npiece):
                    sl = wgu_n % 2
                    wgu_n += 1
                    f0 = p_ * 256
                    DMA("gpsimd", WGU[sl][:, :, 0:256], w_fg_r[:, :, f0:f0 + 256], r=[], w=[("WGUg", sl)])
                    DMA("gpsimd", WGU[sl][:, :, 256:512], w_fu_r[:, :, f0:f0 + 256], r=[], w=[("WGUu", sl)])
                    for f2 in range(2):
                        ft = 2 * p_ + f2
                        bG, kG = rot(7)
                        for kc in range(KC):
                            MM(bG[:], WGU[sl][:, kc, f2 * 128:(f2 + 1) * 128], vT[:, kc, :], kc == 0, kc == KC - 1,
                               r=[("WGUg", sl)] + vTk, w=[kG])
                        bU, kU = rot(7)
                        for kc in range(KC):
                            MM(bU[:], WGU[sl][:, kc, 256 + f2 * 128:256 + (f2 + 1) * 128], vT[:, kc, :], kc == 0,
                               kc == KC - 1, r=[("WGUu", sl)] + vTk, w=[kU])
                        ss_ = ft % 2
                        ACT(sgb[ss_], bG[:], AF.Silu, r=[kG], w=[("sgb", ss_)])
                        TT("vector", actT[:, ft, :], sgb[ss_], bU[:], ALU.mult, r=[("sgb", ss_), kU], w=[("actT", ft)])
                if _p2 < 2:
                    for j in range(4):
                        DMA("sync", out[(4 * c + j) * 128:(4 * c + j + 1) * 128, :], hbuf[:, j, :], r=[("h", j)], w=[("out", 4 * c + j)])
                    continue
                for hf in range(2):
                    hs = slice(hf * 512, (hf + 1) * 512)
                    accs = [(psf[3 + j], f"ps{3 + j}") for j in range(4)]
                    for p_ in range(npiece):
                        sl = wd_n % 2
                        wd_n += 1
                        DMA("gpsimd", WDp[sl], w_fd_r[:, 2 * p_:2 * p_ + 2, hs], r=[], w=[("WDp", sl)])
                        for f2 in range(2):
                            ft = 2 * p_ + f2
                            for j in range(4):
                                MM(accs[j][0][:], actT[:, ft, j * 128:(j + 1) * 128], WDp[sl][:, f2, :], ft == 0,
                                   ft == NFT - 1, r=[("actT", ft), ("WDp", sl)], w=[accs[j][1]])
                    for j in range(4):
                        ACT(junk[:, 0:512], accs[j][0][:], AF.Square, r=[accs[j][1]], w=[("ssf", j, hf)],
                            accum=ssf[:, j, hf:hf + 1])
                        CP("vector", ffb[:, j, hs], accs[j][0][:], r=[accs[j][1]], w=[("ffb", j)])
                for j in range(4):
                    t = 4 * c + j
                    TT("gpsimd", rs3[:, j:j + 1], ssf[:, j, 0:1], ssf[:, j, 1:2], ALU.add,
                       r=[("ssf", j, 0), ("ssf", j, 1)], w=[("rs3", j)])
                    TS("gpsimd", rs3[:, j:j + 1], rs3[:, j:j + 1], 1.0 / D, EPS, ALU.mult, ALU.add, r=[("rs3", j)],
                       w=[("rs3", j)])
                    TT("gpsimd", rs3[:, j:j + 1], rs3[:, j:j + 1], nhalf, ALU.pow, r=[("rs3", j), "nhalf"],
                       w=[("rs3", j)])
                    STT(ffb[:, j, :], ffb[:, j, :], rs3[:, j:j + 1], g4, ALU.mult, ALU.mult,
                        r=[("ffb", j), ("rs3", j), "g4"], w=[("ffb", j)])
                    TT("vector", ffb[:, j, :], ffb[:, j, :], hbuf[:, j, :], ALU.add, r=[("ffb", j), ("h", j)],
                       w=[("ffb", j)])
                    DMA("sync", out[t * 128:(t + 1) * 128, :], ffb[:, j, :], r=[("ffb", j)], w=[("out", t)])
            P.add("sync", None, r=[("out", t) for t in range(T)])
        P.barrier()
        print("arena peak bytes", A.peak * 2, "ops", {e: len(P.ops[e]) for e in ENGS})
        P.emit(block, sems)
    return nc


_CACHE = {}


def kernel(**inputs):
    S = 4096
    TOPK = 256
    B = 8
    x = np.asarray(inputs["x"], dtype=np.float32)
    consts = host_consts(S, TOPK)
    shared = {}
    for k in ("norm_mix_pre", "w_in", "b_forget", "b_gate", "w_branch_fox", "w_branch_dsa", "w_out",
              "norm_mix_post", "norm_ffn_pre", "w_ffn_gate", "w_ffn_up", "w_ffn_down", "norm_ffn_post"):
        shared[k] = np.ascontiguousarray(np.asarray(inputs[k], dtype=np.float32)[0])
    shared.update(consts)
    if "nc" not in _CACHE:
        _CACHE["nc"] = build(S, TOPK)
    nc = _CACHE["nc"]
    in_maps = []
    for b in range(B):
        m = dict(shared)
        m["x"] = np.ascontiguousarray(x[b])
        in_maps.append(m)
    res = run_bass_kernel_spmd(nc, in_maps, core_ids=list(range(B)))
    return np.stack([np.asarray(r["out"], dtype=np.float32) for r in res.results], axis=0)
```

```python
import numpy as np
from contextlib import ExitStack
import concourse.bass as bass
import concourse.mybir as mybir
from concourse.bass_utils import run_bass_kernel_spmd

F32 = mybir.dt.float32
BF16 = mybir.dt.bfloat16
ALU = mybir.AluOpType
AF = mybir.ActivationFunctionType
AX = mybir.AxisListType

ENGS = ("sync", "scalar", "tensor", "vector", "gpsimd")

D = 1024
KC = 8
DFF = 2816
NFT = DFF // 128
HD = 128
NIT = 14
ARENA_BYTES = 200 * 1024
EPS = 1e-6
NEG = -30000.0


class Prog:
    NDMA_SEM = 6

    def __init__(self):
        self.ops = {e: [] for e in ENGS}
        self.last_w = {}
        self.readers = {}
        self.last_compute = {e: None for e in ENGS}
        self.last_dmas = {e: [] for e in ENGS}

    def add(self, eng, fn, r=(), w=(), dma=False, extra=()):
        idx = len(self.ops[eng])
        deps = set(extra)
        px = [k for k in r if isinstance(k, str) and k.startswith("ps")]
        if px:
            r = [k for k in r if k not in px]
            w = list(w) + px
        for k in r:
            if k in self.last_w:
                deps.add(self.last_w[k])
        for k in w:
            if k in self.last_w:
                deps.add(self.last_w[k])
            for rd in self.readers.get(k, ()):
                deps.add(rd)
        me = (eng, idx)
        deps.discard(me)
        self.ops[eng].append(dict(fn=fn, deps=deps, dma=dma, signal=False, sig=None))
        for k in r:
            self.readers.setdefault(k, []).append(me)
        for k in w:
            self.last_w[k] = me
            self.readers[k] = []
        if fn is not None:
            if dma:
                self.last_dmas[eng] = (self.last_dmas[eng] + [me])[-self.NDMA_SEM:]
            else:
                self.last_compute[eng] = me
        return me

    def barrier(self):
        deps = []
        for e in ENGS:
            if self.last_compute[e] is not None:
                deps.append(self.last_compute[e])
            deps.extend(self.last_dmas[e])
        for e in ENGS:
            self.add(e, None, extra=deps)
        self.last_w = {}
        self.readers = {}

    def emit(self, block, sems):
        ops = self.ops
        for e in ENGS:
            for op in ops[e]:
                for (e2, j) in op["deps"]:
                    p = ops[e2][j]
                    if e2 == e and e == "tensor" and not p["dma"]:
                        continue
                    p["signal"] = True
        for e in ENGS:
            cnt = 0
            ndma = 0
            for op in ops[e]:
                if op["fn"] is None:
                    continue
                if op["dma"]:
                    s = sems["dma"][e][ndma % self.NDMA_SEM]
                    v = 16 * (ndma // self.NDMA_SEM + 1)
                    op["sig"] = (s, v)
                    op["prev"] = (s, v - 16)
                    ndma += 1
                elif op["signal"]:
                    cnt += 1
                    op["sig"] = (sems["cnt"][e], cnt)

        def make(e):
            def body(eng):
                waited = {}
                for op in ops[e]:
                    need = {}
                    for (e2, j) in op["deps"]:
                        p = ops[e2][j]
                        if p["sig"] is None:
                            continue
                        if e2 == e and e == "tensor" and not p["dma"]:
                            continue
                        s, v = p["sig"]
                        if need.get(id(s), (None, 0))[1] < v:
                            need[id(s)] = (s, v)
                    if op["dma"] and op["fn"] is not None:
                        s, v = op["prev"]
                        if v > 0 and need.get(id(s), (None, 0))[1] < v:
                            need[id(s)] = (s, v)
                    for sid, (s, v) in need.items():
                        if waited.get(sid, 0) < v:
                            eng.wait_ge(s, v)
                            waited[sid] = v
                    if op["fn"] is None:
                        continue
                    ins = op["fn"](eng)
                    if op["dma"]:
                        ins.then_inc(op["sig"][0], 16)
                    elif op["signal"]:
                        ins.then_inc(op["sig"][0], 1)
            return body

        block.sync(make("sync"))
        block.scalar(make("scalar"))
        block.tensor(make("tensor"))
        block.vector(make("vector"))
        block.gpsimd(make("gpsimd"))


class Arena:
    def __init__(self, t, nbytes):
        self.t = t
        self.cap = nbytes // 2
        self.off = 0
        self.peak = 0

    def alloc(self, shape, dtype):
        n = int(np.prod(shape))
        size = 4 if dtype == F32 else 2
        nel = n * size // 2
        nel = (nel + 15) // 16 * 16
        assert self.off + nel <= self.cap, f"arena overflow {self.off + nel} > {self.cap}"
        ap = self.t[:, self.off:self.off + n * size // 2]
        self.off += nel
        self.peak = max(self.peak, self.off)
        if dtype != BF16:
            ap = ap.bitcast(dtype)
        if len(shape) == 2:
            ap = ap.rearrange("p (a b) -> p a b", a=shape[0], b=shape[1])
        elif len(shape) == 3:
            ap = ap.rearrange("p (a b c) -> p a b c", a=shape[0], b=shape[1], c=shape[2])
        return ap

    def mark(self):
        return self.off

    def reset(self, m):
        self.off = m


def host_consts(S, TOPK):
    T = S // 128
    i = np.arange(128)
    c = {}
    c["ident"] = np.eye(128, dtype=np.float32)
    c["ident4"] = np.tile(np.eye(128, dtype=np.float32), (1, 4))
    c["cmT"] = np.where(i[:, None] > i[None, :], NEG, 0.0).astype(np.float32)
    c["cmQ"] = np.where(i[None, :] > i[:, None], NEG, 0.0).astype(np.float32)
    c["negm"] = np.where(i[None, :] > i[:, None], -1e30, 0.0).astype(np.float32)
    c["posm"] = np.where(i[None, :] > i[:, None], 1e30, 0.0).astype(np.float32)
    c["tri"] = (i[:, None] <= i[None, :]).astype(np.float32)
    c["ones"] = np.ones((128, 128), np.float32)
    sel0 = np.zeros((128, 128), np.float32)
    sel0[0, :] = 1.0
    c["sel0"] = sel0
    pos = np.arange(S, dtype=np.float32)

    def tab(rot):
        half = rot // 2
        inv = np.float32(500000.0) ** (-np.arange(half, dtype=np.float32) * np.float32(2.0) / np.float32(rot))
        ang = (pos[:, None] * inv[None, :]).astype(np.float32)
        cs = np.cos(ang).astype(np.float32).reshape(T, 128, half).transpose(1, 0, 2)
        sn = np.sin(ang).astype(np.float32).reshape(T, 128, half).transpose(1, 0, 2)
        return np.ascontiguousarray(cs), np.ascontiguousarray(sn)

    c["cosh"], c["sinh"] = tab(32)
    c["cosi"], c["sini"] = tab(16)
    c["cvec"] = np.tile((2.0 ** -(np.arange(NIT) + 1.0)).astype(np.float32)[None, :], (128, 1))
    return c


CONST_SHAPES = lambda S: {
    "ident": [128, 128], "ident4": [128, 512], "cmT": [128, 128], "cmQ": [128, 128],
    "negm": [128, 128], "posm": [128, 128], "tri": [128, 128], "ones": [128, 128],
    "sel0": [128, 128], "cosh": [128, S // 128, 16], "sinh": [128, S // 128, 16],
    "cosi": [128, S // 128, 8], "sini": [128, S // 128, 8], "cvec": [128, NIT],
}


def build(S, TOPK, passes="FDM2", dbg=None):
    T = S // 128
    NCH = S // 512
    KT0 = TOPK // 128
    nc = bass.Bass("TRN2", target_bir_lowering=False)
    dr = lambda n, s: nc.dram_tensor(n, s, F32, kind="ExternalInput").ap()
    x = dr("x", [S, D])
    g_pre = dr("norm_mix_pre", [D])
    w_in = dr("w_in", [D, 4940])
    b_forget = dr("b_forget", [4])
    b_gate = dr("b_gate", [2, D])
    w_bf = dr("w_branch_fox", [512, D])
    w_bd = dr("w_branch_dsa", [512, D])
    w_out = dr("w_out", [D, D])
    g_post = dr("norm_mix_post", [D])
    g_fpre = dr("norm_ffn_pre", [D])
    w_fg = dr("w_ffn_gate", [D, DFF])
    w_fu = dr("w_ffn_up", [D, DFF])
    w_fd = dr("w_ffn_down", [DFF, D])
    g_fpost = dr("norm_ffn_post", [D])
    cst = {k: dr(k, s) for k, s in CONST_SHAPES(S).items()}
    out = nc.dram_tensor("out", [S, D], F32, kind="ExternalOutput").ap()
    scr_g = nc.dram_tensor("scr_g", [D, DFF], BF16, kind="Internal").ap()
    scr_u = nc.dram_tensor("scr_u", [D, DFF], BF16, kind="Internal").ap()
    scr_d = nc.dram_tensor("scr_d", [DFF, D], BF16, kind="Internal").ap()
    dbg_out = nc.dram_tensor("dbg", [128, 8 * S], F32, kind="ExternalOutput").ap() if dbg else None
    dbg_in = nc.dram_tensor("dbg_in", [128, 8 * S], F32, kind="ExternalInput").ap() if dbg in ("Mi", "2i", "Mif", "Mid") else None

    P = Prog()
    scale = float(HD ** -0.5)
    idx_scale = float((64 ** -0.5) * (8 ** -0.5))

    with ExitStack() as es:
        arena_t = es.enter_context(nc.sbuf_tensor("arena", [128, ARENA_BYTES // 2], BF16))
        A = Arena(arena_t, ARENA_BYTES)
        psf = [es.enter_context(nc.psum_tensor(f"ps{i}", [128, 512], F32)) for i in range(7)]
        pst = es.enter_context(nc.psum_tensor("pst", [128, 1024], BF16))
        sems = {"cnt": {e: es.enter_context(nc.semaphore("c_" + e)) for e in ENGS},
                "dma": {e: [es.enter_context(nc.semaphore(f"d_{e}{i}")) for i in range(Prog.NDMA_SEM)]
                        for e in ("sync", "gpsimd")}}
        sems["dma"]["scalar"] = []
        block = es.enter_context(nc.Block())

        def MM(out_, lhsT, rhs, start, stop, r, w):
            P.add("tensor", lambda e: e.matmul(out=out_, lhsT=lhsT, rhs=rhs, start=start, stop=stop), r=r, w=w)

        def TR(out_, in_, idn, r, w):
            P.add("tensor", lambda e: e.transpose(out=out_, in_=in_, identity=idn), r=r, w=w)

        def ACT(out_, in_, func, r, w, bias=0.0, scale_=1.0, accum=None):
            if accum is None:
                P.add("scalar", lambda e: e.activation(out=out_, in_=in_, func=func, bias=bias, scale=scale_), r=r, w=w)
            else:
                P.add("scalar", lambda e: e.activation(out=out_, in_=in_, func=func, bias=bias, scale=scale_,
                                                       accum_out=accum), r=r, w=w)

        def TS(eng, out_, in0, s1, s2, op0, op1, r, w, accum=None):
            if accum is not None:
                P.add(eng, lambda e: e.tensor_scalar(out=out_, in0=in0, scalar1=s1, scalar2=s2, op0=op0, op1=op1,
                                                     accum_out=accum), r=r, w=w)
            elif op1 is None:
                P.add(eng, lambda e: e.tensor_scalar(out=out_, in0=in0, scalar1=s1, scalar2=None, op0=op0), r=r, w=w)
            else:
                P.add(eng, lambda e: e.tensor_scalar(out=out_, in0=in0, scalar1=s1, scalar2=s2, op0=op0, op1=op1),
                      r=r, w=w)

        def TT(eng, out_, in0, in1, op, r, w):
            P.add(eng, lambda e: e.tensor_tensor(out=out_, in0=in0, in1=in1, op=op), r=r, w=w)

        def STT(out_, in0, sc, in1, op0, op1, r, w):
            P.add("vector", lambda e: e.scalar_tensor_tensor(out=out_, in0=in0, scalar=sc, in1=in1, op0=op0, op1=op1),
                  r=r, w=w)

        def CP(eng, out_, in_, r, w):
            if eng == "scalar":
                P.add(eng, lambda e: e.copy(out=out_, in_=in_), r=r, w=w)
            else:
                P.add(eng, lambda e: e.tensor_copy(out=out_, in_=in_), r=r, w=w)

        def RCP(out_, in_, r, w):
            P.add("vector", lambda e: e.reciprocal(out=out_, in_=in_), r=r, w=w)

        def RED(out_, in_, op, r, w):
            P.add("vector", lambda e: e.tensor_reduce(out=out_, in_=in_, axis=AX.X, op=op), r=r, w=w)

        def MS(eng, ap, val, w):
            P.add(eng, lambda e: e.memset(ap, val), w=w)

        def DMA(eng, out_, in_, r, w):
            P.add(eng, lambda e: e.dma_start(out=out_, in_=in_), r=r, w=w, dma=True)

        rot_state = {"i": 0}

        def rot(nb=3):
            i = rot_state["i"] % nb
            rot_state["i"] += 1
            return psf[i], f"ps{i}"

        identb = A.alloc([128], BF16)
        ident4b = A.alloc([512], BF16)
        cmTb = A.alloc([128], BF16)
        cmQb = A.alloc([128], BF16)
        onesb = A.alloc([128], BF16)
        negm = A.alloc([128], F32)
        posm = A.alloc([128], F32)
        cosh = A.alloc([T, 16], F32)
        sinh = A.alloc([T, 16], F32)
        cosi = A.alloc([T, 8], F32)
        sini = A.alloc([T, 8], F32)
        cvec = A.alloc([NIT], F32)
        nhalf = A.alloc([1], F32)
        identf = A.alloc([128], F32)
        g8 = A.alloc([128], F32)
        gT = A.alloc([8], F32)
        odT = A.alloc([4, S], BF16)
        A.full_cap = A.cap
        ofT = arena_t[:, A.cap - 4 * S:A.cap].rearrange("p (a b) -> p a b", a=4, b=S)
        A.cap = A.full_cap - 4 * S
        for ap_, nm in ((identb, "ident"), (ident4b, "ident4"), (cmTb, "cmT"), (cmQb, "cmQ"), (onesb, "ones")):
            DMA("gpsimd", ap_, cst[nm], r=[], w=["c_" + nm + "b"])
        for ap_, nm in ((negm, "negm"), (posm, "posm"), (cosh, "cosh"), (sinh, "sinh"), (cosi, "cosi"), (sini, "sini"),
                        (cvec, "cvec")):
            DMA("sync", ap_, cst[nm], r=[], w=["c_" + nm])
        DMA("sync", identf, cst["ident"], r=[], w=["c_ident"])
        DMA("sync", g8[0:8, :], g_pre.rearrange("(kc p) -> kc p", p=128), r=[], w=["g8"])
        TR(psf[6][:, 0:8], g8[0:8, :], identf[0:8, 0:8], r=["g8", "c_ident"], w=["ps6"])
        CP("vector", gT, psf[6][:, 0:8], r=["ps6"], w=["gT"])
        MS("gpsimd", nhalf, -0.5, w=["nhalf"])
        persist_mark = A.mark()
        if "2" in passes:
            for hlf in range(2):
                DMA("gpsimd", scr_g[hlf * 512:(hlf + 1) * 512, :], w_fg[hlf * 512:(hlf + 1) * 512, :], r=[], w=[("scr_g", hlf)])
                DMA("gpsimd", scr_u[hlf * 512:(hlf + 1) * 512, :], w_fu[hlf * 512:(hlf + 1) * 512, :], r=[], w=[("scr_u", hlf)])
                DMA("gpsimd", scr_d[hlf * 1408:(hlf + 1) * 1408, :], w_fd[hlf * 1408:(hlf + 1) * 1408, :], r=[], w=[("scr_d", hlf)])
            if "D" not in passes and "F" not in passes and "M" not in passes:
                P.barrier()

        def u_stage(t, j, bufs):
            s = t % 2
            xs = t % len(bufs["xt"])
            xt, ssq, rstd, ub, uT = bufs["xt"][xs], bufs["ss"][s], bufs["rstd"][s], bufs["ub"][s], bufs["uT"]
            DMA("sync", xt, x[t * 128:(t + 1) * 128, :], r=[], w=[("xt", xs)])
            ACT(ub, xt, AF.Square, r=[("xt", xs)], w=[("ss", s), ("ub", s)], accum=ssq)
            TS("gpsimd", rstd, ssq, 1.0 / D, EPS, ALU.mult, ALU.add, r=[("ss", s)], w=[("rstd", s)])
            TT("gpsimd", rstd, rstd, nhalf, ALU.pow, r=[("rstd", s), "nhalf"], w=[("rstd", s)])
            ACT(ub, xt, AF.Copy, r=[("xt", xs), ("rstd", s)], w=[("ub", s)], scale_=rstd)
            for kc in range(KC):
                TR(pst[:, kc * 128:(kc + 1) * 128], ub[:, kc * 128:(kc + 1) * 128], identb,
                   r=[("ub", s), "c_identb"], w=["pst"])
            CP("scalar", uT[:, :, j * 128:(j + 1) * 128], pst[:].rearrange("p (a b) -> p a b", a=KC, b=128),
               r=["pst"], w=[("uT", j)])

        def fold_gain(W, keys):
            for kc in range(KC):
                TS("vector", W[:, kc, :], W[:, kc, :], gT[:, kc:kc + 1], None, ALU.mult, None, r=list(keys) + ["gT"],
                   w=list(keys))

        def u_bufs(nx=2):
            return dict(xt=[A.alloc([D], F32) for _ in range(nx)], ss=[A.alloc([1], F32) for _ in range(2)],
                        rstd=[A.alloc([1], F32) for _ in range(2)], ub=[A.alloc([D], BF16) for _ in range(2)],
                        uT=A.alloc([KC, 512], BF16))

        w_in_r = w_in.rearrange("(kc p) c -> p kc c", p=128)

        if "D" in passes:
            A.cap = A.full_cap
            WD_ = A.alloc([KC, 1352], BF16)
            KdT = A.alloc([S], BF16)
            Vd = A.alloc([T, 128], BF16)
            kiT = A.alloc([S], BF16)
            QQ = [A.alloc([1024], BF16) for _ in range(3)]
            sgnD = [A.alloc([8, 128], BF16) for _ in range(3)]
            scs = [A.alloc([S], F32) for _ in range(2)]
            Mneg = [A.alloc([S], BF16) for _ in range(2)]
            R = [A.alloc([8, 512], BF16) for _ in range(1)]
            qd_f = A.alloc([4, 128], F32)
            qi_f = A.alloc([8, 64], F32)
            g4_f = A.alloc([328], F32)
            qd_b = A.alloc([4, 128], BF16)
            qi_b = A.alloc([8, 64], BF16)
            kk_b = A.alloc([256], BF16)
            rt = [A.alloc([8, 16], F32) for _ in range(4)]
            aw = A.alloc([8], F32)
            sg01 = A.alloc([8], F32)
            sgn = A.alloc([8], F32)
            tmpds = [A.alloc([128], F32) for _ in range(2)]
            sts = [A.alloc([8], F32) for _ in range(2)]
            Wc = A.alloc([NIT], F32)
            mids = A.alloc([NIT + 1], F32)
            cnts = A.alloc([NIT], F32)
            sgs = A.alloc([NIT], F32)
            pT = [A.alloc([512], BF16) for _ in range(2)]
            oS = [A.alloc([512], F32) for _ in range(2)]
            dS = [A.alloc([512], F32) for _ in range(2)]
            rden = A.alloc([512], F32)
            ub_ = u_bufs(2)
            for (d0, s0, n_) in ((0, 1540, 512), (512, 2308, 512), (1024, 2052, 256), (1280, 2820, 72)):
                DMA("gpsimd", WD_[:, :, d0:d0 + n_], w_in_r[:, :, s0:s0 + n_], r=[], w=[("WD", d0)])
            WDK = [("WD", 0), ("WD", 512), ("WD", 1024), ("WD", 1280)]
            fold_gain(WD_, WDK)

            def rope(src3, dst3, nh, half, cos_t, sin_t, key_src, key_dst):
                x1 = src3[:, :, 0:half]
                x2 = src3[:, :, half:2 * half]
                cb_ = cos_t.unsqueeze(1).broadcast_to([128, nh, half])
                sb_ = sin_t.unsqueeze(1).broadcast_to([128, nh, half])
                ta, tb, tc, td = [r_[:, 0:nh, 0:half] for r_ in rt]
                TT("gpsimd", ta, x1, cb_, ALU.mult, r=[key_src], w=["rt0"])
                TT("gpsimd", tb, x2, sb_, ALU.mult, r=[key_src], w=["rt1"])
                TT("gpsimd", tc, x2, cb_, ALU.mult, r=[key_src], w=["rt2"])
                TT("gpsimd", td, x1, sb_, ALU.mult, r=[key_src], w=["rt3"])
                TT("gpsimd", dst3[:, :, 0:half], ta, tb, ALU.subtract, r=["rt0", "rt1"], w=[key_dst])
                TT("gpsimd", dst3[:, :, half:2 * half], tc, td, ALU.add, r=["rt2", "rt3"], w=[key_dst])

            def prepP(t):
                c, j = t // 4, t % 4
                qs = t % 3
                if j == 0:
                    for jj in range(4):
                        u_stage(4 * c + jj, jj, ub_)
                uT = ub_["uT"]
                for (dst, c0, wn, key) in ((qd_f, 0, 512, "qd_f"), (qi_f, 512, 512, "qi_f"), (g4_f, 1024, 328, "g4_f")):
                    bank, bk = rot()
                    for kc in range(KC):
                        MM(bank[:, 0:wn], uT[:, kc, j * 128:(j + 1) * 128], WD_[:, kc, c0:c0 + wn], kc == 0,
                           kc == KC - 1, r=WDK + [("uT", j)], w=[bk])
                    dflat = dst if key == "g4_f" else dst.rearrange("p a b -> p (a b)")
                    CP("scalar", dflat, bank[:, 0:wn], r=[bk], w=[key])
                rope(qd_f, qd_b, 4, 16, cosh[:, t, :], sinh[:, t, :], "qd_f", "qd_b")
                CP("gpsimd", qd_b[:, :, 32:128], qd_f[:, :, 32:128], r=["qd_f"], w=["qd_b"])
                TS("gpsimd", sg01, g4_f[:, 320:328], 0.0, None, ALU.is_ge, None, r=["g4_f"], w=["sg01"])
                TS("gpsimd", sgn, sg01, 2.0, -1.0, ALU.mult, ALU.add, r=["sg01"], w=["sgn"])
                TS("gpsimd", aw, sg01, 2.0 * idx_scale, -idx_scale, ALU.mult, ALU.add, r=["sg01"], w=["aw"])
                TT("gpsimd", aw, aw, g4_f[:, 320:328], ALU.mult, r=["aw", "g4_f"], w=["aw"])
                rope(qi_f, qi_f, 8, 8, cosi[:, t, :], sini[:, t, :], "qi_f", "qi_f")
                TT("gpsimd", qi_b, qi_f, aw.unsqueeze(2).broadcast_to([128, 8, 64]), ALU.mult, r=["qi_f", "aw"],
                   w=["qi_b"])
                kd3 = g4_f[:, 0:128].rearrange("p (a b) -> p a b", a=1, b=128)
                kdb3 = kk_b[:, 0:128].rearrange("p (a b) -> p a b", a=1, b=128)
                rope(kd3, kdb3, 1, 16, cosh[:, t, :], sinh[:, t, :], "g4_f", "kk_b")
                CP("gpsimd", kk_b[:, 32:128], g4_f[:, 32:128], r=["g4_f"], w=["kk_b"])
                ki3 = g4_f[:, 256:320].rearrange("p (a b) -> p a b", a=1, b=64)
                kib3 = kk_b[:, 128:192].rearrange("p (a b) -> p a b", a=1, b=64)
                rope(ki3, kib3, 1, 8, cosi[:, t, :], sini[:, t, :], "g4_f", "kk_b")
                CP("gpsimd", kk_b[:, 144:192], g4_f[:, 272:320], r=["g4_f"], w=["kk_b"])
                CP("gpsimd", kk_b[:, 192:256], kk_b[:, 128:192], r=["kk_b"], w=["kk_b2"])
                CP("gpsimd", Vd[:, t, :], g4_f[:, 128:256], r=["g4_f"], w=[("Vd", t)])
                TT("gpsimd", sgnD[qs], identb.unsqueeze(1).broadcast_to([128, 8, 128]),
                   sgn.unsqueeze(2).broadcast_to([128, 8, 128]), ALU.mult, r=["c_identb", "sgn"], w=[("sgnD", qs)])

            def prepT(t):
                qs = t % 3
                for i_ in range(2):
                    TR(pst[:, i_ * 128:(i_ + 1) * 128], kk_b[:, i_ * 128:(i_ + 1) * 128], identb,
                       r=["kk_b", "kk_b2", "c_identb"], w=["pst"])
                CP("scalar", KdT[:, t * 128:(t + 1) * 128], pst[:, 0:128], r=["pst"], w=[("KdT", t)])
                CP("scalar", kiT[:, t * 128:(t + 1) * 128], pst[:, 128:256], r=["pst"], w=[("kiT", t)])
                qdb2 = qd_b.rearrange("p a b -> p (a b)")
                qib2 = qi_b.rearrange("p a b -> p (a b)")
                for i_ in range(4):
                    TR(pst[:, i_ * 128:(i_ + 1) * 128], qdb2[:, i_ * 128:(i_ + 1) * 128], identb,
                       r=["qd_b", "c_identb"], w=["pst"])
                for i_ in range(4):
                    TR(pst[:, 512 + i_ * 128:512 + (i_ + 1) * 128], qib2[:, i_ * 128:(i_ + 1) * 128], identb,
                       r=["qi_b", "c_identb"], w=["pst"])
                CP("scalar", QQ[qs], pst[:], r=["pst"], w=[("QQ", qs)])

            def stageA(t):
                q3 = t % 3
                qs = t % 2
                n = (t + 1) * 128
                sc = scs[qs]
                tmpd = tmpds[qs]
                if t < KT0:
                    return
                nk5 = (n + 511) // 512
                for k5 in range(nk5):
                    c0 = k5 * 512
                    wn = min(512, n - c0)
                    rs = 0
                    kik = [("kiT", tt) for tt in range(c0 // 128, (c0 + wn) // 128)]
                    for hd in range(8):
                        hp, hf = hd // 2, hd % 2
                        bank, bk = rot()
                        MM(bank[:, 0:wn], QQ[q3][64 * hf:64 * hf + 64, 512 + hp * 128:512 + (hp + 1) * 128],
                           kiT[64 * hf:64 * hf + 64, c0:c0 + wn], True, True, r=[("QQ", q3)] + kik, w=[bk])
                        ACT(R[rs][:, hd, 0:wn], bank[:, 0:wn], AF.Relu, r=[bk], w=[("R", rs, hd)])
                    sb_, sbk = psf[3], "ps3"
                    for hd in range(8):
                        MM(sb_[:, 0:wn], sgnD[q3][:, hd, :], R[rs][:, hd, 0:wn], hd == 0, hd == 7,
                           r=[("sgnD", q3), ("R", rs, hd)], w=[sbk])
                    CP("scalar", sc[:, c0:c0 + wn], sb_[:, 0:wn], r=[sbk], w=[("sc", qs, k5)])
                sck = [("sc", qs, k5) for k5 in range(nk5)]
                TT("gpsimd", tmpd, sc[:, n - 128:n], posm, ALU.add, r=sck + ["c_posm"], w=[("tmpd", qs)])
                TT("gpsimd", sc[:, n - 128:n], sc[:, n - 128:n], negm, ALU.add, r=sck + ["c_negm", ("tmpd", qs)],
                   w=[("sc", qs, nk5 - 1)])

            def stageB(t):
                qs = t % 2
                ms_ = qs
                n = (t + 1) * 128
                sc = scs[qs]
                tmpd = tmpds[qs]
                st = sts[qs]
                if t < KT0:
                    if n > 128:
                        MS("gpsimd", Mneg[ms_][:, 0:n - 128], 0.0, w=[("Mneg", ms_)])
                    CP("gpsimd", Mneg[ms_][:, n - 128:n], cmQb, r=["c_cmQb"], w=[("Mneg", ms_)])
                    return
                nk5 = (n + 511) // 512
                sck = [("sc", qs, k5) for k5 in range(nk5)]
                RED(st[:, 0:1], sc[:, 0:n], ALU.max, r=sck, w=[("hi", qs)])
                RED(st[:, 1:2], tmpd, ALU.min, r=[("tmpd", qs)], w=[("m1", qs)])
                RED(st[:, 2:3], sc[:, 0:n - 128], ALU.min, r=sck, w=[("m2", qs)])
                TT("vector", st[:, 3:4], st[:, 1:2], st[:, 2:3], ALU.min, r=[("m1", qs), ("m2", qs)], w=[("lo", qs)])
                TT("vector", st[:, 4:5], st[:, 0:1], st[:, 3:4], ALU.subtract, r=[("hi", qs), ("lo", qs)],
                   w=[("Wd", qs)])
                TS("vector", Wc, cvec, st[:, 4:5], None, ALU.mult, None, r=["c_cvec", ("Wd", qs)], w=["Wc"])
                TT("vector", mids[:, 0:1], st[:, 3:4], Wc[:, 0:1], ALU.add, r=[("lo", qs), "Wc"], w=[("mid", 0)])
                for it in range(NIT):
                    TS("vector", Mneg[ms_][:, 0:n], sc[:, 0:n], mids[:, it:it + 1], None, ALU.is_ge, ALU.add,
                       r=sck + [("mid", it)], w=[("cnt", it), ("Mneg", ms_)], accum=cnts[:, it:it + 1])
                    TS("vector", sgs[:, it:it + 1], cnts[:, it:it + 1], TOPK - 0.5, 0.5, ALU.is_ge, ALU.subtract,
                       r=[("cnt", it)], w=[("sg", it)])
                    STT(mids[:, it + 1:it + 2], sgs[:, it:it + 1], Wc[:, it:it + 1], mids[:, it:it + 1], ALU.mult,
                        ALU.add, r=[("sg", it), "Wc", ("mid", it)], w=[("mid", it + 1)])
                STT(st[:, 5:6], st[:, 4:5], -(2.0 ** -(NIT + 1)), mids[:, NIT:NIT + 1], ALU.mult, ALU.add,
                    r=[("Wd", qs), ("mid", NIT)], w=[("tau", qs)])
                TS("vector", Mneg[ms_][:, 0:n], sc[:, 0:n], st[:, 5:6], NEG, ALU.is_lt, ALU.mult,
                   r=sck + [("tau", qs)], w=[("Mneg", ms_)])

            def stageC(t):
                qs = t % 2
                q3 = t % 3
                ms_ = qs
                ob, obk, db, dbk = psf[4], "ps4", psf[5], "ps5"
                def qk(kt):
                    bank, bk = rot()
                    MM(bank[:], KdT[:, kt * 128:(kt + 1) * 128], QQ[q3][:, 0:512], True, False,
                       r=[("KdT", kt), ("QQ", q3)], w=[bk])
                    MM(bank[:], Mneg[ms_][:, kt * 128:(kt + 1) * 128], ident4b, False, True,
                       r=[("Mneg", ms_), "c_ident4b"], w=[bk])
                    return bank, bk
                cur = qk(0)
                for kt in range(t + 1):
                    nxt = qk(kt + 1) if kt + 1 <= t else None
                    bank, bk = cur
                    ps_ = kt % 2
                    ACT(pT[ps_], bank[:], AF.Exp, r=[bk], w=[("pT", ps_)], scale_=scale)
                    MM(ob[:], Vd[:, kt, :], pT[ps_], kt == 0, kt == t, r=[("Vd", kt), ("pT", ps_)], w=[obk])
                    MM(db[:], onesb, pT[ps_], kt == 0, kt == t, r=["c_onesb", ("pT", ps_)], w=[dbk])
                    cur = nxt
                CP("scalar", oS[qs], ob[:], r=[obk], w=[("oS", qs)])
                CP("scalar", dS[qs], db[:], r=[dbk], w=[("dS", qs)])

            def stageN(t):
                qs = t % 2
                RCP(rden, dS[qs], r=[("dS", qs)], w=["rden"])
                TT("vector", odT[:, :, t * 128:(t + 1) * 128], oS[qs].rearrange("p (a b) -> p a b", a=4, b=128),
                   rden.rearrange("p (a b) -> p a b", a=4, b=128), ALU.mult, r=[("oS", qs), "rden"], w=[("odT", t)])

            prepP(0)
            prepT(0)
            stageA(0)
            if T > 1:
                prepP(1)
                prepT(1)
            for t in range(T):
                if t + 2 < T:
                    prepP(t + 2)
                if t + 1 < T:
                    stageA(t + 1)
                stageB(t)
                if t >= 1:
                    stageN(t - 1)
                if t + 2 < T:
                    prepT(t + 2)
                stageC(t)
            stageN(T - 1)
            P.barrier()
            A.reset(persist_mark)
            A.cap = A.full_cap - 4 * S
            if dbg in ("D", "Y"):
                if dbg == "D":
                    DMA("gpsimd", dbg_out[:, 4 * S:8 * S], odT.rearrange("p a b -> p (a b)"), r=[], w=["dbgD"])
                if dbg == "Y":
                    DMA("gpsimd", dbg_out[:, 0:4 * S], ofT.rearrange("p a b -> p (a b)"), r=[], w=["dbgD"])
                P.add("sync", None, r=["dbgD"])
                P.barrier()

        if "F" in passes:
            tri = A.alloc([128], F32)
            onesf = A.alloc([128], F32)
            sel0 = A.alloc([128], F32)
            for ap_, nm in ((tri, "tri"), (onesf, "ones"), (sel0, "sel0")):
                DMA("sync", ap_, cst[nm], r=[], w=["c_" + nm])
            WF = A.alloc([KC, 1540], BF16)
            KfT = A.alloc([4, S], BF16)
            Vf = A.alloc([T, 512], BF16)
            negc = A.alloc([T, 4], F32)
            biasc = A.alloc([T, 4], F32)
            bfb = A.alloc([4, 4], F32)
            carry = A.alloc([4], F32)
            refb = A.alloc([4], F32)
            v3 = lambda a_: a_.rearrange("p (a b) -> p a b", a=4, b=4)
            ztf = A.alloc([16], F32)
            etf = A.alloc([16], F32)
            ltf = A.alloc([16], F32)
            Lsf = A.alloc([16], F32)
            zt, et, lt, Ls = v3(ztf), v3(etf), v3(ltf), v3(Lsf)
            ltot = A.alloc([4], F32)
            QfT = A.alloc([4, 512], BF16)
            pT = [A.alloc([512], BF16) for _ in range(2)]
            rden = A.alloc([512], F32)
            ub_ = u_bufs()
            for half in range(2):
                DMA("gpsimd", WF[:, half * 4:(half + 1) * 4, :], w_in_r[:, half * 4:(half + 1) * 4, 0:1540],
                    r=[], w=[("WF", half)])
            WFK = [("WF", 0), ("WF", 1)]
            fold_gain(WF, WFK)
            for jj in range(4):
                DMA("sync", bfb[:, jj, :], b_forget.partition_broadcast(128), r=[], w=[("bfb", jj)])
            MS("gpsimd", carry, 0.0, w=["carry"])
            MS("gpsimd", Ls[:, 0, :], 0.0, w=["Ls0"])
            oset = 0
            for c in range(NCH):
                for j in range(4):
                    u_stage(4 * c + j, j, ub_)
                uT = ub_["uT"]
                uTk = [("uT", j) for j in range(4)]
                for g in range(8):
                    bank, bk = rot()
                    for kc in range(KC):
                        MM(bank[:], WF[:, kc, g * 128:(g + 1) * 128], uT[:, kc, :], kc == 0, kc == KC - 1,
                           r=WFK + uTk, w=[bk])
                    if g < 4:
                        CP("vector", QfT[:, g, :], bank[:], r=[bk], w=[("QfT", g)])
                    else:
                        CP("vector", KfT[:, g - 4, c * 512:(c + 1) * 512], bank[:], r=[bk], w=[("KfT", g - 4, c)])
                fbank, fbk = psf[3], "ps3"
                for j in range(4):
                    t = 4 * c + j
                    bank, bk = rot()
                    for kc in range(KC):
                        MM(bank[:], uT[:, kc, j * 128:(j + 1) * 128], WF[:, kc, 1024:1536], kc == 0, kc == KC - 1,
                           r=WFK + [("uT", j)], w=[bk])
                    CP("vector", Vf[:, t, :], bank[:], r=[bk], w=[("Vf", t)])
                    for kc in range(KC):
                        MM(fbank[:, j * 4:(j + 1) * 4], uT[:, kc, j * 128:(j + 1) * 128], WF[:, kc, 1536:1540],
                           kc == 0, kc == KC - 1, r=WFK + [("uT", j)], w=[fbk])
                TT("vector", zt, fbank[:, 0:16].rearrange("p (a b) -> p a b", a=4, b=4), bfb, ALU.add,
                   r=[fbk] + [("bfb", jj) for jj in range(4)], w=["zt"])
                ACT(et, zt, AF.Exp, r=["zt"], w=["et"], scale_=-1.0)
                ACT(lt, et, AF.Ln, r=["et"], w=["lt"], bias=1.0)
                CP("gpsimd", Ls[:, 1, :], lt[:, 0, :], r=["lt"], w=["Ls1"])
                TT("gpsimd", Ls[:, 2, :], Ls[:, 1, :], lt[:, 1, :], ALU.add, r=["lt", "Ls1"], w=["Ls2"])
                TT("gpsimd", Ls[:, 3, :], Ls[:, 2, :], lt[:, 2, :], ALU.add, r=["lt", "Ls2"], w=["Ls3"])
                TT("gpsimd", ltot, Ls[:, 3, :], lt[:, 3, :], ALU.add, r=["lt", "Ls3"], w=["ltot"])
                cb, cbk = psf[4], "ps4"
                MM(cb[:, 0:16], tri, ltf, True, False, r=["c_tri", "lt"], w=[cbk])
                MM(cb[:, 0:16], onesf, Lsf, False, True, r=["c_ones", "Ls0", "Ls1", "Ls2", "Ls3"], w=[cbk])
                MM(cb[:, 16:20], onesf, ltot, True, True, r=["c_ones", "ltot"], w=[cbk])
                TT("vector", negc[:, 4 * c:4 * c + 4, :], cb[:, 0:16].rearrange("p (a b) -> p a b", a=4, b=4),
                   carry.unsqueeze(1).broadcast_to([128, 4, 4]), ALU.add, r=[cbk, "carry"], w=[("negc", c)])
                TT("vector", carry, cb[:, 16:20], carry, ALU.add, r=[cbk, "carry"], w=["carry"])
                rb, rbk = psf[5], "ps5"
                MM(rb[:, 0:4], sel0, negc[:, 4 * c + 2, :], True, True, r=["c_sel0", ("negc", c)], w=[rbk])
                CP("vector", refb, rb[:, 0:4], r=[rbk], w=["refb"])
                nkt = 4 * c + 4
                for h in range(4):
                    TS("vector", biasc[:, 0:nkt, h], negc[:, 0:nkt, h], refb[:, h:h + 1], None, ALU.subtract, None,
                       r=[("negc", cc) for cc in range(c + 1)] + ["refb"], w=[("biasc", h)])
                for h in range(4):
                    ob, obk, db, dbk = (psf[3], "ps3", psf[4], "ps4") if oset == 0 else (psf[5], "ps5", psf[6], "ps6")
                    oset ^= 1
                    def qkf(kt):
                        off = max(0, kt - 4 * c) * 128
                        diag = kt >= 4 * c
                        bank, bk = rot()
                        MM(bank[:, off:512], KfT[:, h, kt * 128:(kt + 1) * 128], QfT[:, h, off:512], True, not diag,
                           r=[("KfT", h, kt // 4), ("QfT", h)], w=[bk])
                        if diag:
                            MM(bank[:, off:off + 128], identb, cmTb, False, True, r=["c_identb", "c_cmTb"], w=[bk])
                        return bank, bk, off
                    cur = qkf(0)
                    for kt in range(nkt):
                        nxt = qkf(kt + 1) if kt + 1 < nkt else None
                        bank, bk, off = cur
                        ps_ = kt % 2
                        ACT(pT[ps_][:, off:512], bank[:, off:512], AF.Exp, r=[bk, ("biasc", h)], w=[("pT", ps_)],
                            bias=biasc[:, kt, h:h + 1], scale_=scale)
                        MM(ob[:, off:512], Vf[:, kt, h * 128:(h + 1) * 128], pT[ps_][:, off:512], kt == 0,
                           kt == nkt - 1, r=[("Vf", kt), ("pT", ps_)], w=[obk])
                        MM(db[:, off:512], onesb, pT[ps_][:, off:512], kt == 0, kt == nkt - 1,
                           r=["c_onesb", ("pT", ps_)], w=[dbk])
                        cur = nxt
                    RCP(rden, db[:], r=[dbk], w=["rden"])
                    TT("vector", ofT[:, h, c * 512:(c + 1) * 512], ob[:], rden, ALU.mult, r=[obk, "rden"],
                       w=[("ofT", h, c)])
            P.barrier()
            A.reset(persist_mark)
            if dbg == "F":
                DMA("gpsimd", dbg_out[:, 0:4 * S], ofT.rearrange("p a b -> p (a b)"), r=[], w=["dbgF"])
                P.add("sync", None, r=["dbgF"])
                P.barrier()

        if dbg in ("Mi", "2i", "Mif", "Mid"):
            if dbg != "Mif":
                DMA("gpsimd", ofT.rearrange("p a b -> p (a b)"), dbg_in[:, 0:4 * S], r=[], w=["dbgi"])
            if dbg != "Mid":
                DMA("gpsimd", odT.rearrange("p a b -> p (a b)"), dbg_in[:, 4 * S:8 * S], r=[], w=["dbgi2"])
            P.barrier()
        if "M" in passes:
            Wg = A.alloc([KC, 2048], BF16)
            Wbf = A.alloc([4, D], BF16)
            Wbd = A.alloc([4, D], BF16)
            bg16 = A.alloc([128], F32)
            bgT = A.alloc([16], F32)
            sigf = A.alloc([512], F32)
            sigd = A.alloc([512], F32)
            t1 = A.alloc([512], F32)
            t2 = A.alloc([512], F32)
            mixt = A.alloc([8, 512], BF16)
            ub_ = u_bufs()
            for q4 in range(4):
                DMA("gpsimd", Wg[:, q4 * 2:(q4 + 1) * 2, :], w_in_r[:, q4 * 2:(q4 + 1) * 2, 2892:4940], r=[],
                    w=[("Wg", q4)])
            WGK = [("Wg", q4) for q4 in range(4)]
            fold_gain(Wg, WGK)
            DMA("gpsimd", Wbf, w_bf.rearrange("(h p) c -> p h c", p=128), r=[], w=["Wbf"])
            DMA("gpsimd", Wbd, w_bd.rearrange("(h p) c -> p h c", p=128), r=[], w=["Wbd"])
            DMA("sync", bg16[0:16, :], b_gate.rearrange("b (kc p) -> (b kc) p", p=128), r=[], w=["bg16"])
            tb_, tbk = psf[6], "ps6"
            TR(tb_[:, 0:16], bg16[0:16, :], identf[0:16, 0:16], r=["bg16", "c_ident"], w=[tbk])
            CP("vector", bgT, tb_[:, 0:16], r=[tbk], w=["bgT"])
            for c in range(NCH):
                for j in range(4):
                    u_stage(4 * c + j, j, ub_)
                uT = ub_["uT"]
                uTk = [("uT", j) for j in range(4)]
                cs = slice(c * 512, (c + 1) * 512)
                for cc in range(8):
                    bA, kA = rot(7)
                    for kc in range(KC):
                        MM(bA[:], Wg[:, kc, cc * 128:(cc + 1) * 128], uT[:, kc, :], kc == 0, kc == KC - 1,
                           r=WGK + uTk, w=[kA])
                    bB, kB = rot(7)
                    for kc in range(KC):
                        MM(bB[:], Wg[:, kc, 1024 + cc * 128:1024 + (cc + 1) * 128], uT[:, kc, :], kc == 0,
                           kc == KC - 1, r=WGK + uTk, w=[kB])
                    bC, kCk = rot(7)
                    for h in range(4):
                        MM(bC[:], Wbf[:, h, cc * 128:(cc + 1) * 128], ofT[:, h, cs], h == 0, h == 3,
                           r=["Wbf", ("ofT", h, c)], w=[kCk])
                    bD, kDk = rot(7)
                    for h in range(4):
                        MM(bD[:], Wbd[:, h, cc * 128:(cc + 1) * 128], odT[:, h, cs], h == 0, h == 3,
                           r=["Wbd", ("odT", h, c)], w=[kDk])
                    ACT(sigf, bA[:], AF.Sigmoid, r=[kA, "bgT"], w=["sigf"], bias=bgT[:, cc:cc + 1])
                    ACT(sigd, bB[:], AF.Sigmoid, r=[kB, "bgT"], w=["sigd"], bias=bgT[:, 8 + cc:9 + cc])
                    TT("vector", t1, sigf, bC[:], ALU.mult, r=["sigf", kCk], w=["t1"])
                    TT("vector", t2, sigd, bD[:], ALU.mult, r=["sigd", kDk], w=["t2"])
                    TT("vector", mixt[:, cc, :], t1, t2, ALU.add, r=["t1", "t2"], w=[("mixt", cc)])
                CP("gpsimd", ofT[:, :, cs], mixt[:, 0:4, :], r=[("mixt", cc) for cc in range(4)],
                   w=[("ofT", h, c) for h in range(4)])
                CP("gpsimd", odT[:, :, cs], mixt[:, 4:8, :], r=[("mixt", cc) for cc in range(4, 8)],
                   w=[("odT", h, c) for h in range(4)])
            P.barrier()
            A.reset(persist_mark)
            if dbg in ("M", "Mi", "Mif", "Mid"):
                DMA("gpsimd", dbg_out[:, 0:4 * S], ofT.rearrange("p a b -> p (a b)"), r=[], w=["dbgF"])
                DMA("gpsimd", dbg_out[:, 4 * S:8 * S], odT.rearrange("p a b -> p (a b)"), r=[], w=["dbgD"])
                P.add("sync", None, r=["dbgF", "dbgD"])
                P.barrier()

        if "2" in passes:
            Wo = A.alloc([KC, D], BF16)
            gpost = A.alloc([D], F32)
            g3 = A.alloc([D], F32)
            g4 = A.alloc([D], F32)
            hbuf = A.alloc([4, D], F32)
            ffb = A.alloc([4, D], F32)
            tmpy = A.alloc([512], F32)
            vb = A.alloc([D], BF16)
            vT = A.alloc([KC, 512], BF16)
            WGU = [A.alloc([KC, 512], BF16) for _ in range(2)]
            WDp = [A.alloc([2, 512], BF16) for _ in range(4)]
            sgb = [WDp[2 + i_].rearrange("p a b -> p (a b)").bitcast(F32) for i_ in range(2)]
            wdk = [("WDp", 0), ("WDp", 1), ("sgb", 0), ("sgb", 1)]
            actT = A.alloc([NFT, 512], BF16)
            junk = A.alloc([D], BF16)
            ssy = A.alloc([4, 2], F32)
            ssh = A.alloc([4], F32)
            ssf = A.alloc([4, 2], F32)
            rs1 = A.alloc([4], F32)
            rs2 = A.alloc([4], F32)
            rs3 = A.alloc([4], F32)
            DMA("gpsimd", Wo, w_out.rearrange("(kc p) c -> p kc c", p=128), r=[], w=["Wo"])
            DMA("sync", gpost, g_post.partition_broadcast(128), r=[], w=["gpost"])
            DMA("sync", g3, g_fpre.partition_broadcast(128), r=[], w=["g3"])
            DMA("sync", g4, g_fpost.partition_broadcast(128), r=[], w=["g4"])
            w_fg_r = scr_g.rearrange("(kc p) f -> p kc f", p=128)
            w_fu_r = scr_u.rearrange("(kc p) f -> p kc f", p=128)
            w_fd_r = scr_d.rearrange("(ft p) c -> p ft c", p=128)
            npiece = NFT // 2
            wgu_n = 0
            wd_n = 0

            def mixT(kc, sl):
                return ofT[:, kc, sl] if kc < 4 else odT[:, kc - 4, sl]

            import os as _os
            _p2 = int(_os.environ.get("K_P2", "9"))
            def pre_b(j, banks):
                TT("gpsimd", rs1[:, j:j + 1], ssy[:, j, 0:1], ssy[:, j, 1:2], ALU.add,
                   r=[("ssy", j, 0), ("ssy", j, 1)], w=[("rs1", j)])
                TS("gpsimd", rs1[:, j:j + 1], rs1[:, j:j + 1], 1.0 / D, EPS, ALU.mult, ALU.add, r=[("rs1", j)],
                   w=[("rs1", j)])
                TT("gpsimd", rs1[:, j:j + 1], rs1[:, j:j + 1], nhalf, ALU.pow, r=[("rs1", j), "nhalf"],
                   w=[("rs1", j)])
                for hf in range(2):
                    bank, bk = banks[hf]
                    hs = slice(hf * 512, (hf + 1) * 512)
                    STT(tmpy, bank[:], rs1[:, j:j + 1], gpost[:, hs], ALU.mult, ALU.mult,
                        r=[bk, ("rs1", j), "gpost"], w=["tmpy"])
                    TT("vector", hbuf[:, j, hs], hbuf[:, j, hs], tmpy, ALU.add, r=["tmpy", ("h", j)],
                       w=[("h", j)])
                ACT(junk, hbuf[:, j, :], AF.Square, r=[("h", j)], w=[("ssh", j)], accum=ssh[:, j:j + 1])
                TS("gpsimd", rs2[:, j:j + 1], ssh[:, j:j + 1], 1.0 / D, EPS, ALU.mult, ALU.add, r=[("ssh", j)],
                   w=[("rs2", j)])
                TT("gpsimd", rs2[:, j:j + 1], rs2[:, j:j + 1], nhalf, ALU.pow, r=[("rs2", j), "nhalf"],
                   w=[("rs2", j)])
                STT(vb, hbuf[:, j, :], rs2[:, j:j + 1], g3, ALU.mult, ALU.mult,
                    r=[("h", j), ("rs2", j), "g3"], w=["vb"])
                for kc in range(KC):
                    TR(pst[:, kc * 128:(kc + 1) * 128], vb[:, kc * 128:(kc + 1) * 128], identb,
                       r=["vb", "c_identb"], w=["pst"])
                CP("vector", vT[:, :, j * 128:(j + 1) * 128], pst[:].rearrange("p (a b) -> p a b", a=KC, b=128),
                   r=["pst"], w=[("vT", j)])

            for c in range(NCH):
                ybanks = {}
                for j in range(4):
                    t = 4 * c + j
                    ts_ = slice(t * 128, (t + 1) * 128)
                    DMA("sync", hbuf[:, j, :], x[ts_, :], r=[], w=[("h", j)])
                    banks = []
                    for hf in range(2):
                        bank, bk = rot(7)
                        banks.append((bank, bk))
                        for kc in range(KC):
                            MM(bank[:], mixT(kc, ts_), Wo[:, kc, hf * 512:(hf + 1) * 512], kc == 0, kc == KC - 1,
                               r=["Wo"], w=[bk])
                        ACT(junk[:, 0:512], bank[:], AF.Square, r=[bk], w=[("ssy", j, hf)], accum=ssy[:, j, hf:hf + 1])
                    ybanks[j] = banks
                    if j >= 1:
                        pre_b(j - 1, ybanks[j - 1])
                pre_b(3, ybanks[3])
                vTk = [("vT", j) for j in range(4)]
                if _p2 < 1:
                    for j in range(4):
                        DMA("sync", out[(4 * c + j) * 128:(4 * c + j + 1) * 128, :], hbuf[:, j, :], r=[("h", j)], w=[("out", 4 * c + j)])
                    continue
                for p_ in range(npiece):
                    sl = wgu_n % 2
                    wgu_n += 1
                    f0 = p_ * 256
                    DMA("sync", WGU[sl][:, :, 0:256], w_fg_r[:, :, f0:f0 + 256], r=[], w=[("WGUg", sl)])
                    DMA("sync", WGU[sl][:, :, 256:512], w_fu_r[:, :, f0:f0 + 256], r=[], w=[("WGUu", sl)])
                    for f2 in range(2):
                        ft = 2 * p_ + f2
                        bG, kG = rot(7)
                        for kc in range(KC):
                            MM(bG[:], WGU[sl][:, kc, f2 * 128:(f2 + 1) * 128], vT[:, kc, :], kc == 0, kc == KC - 1,
                               r=[("WGUg", sl)] + vTk, w=[kG])
                        bU, kU = rot(7)
                        for kc in range(KC):
                            MM(bU[:], WGU[sl][:, kc, 256 + f2 * 128:256 + (f2 + 1) * 128], vT[:, kc, :], kc == 0,
                               kc == KC - 1, r=[("WGUu", sl)] + vTk, w=[kU])
                        ss_ = ft % 2
                        ACT(sgb[ss_], bG[:], AF.Silu, r=[kG], w=[("sgb", ss_)])
                        TT("vector", actT[:, ft, :], sgb[ss_], bU[:], ALU.mult, r=[("sgb", ss_), kU], w=[("actT", ft)])
                if _p2 < 2:
                    for j in range(4):
                        DMA("sync", out[(4 * c + j) * 128:(4 * c + j + 1) * 128, :], hbuf[:, j, :], r=[("h", j)], w=[("out", 4 * c + j)])
                    continue
                for hf in range(2):
                    hs = slice(hf * 512, (hf + 1) * 512)
                    accs = [(psf[3 + j], f"ps{3 + j}") for j in range(4)]
                    for p_ in range(npiece):
                        sl = wd_n % 4
                        wd_n += 1
                        DMA("sync", WDp[sl], w_fd_r[:, 2 * p_:2 * p_ + 2, hs], r=[], w=[wdk[sl]])
                        for f2 in range(2):
                            ft = 2 * p_ + f2
                            for j in range(4):
                                MM(accs[j][0][:], actT[:, ft, j * 128:(j + 1) * 128], WDp[sl][:, f2, :], ft == 0,
                                   ft == NFT - 1, r=[("actT", ft), wdk[sl]], w=[accs[j][1]])
                    for j in range(4):
                        ACT(junk[:, 0:512], accs[j][0][:], AF.Square, r=[accs[j][1]], w=[("ssf", j, hf)],
                            accum=ssf[:, j, hf:hf + 1])
                        CP("vector", ffb[:, j, hs], accs[j][0][:], r=[accs[j][1]], w=[("ffb", j)])
                for j in range(4):
                    t = 4 * c + j
                    TT("gpsimd", rs3[:, j:j + 1], ssf[:, j, 0:1], ssf[:, j, 1:2], ALU.add,
                       r=[("ssf", j, 0), ("ssf", j, 1)], w=[("rs3", j)])
                    TS("gpsimd", rs3[:, j:j + 1], rs3[:, j:j + 1], 1.0 / D, EPS, ALU.mult, ALU.add, r=[("rs3", j)],
                       w=[("rs3", j)])
                    TT("gpsimd", rs3[:, j:j + 1], rs3[:, j:j + 1], nhalf, ALU.pow, r=[("rs3", j), "nhalf"],
                       w=[("rs3", j)])
                    STT(ffb[:, j, :], ffb[:, j, :], rs3[:, j:j + 1], g4, ALU.mult, ALU.mult,
                        r=[("ffb", j), ("rs3", j), "g4"], w=[("ffb", j)])
                    TT("vector", ffb[:, j, :], ffb[:, j, :], hbuf[:, j, :], ALU.add, r=[("ffb", j), ("h", j)],
                       w=[("ffb", j)])
                    DMA("sync", out[t * 128:(t + 1) * 128, :], ffb[:, j, :], r=[("ffb", j)], w=[("out", t)])
            P.add("sync", None, r=[("out", t) for t in range(T)])
        P.barrier()
        print("arena peak bytes", A.peak * 2, "ops", {e: len(P.ops[e]) for e in ENGS})
        P.emit(block, sems)
    return nc


_CACHE = {}


def kernel(**inputs):
    S = 4096
    TOPK = 256
    B = 8
    x = np.asarray(inputs["x"], dtype=np.float32)
    consts = host_consts(S, TOPK)
    shared = {}
    for k in ("norm_mix_pre", "w_in", "b_forget", "b_gate", "w_branch_fox", "w_branch_dsa", "w_out",
              "norm_mix_post", "norm_ffn_pre", "w_ffn_gate", "w_ffn_up", "w_ffn_down", "norm_ffn_post"):
        shared[k] = np.ascontiguousarray(np.asarray(inputs[k], dtype=np.float32)[0])
    shared.update(consts)
    if "nc" not in _CACHE:
        _CACHE["nc"] = build(S, TOPK)
    nc = _CACHE["nc"]
    in_maps = []
    for b in range(B):
        m = dict(shared)
        m["x"] = np.ascontiguousarray(x[b])
        in_maps.append(m)
    res = run_bass_kernel_spmd(nc, in_maps, core_ids=list(range(B)))
    return np.stack([np.asarray(r["out"], dtype=np.float32) for r in res.results], axis=0)
```

```python
import numpy as np
from contextlib import ExitStack
import concourse.bass as bass
import concourse.mybir as mybir
from concourse.bass_utils import run_bass_kernel_spmd

F32 = mybir.dt.float32
BF16 = mybir.dt.bfloat16
ALU = mybir.AluOpType
AF = mybir.ActivationFunctionType
AX = mybir.AxisListType

ENGS = ("sync", "scalar", "tensor", "vector", "gpsimd")

D = 1024
KC = 8
DFF = 2816
NFT = DFF // 128
HD = 128
NIT = 12
ARENA_BYTES = 200 * 1024
EPS = 1e-6
NEG = -30000.0


class Prog:
    NDMA_SEM = 6

    def __init__(self):
        self.ops = {e: [] for e in ENGS}
        self.last_w = {}
        self.readers = {}
        self.last_compute = {e: None for e in ENGS}
        self.last_dmas = {e: [] for e in ENGS}

    def add(self, eng, fn, r=(), w=(), dma=False, extra=()):
        idx = len(self.ops[eng])
        deps = set(extra)
        px = [k for k in r if isinstance(k, str) and k.startswith("ps")]
        if px:
            r = [k for k in r if k not in px]
            w = list(w) + px
        for k in r:
            if k in self.last_w:
                deps.add(self.last_w[k])
        for k in w:
            if k in self.last_w:
                deps.add(self.last_w[k])
            for rd in self.readers.get(k, ()):
                deps.add(rd)
        me = (eng, idx)
        deps.discard(me)
        self.ops[eng].append(dict(fn=fn, deps=deps, dma=dma, signal=False, sig=None))
        for k in r:
            self.readers.setdefault(k, []).append(me)
        for k in w:
            self.last_w[k] = me
            self.readers[k] = []
        if fn is not None:
            if dma:
                self.last_dmas[eng] = (self.last_dmas[eng] + [me])[-self.NDMA_SEM:]
            else:
                self.last_compute[eng] = me
        return me

    def barrier(self):
        deps = []
        for e in ENGS:
            if self.last_compute[e] is not None:
                deps.append(self.last_compute[e])
            deps.extend(self.last_dmas[e])
        for e in ENGS:
            self.add(e, None, extra=deps)
        self.last_w = {}
        self.readers = {}

    def emit(self, block, sems):
        ops = self.ops
        for e in ENGS:
            for op in ops[e]:
                for (e2, j) in op["deps"]:
                    p = ops[e2][j]
                    if e2 == e and e == "tensor" and not p["dma"]:
                        continue
                    p["signal"] = True
        for e in ENGS:
            cnt = 0
            ndma = 0
            for op in ops[e]:
                if op["fn"] is None:
                    continue
                if op["dma"]:
                    s = sems["dma"][e][ndma % self.NDMA_SEM]
                    v = 16 * (ndma // self.NDMA_SEM + 1)
                    op["sig"] = (s, v)
                    op["prev"] = (s, v - 16)
                    ndma += 1
                elif op["signal"]:
                    cnt += 1
                    op["sig"] = (sems["cnt"][e], cnt)

        def make(e):
            def body(eng):
                waited = {}
                for op in ops[e]:
                    need = {}
                    for (e2, j) in op["deps"]:
                        p = ops[e2][j]
                        if p["sig"] is None:
                            continue
                        if e2 == e and e == "tensor" and not p["dma"]:
                            continue
                        s, v = p["sig"]
                        if need.get(id(s), (None, 0))[1] < v:
                            need[id(s)] = (s, v)
                    if op["dma"] and op["fn"] is not None:
                        s, v = op["prev"]
                        if v > 0 and need.get(id(s), (None, 0))[1] < v:
                            need[id(s)] = (s, v)
                    for sid, (s, v) in need.items():
                        if waited.get(sid, 0) < v:
                            eng.wait_ge(s, v)
                            waited[sid] = v
                    if op["fn"] is None:
                        continue
                    ins = op["fn"](eng)
                    if op["dma"]:
                        ins.then_inc(op["sig"][0], 16)
                    elif op["signal"]:
                        ins.then_inc(op["sig"][0], 1)
            return body

        block.sync(make("sync"))
        block.scalar(make("scalar"))
        block.tensor(make("tensor"))
        block.vector(make("vector"))
        block.gpsimd(make("gpsimd"))


class Arena:
    def __init__(self, t, nbytes):
        self.t = t
        self.cap = nbytes // 2
        self.off = 0
        self.peak = 0

    def alloc(self, shape, dtype):
        n = int(np.prod(shape))
        size = 4 if dtype == F32 else 2
        nel = n * size // 2
        nel = (nel + 15) // 16 * 16
        assert self.off + nel <= self.cap, f"arena overflow {self.off + nel} > {self.cap}"
        ap = self.t[:, self.off:self.off + n * size // 2]
        self.off += nel
        self.peak = max(self.peak, self.off)
        if dtype != BF16:
            ap = ap.bitcast(dtype)
        if len(shape) == 2:
            ap = ap.rearrange("p (a b) -> p a b", a=shape[0], b=shape[1])
        elif len(shape) == 3:
            ap = ap.rearrange("p (a b c) -> p a b c", a=shape[0], b=shape[1], c=shape[2])
        return ap

    def mark(self):
        return self.off

    def reset(self, m):
        self.off = m


def host_consts(S, TOPK):
    T = S // 128
    i = np.arange(128)
    c = {}
    c["ident"] = np.eye(128, dtype=np.float32)
    c["ident4"] = np.tile(np.eye(128, dtype=np.float32), (1, 4))
    c["cmT"] = np.where(i[:, None] > i[None, :], NEG, 0.0).astype(np.float32)
    c["cmQ"] = np.where(i[None, :] > i[:, None], NEG, 0.0).astype(np.float32)
    c["negm"] = np.where(i[None, :] > i[:, None], -1e30, 0.0).astype(np.float32)
    c["posm"] = np.where(i[None, :] > i[:, None], 1e30, 0.0).astype(np.float32)
    c["tri"] = (i[:, None] <= i[None, :]).astype(np.float32)
    c["ones"] = np.ones((128, 128), np.float32)
    sel0 = np.zeros((128, 128), np.float32)
    sel0[0, :] = 1.0
    c["sel0"] = sel0
    pos = np.arange(S, dtype=np.float32)

    def tab(rot):
        half = rot // 2
        inv = np.float32(500000.0) ** (-np.arange(half, dtype=np.float32) * np.float32(2.0) / np.float32(rot))
        ang = (pos[:, None] * inv[None, :]).astype(np.float32)
        cs = np.cos(ang).astype(np.float32).reshape(T, 128, half).transpose(1, 0, 2)
        sn = np.sin(ang).astype(np.float32).reshape(T, 128, half).transpose(1, 0, 2)
        return np.ascontiguousarray(cs), np.ascontiguousarray(sn)

    c["cosh"], c["sinh"] = tab(32)
    c["cosi"], c["sini"] = tab(16)
    c["cvec"] = np.tile((2.0 ** -(np.arange(NIT) + 1.0)).astype(np.float32)[None, :], (128, 1))
    return c


CONST_SHAPES = lambda S: {
    "ident": [128, 128], "ident4": [128, 512], "cmT": [128, 128], "cmQ": [128, 128],
    "negm": [128, 128], "posm": [128, 128], "tri": [128, 128], "ones": [128, 128],
    "sel0": [128, 128], "cosh": [128, S // 128, 16], "sinh": [128, S // 128, 16],
    "cosi": [128, S // 128, 8], "sini": [128, S // 128, 8], "cvec": [128, NIT],
}


def build(S, TOPK, passes="FDM2", dbg=None):
    T = S // 128
    NCH = S // 512
    KT0 = TOPK // 128
    nc = bass.Bass("TRN2", target_bir_lowering=False)
    dr = lambda n, s: nc.dram_tensor(n, s, F32, kind="ExternalInput").ap()
    x = dr("x", [S, D])
    g_pre = dr("norm_mix_pre", [D])
    w_in = dr("w_in", [D, 4940])
    b_forget = dr("b_forget", [4])
    b_gate = dr("b_gate", [2, D])
    w_bf = dr("w_branch_fox", [512, D])
    w_bd = dr("w_branch_dsa", [512, D])
    w_out = dr("w_out", [D, D])
    g_post = dr("norm_mix_post", [D])
    g_fpre = dr("norm_ffn_pre", [D])
    w_fg = dr("w_ffn_gate", [D, DFF])
    w_fu = dr("w_ffn_up", [D, DFF])
    w_fd = dr("w_ffn_down", [DFF, D])
    g_fpost = dr("norm_ffn_post", [D])
    cst = {k: dr(k, s) for k, s in CONST_SHAPES(S).items()}
    out = nc.dram_tensor("out", [S, D], F32, kind="ExternalOutput").ap()
    scr_g = nc.dram_tensor("scr_g", [D, DFF], BF16, kind="Internal").ap()
    scr_u = nc.dram_tensor("scr_u", [D, DFF], BF16, kind="Internal").ap()
    scr_d = nc.dram_tensor("scr_d", [DFF, D], BF16, kind="Internal").ap()
    dbg_out = nc.dram_tensor("dbg", [128, 8 * S], F32, kind="ExternalOutput").ap() if dbg else None
    dbg_in = nc.dram_tensor("dbg_in", [128, 8 * S], F32, kind="ExternalInput").ap() if dbg in ("Mi", "2i", "Mif", "Mid") else None

    P = Prog()
    scale = float(HD ** -0.5)
    idx_scale = float((64 ** -0.5) * (8 ** -0.5))

    with ExitStack() as es:
        arena_t = es.enter_context(nc.sbuf_tensor("arena", [128, ARENA_BYTES // 2], BF16))
        A = Arena(arena_t, ARENA_BYTES)
        psf = [es.enter_context(nc.psum_tensor(f"ps{i}", [128, 512], F32)) for i in range(7)]
        pst = es.enter_context(nc.psum_tensor("pst", [128, 1024], BF16))
        sems = {"cnt": {e: es.enter_context(nc.semaphore("c_" + e)) for e in ENGS},
                "dma": {e: [es.enter_context(nc.semaphore(f"d_{e}{i}")) for i in range(Prog.NDMA_SEM)]
                        for e in ("sync", "gpsimd")}}
        sems["dma"]["scalar"] = []
        block = es.enter_context(nc.Block())

        def MM(out_, lhsT, rhs, start, stop, r, w):
            P.add("tensor", lambda e: e.matmul(out=out_, lhsT=lhsT, rhs=rhs, start=start, stop=stop), r=r, w=w)

        def TR(out_, in_, idn, r, w):
            P.add("tensor", lambda e: e.transpose(out=out_, in_=in_, identity=idn), r=r, w=w)

        def ACT(out_, in_, func, r, w, bias=0.0, scale_=1.0, accum=None):
            if accum is None:
                P.add("scalar", lambda e: e.activation(out=out_, in_=in_, func=func, bias=bias, scale=scale_), r=r, w=w)
            else:
                P.add("scalar", lambda e: e.activation(out=out_, in_=in_, func=func, bias=bias, scale=scale_,
                                                       accum_out=accum), r=r, w=w)

        def TS(eng, out_, in0, s1, s2, op0, op1, r, w, accum=None):
            if accum is not None:
                P.add(eng, lambda e: e.tensor_scalar(out=out_, in0=in0, scalar1=s1, scalar2=s2, op0=op0, op1=op1,
                                                     accum_out=accum), r=r, w=w)
            elif op1 is None:
                P.add(eng, lambda e: e.tensor_scalar(out=out_, in0=in0, scalar1=s1, scalar2=None, op0=op0), r=r, w=w)
            else:
                P.add(eng, lambda e: e.tensor_scalar(out=out_, in0=in0, scalar1=s1, scalar2=s2, op0=op0, op1=op1),
                      r=r, w=w)

        def TT(eng, out_, in0, in1, op, r, w):
            P.add(eng, lambda e: e.tensor_tensor(out=out_, in0=in0, in1=in1, op=op), r=r, w=w)

        def STT(out_, in0, sc, in1, op0, op1, r, w):
            P.add("vector", lambda e: e.scalar_tensor_tensor(out=out_, in0=in0, scalar=sc, in1=in1, op0=op0, op1=op1),
                  r=r, w=w)

        def CP(eng, out_, in_, r, w):
            if eng == "scalar":
                P.add(eng, lambda e: e.copy(out=out_, in_=in_), r=r, w=w)
            else:
                P.add(eng, lambda e: e.tensor_copy(out=out_, in_=in_), r=r, w=w)

        def RCP(out_, in_, r, w):
            P.add("vector", lambda e: e.reciprocal(out=out_, in_=in_), r=r, w=w)

        def RED(out_, in_, op, r, w):
            P.add("vector", lambda e: e.tensor_reduce(out=out_, in_=in_, axis=AX.X, op=op), r=r, w=w)

        def MS(eng, ap, val, w):
            P.add(eng, lambda e: e.memset(ap, val), w=w)

        def DMA(eng, out_, in_, r, w):
            P.add(eng, lambda e: e.dma_start(out=out_, in_=in_), r=r, w=w, dma=True)

        rot_state = {"i": 0}

        def rot(nb=3):
            i = rot_state["i"] % nb
            rot_state["i"] += 1
            return psf[i], f"ps{i}"

        identb = A.alloc([128], BF16)
        ident4b = A.alloc([512], BF16)
        cmTb = A.alloc([128], BF16)
        cmQb = A.alloc([128], BF16)
        onesb = A.alloc([128], BF16)
        negm = A.alloc([128], F32)
        posm = A.alloc([128], F32)
        cosh = A.alloc([T, 16], F32)
        sinh = A.alloc([T, 16], F32)
        cosi = A.alloc([T, 8], F32)
        sini = A.alloc([T, 8], F32)
        cvec = A.alloc([NIT], F32)
        nhalf = A.alloc([1], F32)
        identf = A.alloc([128], F32)
        g8 = A.alloc([128], F32)
        gT = A.alloc([8], F32)
        odT = A.alloc([4, S], BF16)
        A.full_cap = A.cap
        ofT = arena_t[:, A.cap - 4 * S:A.cap].rearrange("p (a b) -> p a b", a=4, b=S)
        A.cap = A.full_cap - 4 * S
        for ap_, nm in ((identb, "ident"), (ident4b, "ident4"), (cmTb, "cmT"), (cmQb, "cmQ"), (onesb, "ones")):
            DMA("gpsimd", ap_, cst[nm], r=[], w=["c_" + nm + "b"])
        for ap_, nm in ((negm, "negm"), (posm, "posm"), (cosh, "cosh"), (sinh, "sinh"), (cosi, "cosi"), (sini, "sini"),
                        (cvec, "cvec")):
            DMA("sync", ap_, cst[nm], r=[], w=["c_" + nm])
        DMA("sync", identf, cst["ident"], r=[], w=["c_ident"])
        DMA("sync", g8[0:8, :], g_pre.rearrange("(kc p) -> kc p", p=128), r=[], w=["g8"])
        TR(psf[6][:, 0:8], g8[0:8, :], identf[0:8, 0:8], r=["g8", "c_ident"], w=["ps6"])
        CP("vector", gT, psf[6][:, 0:8], r=["ps6"], w=["gT"])
        MS("gpsimd", nhalf, -0.5, w=["nhalf"])
        persist_mark = A.mark()
        if "2" in passes:
            for hlf in range(2):
                DMA("gpsimd", scr_g[hlf * 512:(hlf + 1) * 512, :], w_fg[hlf * 512:(hlf + 1) * 512, :], r=[], w=[("scr_g", hlf)])
                DMA("gpsimd", scr_u[hlf * 512:(hlf + 1) * 512, :], w_fu[hlf * 512:(hlf + 1) * 512, :], r=[], w=[("scr_u", hlf)])
                DMA("gpsimd", scr_d[hlf * 1408:(hlf + 1) * 1408, :], w_fd[hlf * 1408:(hlf + 1) * 1408, :], r=[], w=[("scr_d", hlf)])
            if "D" not in passes and "F" not in passes and "M" not in passes:
                P.barrier()

        def u_stage(t, j, bufs, key=None):
            s = t % 2
            xs = t % len(bufs["xt"])
            xt, ssq, rstd, ub, uT = bufs["xt"][xs], bufs["ss"][s], bufs["rstd"][s], bufs["ub"][s], bufs["uT"]
            DMA("sync", xt, x[t * 128:(t + 1) * 128, :], r=[], w=[("xt", xs)])
            ACT(ub, xt, AF.Square, r=[("xt", xs)], w=[("ss", s), ("ub", s)], accum=ssq)
            TS("gpsimd", rstd, ssq, 1.0 / D, EPS, ALU.mult, ALU.add, r=[("ss", s)], w=[("rstd", s)])
            TT("gpsimd", rstd, rstd, nhalf, ALU.pow, r=[("rstd", s), "nhalf"], w=[("rstd", s)])
            ACT(ub, xt, AF.Copy, r=[("xt", xs), ("rstd", s)], w=[("ub", s)], scale_=rstd)
            for kc in range(KC):
                TR(pst[:, kc * 128:(kc + 1) * 128], ub[:, kc * 128:(kc + 1) * 128], identb,
                   r=[("ub", s), "c_identb"], w=["pst"])
            CP("scalar", uT[:, :, j * 128:(j + 1) * 128], pst[:].rearrange("p (a b) -> p a b", a=KC, b=128),
               r=["pst"], w=[("uT", j) if key is None else key + (j,)])

        def fold_gain(W, keys):
            for kc in range(KC):
                TS("vector", W[:, kc, :], W[:, kc, :], gT[:, kc:kc + 1], None, ALU.mult, None, r=list(keys) + ["gT"],
                   w=list(keys))

        def u_bufs(nx=2):
            return dict(xt=[A.alloc([D], F32) for _ in range(nx)], ss=[A.alloc([1], F32) for _ in range(2)],
                        rstd=[A.alloc([1], F32) for _ in range(2)], ub=[A.alloc([D], BF16) for _ in range(2)],
                        uT=A.alloc([KC, 512], BF16))

        w_in_r = w_in.rearrange("(kc p) c -> p kc c", p=128)

        if "D" in passes:
            A.cap = A.full_cap
            WD_ = A.alloc([KC, 1352], BF16)
            KdT = A.alloc([S], BF16)
            Vd = A.alloc([T, 128], BF16)
            kiT = A.alloc([S], BF16)
            QQ = [A.alloc([1024], BF16) for _ in range(3)]
            sgnD = [A.alloc([8, 128], BF16) for _ in range(3)]
            scs = [A.alloc([S], F32) for _ in range(2)]
            Mneg = [A.alloc([S], BF16) for _ in range(2)]
            R = [A.alloc([8, 512], BF16) for _ in range(1)]
            qd_f = A.alloc([4, 128], F32)
            qi_f = A.alloc([8, 64], F32)
            g4_f = A.alloc([328], F32)
            qd_b = A.alloc([4, 128], BF16)
            qi_b = A.alloc([8, 64], BF16)
            kk_b = A.alloc([256], BF16)
            rt = [A.alloc([8, 16], F32) for _ in range(4)]
            aw = A.alloc([8], F32)
            sg01 = A.alloc([8], F32)
            sgn = A.alloc([8], F32)
            tmpds = [A.alloc([128], F32) for _ in range(2)]
            sts = [A.alloc([8], F32) for _ in range(2)]
            Wc = A.alloc([NIT], F32)
            mids = A.alloc([NIT + 1], F32)
            cnts = A.alloc([NIT], F32)
            sgs = A.alloc([NIT], F32)
            pT = [A.alloc([512], BF16) for _ in range(2)]
            oS = [A.alloc([512], F32) for _ in range(2)]
            dS = [A.alloc([512], F32) for _ in range(2)]
            rden = A.alloc([512], F32)
            ub_ = u_bufs(2)
            for (d0, s0, n_) in ((0, 1540, 512), (512, 2308, 512), (1024, 2052, 256), (1280, 2820, 72)):
                DMA("gpsimd", WD_[:, :, d0:d0 + n_], w_in_r[:, :, s0:s0 + n_], r=[], w=[("WD", d0)])
            WDK = [("WD", 0), ("WD", 512), ("WD", 1024), ("WD", 1280)]
            fold_gain(WD_, WDK)

            def rope(src3, dst3, nh, half, cos_t, sin_t, key_src, key_dst):
                x1 = src3[:, :, 0:half]
                x2 = src3[:, :, half:2 * half]
                cb_ = cos_t.unsqueeze(1).broadcast_to([128, nh, half])
                sb_ = sin_t.unsqueeze(1).broadcast_to([128, nh, half])
                ta, tb, tc, td = [r_[:, 0:nh, 0:half] for r_ in rt]
                TT("gpsimd", ta, x1, cb_, ALU.mult, r=[key_src], w=["rt0"])
                TT("gpsimd", tb, x2, sb_, ALU.mult, r=[key_src], w=["rt1"])
                TT("gpsimd", tc, x2, cb_, ALU.mult, r=[key_src], w=["rt2"])
                TT("gpsimd", td, x1, sb_, ALU.mult, r=[key_src], w=["rt3"])
                TT("gpsimd", dst3[:, :, 0:half], ta, tb, ALU.subtract, r=["rt0", "rt1"], w=[key_dst])
                TT("gpsimd", dst3[:, :, half:2 * half], tc, td, ALU.add, r=["rt2", "rt3"], w=[key_dst])

            def prepP(t):
                c, j = t // 4, t % 4
                qs = t % 3
                if j == 0:
                    for jj in range(4):
                        u_stage(4 * c + jj, jj, ub_)
                uT = ub_["uT"]
                for (dst, c0, wn, key) in ((qd_f, 0, 512, "qd_f"), (qi_f, 512, 512, "qi_f"), (g4_f, 1024, 328, "g4_f")):
                    bank, bk = rot()
                    for kc in range(KC):
                        MM(bank[:, 0:wn], uT[:, kc, j * 128:(j + 1) * 128], WD_[:, kc, c0:c0 + wn], kc == 0,
                           kc == KC - 1, r=WDK + [("uT", j)], w=[bk])
                    dflat = dst if key == "g4_f" else dst.rearrange("p a b -> p (a b)")
                    CP("scalar", dflat, bank[:, 0:wn], r=[bk], w=[key])
                rope(qd_f, qd_b, 4, 16, cosh[:, t, :], sinh[:, t, :], "qd_f", "qd_b")
                CP("gpsimd", qd_b[:, :, 32:128], qd_f[:, :, 32:128], r=["qd_f"], w=["qd_b"])
                TS("gpsimd", sg01, g4_f[:, 320:328], 0.0, None, ALU.is_ge, None, r=["g4_f"], w=["sg01"])
                TS("gpsimd", sgn, sg01, 2.0, -1.0, ALU.mult, ALU.add, r=["sg01"], w=["sgn"])
                TS("gpsimd", aw, sg01, 2.0 * idx_scale, -idx_scale, ALU.mult, ALU.add, r=["sg01"], w=["aw"])
                TT("gpsimd", aw, aw, g4_f[:, 320:328], ALU.mult, r=["aw", "g4_f"], w=["aw"])
                rope(qi_f, qi_f, 8, 8, cosi[:, t, :], sini[:, t, :], "qi_f", "qi_f")
                TT("gpsimd", qi_b, qi_f, aw.unsqueeze(2).broadcast_to([128, 8, 64]), ALU.mult, r=["qi_f", "aw"],
                   w=["qi_b"])
                kd3 = g4_f[:, 0:128].rearrange("p (a b) -> p a b", a=1, b=128)
                kdb3 = kk_b[:, 0:128].rearrange("p (a b) -> p a b", a=1, b=128)
                rope(kd3, kdb3, 1, 16, cosh[:, t, :], sinh[:, t, :], "g4_f", "kk_b")
                CP("gpsimd", kk_b[:, 32:128], g4_f[:, 32:128], r=["g4_f"], w=["kk_b"])
                ki3 = g4_f[:, 256:320].rearrange("p (a b) -> p a b", a=1, b=64)
                kib3 = kk_b[:, 128:192].rearrange("p (a b) -> p a b", a=1, b=64)
                rope(ki3, kib3, 1, 8, cosi[:, t, :], sini[:, t, :], "g4_f", "kk_b")
                CP("gpsimd", kk_b[:, 144:192], g4_f[:, 272:320], r=["g4_f"], w=["kk_b"])
                CP("gpsimd", kk_b[:, 192:256], kk_b[:, 128:192], r=["kk_b"], w=["kk_b2"])
                CP("gpsimd", Vd[:, t, :], g4_f[:, 128:256], r=["g4_f"], w=[("Vd", t)])
                TT("gpsimd", sgnD[qs], identb.unsqueeze(1).broadcast_to([128, 8, 128]),
                   sgn.unsqueeze(2).broadcast_to([128, 8, 128]), ALU.mult, r=["c_identb", "sgn"], w=[("sgnD", qs)])

            def prepT(t):
                qs = t % 3
                for i_ in range(2):
                    TR(pst[:, i_ * 128:(i_ + 1) * 128], kk_b[:, i_ * 128:(i_ + 1) * 128], identb,
                       r=["kk_b", "kk_b2", "c_identb"], w=["pst"])
                CP("scalar", KdT[:, t * 128:(t + 1) * 128], pst[:, 0:128], r=["pst"], w=[("KdT", t)])
                CP("scalar", kiT[:, t * 128:(t + 1) * 128], pst[:, 128:256], r=["pst"], w=[("kiT", t)])
                qdb2 = qd_b.rearrange("p a b -> p (a b)")
                qib2 = qi_b.rearrange("p a b -> p (a b)")
                for i_ in range(4):
                    TR(pst[:, i_ * 128:(i_ + 1) * 128], qdb2[:, i_ * 128:(i_ + 1) * 128], identb,
                       r=["qd_b", "c_identb"], w=["pst"])
                for i_ in range(4):
                    TR(pst[:, 512 + i_ * 128:512 + (i_ + 1) * 128], qib2[:, i_ * 128:(i_ + 1) * 128], identb,
                       r=["qi_b", "c_identb"], w=["pst"])
                CP("scalar", QQ[qs], pst[:], r=["pst"], w=[("QQ", qs)])

            def stageA(t):
                q3 = t % 3
                qs = t % 2
                n = (t + 1) * 128
                sc = scs[qs]
                tmpd = tmpds[qs]
                if t < KT0:
                    return
                nk5 = (n + 511) // 512
                for k5 in range(nk5):
                    c0 = k5 * 512
                    wn = min(512, n - c0)
                    rs = 0
                    kik = [("kiT", tt) for tt in range(c0 // 128, (c0 + wn) // 128)]
                    for hd in range(8):
                        hp, hf = hd // 2, hd % 2
                        bank, bk = rot()
                        MM(bank[:, 0:wn], QQ[q3][64 * hf:64 * hf + 64, 512 + hp * 128:512 + (hp + 1) * 128],
                           kiT[64 * hf:64 * hf + 64, c0:c0 + wn], True, True, r=[("QQ", q3)] + kik, w=[bk])
                        ACT(R[rs][:, hd, 0:wn], bank[:, 0:wn], AF.Relu, r=[bk], w=[("R", rs, hd)])
                    sb_, sbk = psf[3], "ps3"
                    for hd in range(8):
                        MM(sb_[:, 0:wn], sgnD[q3][:, hd, :], R[rs][:, hd, 0:wn], hd == 0, hd == 7,
                           r=[("sgnD", q3), ("R", rs, hd)], w=[sbk])
                    CP("scalar", sc[:, c0:c0 + wn], sb_[:, 0:wn], r=[sbk], w=[("sc", qs, k5)])
                sck = [("sc", qs, k5) for k5 in range(nk5)]
                TT("gpsimd", tmpd, sc[:, n - 128:n], posm, ALU.add, r=sck + ["c_posm"], w=[("tmpd", qs)])
                TT("gpsimd", sc[:, n - 128:n], sc[:, n - 128:n], negm, ALU.add, r=sck + ["c_negm", ("tmpd", qs)],
                   w=[("sc", qs, nk5 - 1)])

            def stageB(t):
                qs = t % 2
                ms_ = qs
                n = (t + 1) * 128
                sc = scs[qs]
                tmpd = tmpds[qs]
                st = sts[qs]
                if t < KT0:
                    if n > 128:
                        MS("gpsimd", Mneg[ms_][:, 0:n - 128], 0.0, w=[("Mneg", ms_)])
                    CP("gpsimd", Mneg[ms_][:, n - 128:n], cmQb, r=["c_cmQb"], w=[("Mneg", ms_)])
                    return
                nk5 = (n + 511) // 512
                sck = [("sc", qs, k5) for k5 in range(nk5)]
                RED(st[:, 0:1], sc[:, 0:n], ALU.max, r=sck, w=[("hi", qs)])
                RED(st[:, 1:2], tmpd, ALU.min, r=[("tmpd", qs)], w=[("m1", qs)])
                RED(st[:, 2:3], sc[:, 0:n - 128], ALU.min, r=sck, w=[("m2", qs)])
                TT("vector", st[:, 3:4], st[:, 1:2], st[:, 2:3], ALU.min, r=[("m1", qs), ("m2", qs)], w=[("lo", qs)])
                TT("vector", st[:, 4:5], st[:, 0:1], st[:, 3:4], ALU.subtract, r=[("hi", qs), ("lo", qs)],
                   w=[("Wd", qs)])
                TS("vector", Wc, cvec, st[:, 4:5], None, ALU.mult, None, r=["c_cvec", ("Wd", qs)], w=["Wc"])
                TT("vector", mids[:, 0:1], st[:, 3:4], Wc[:, 0:1], ALU.add, r=[("lo", qs), "Wc"], w=[("mid", 0)])
                for it in range(NIT):
                    TS("vector", Mneg[ms_][:, 0:n], sc[:, 0:n], mids[:, it:it + 1], None, ALU.is_ge, ALU.add,
                       r=sck + [("mid", it)], w=[("cnt", it), ("Mneg", ms_)], accum=cnts[:, it:it + 1])
                    TS("vector", sgs[:, it:it + 1], cnts[:, it:it + 1], TOPK - 0.5, 0.5, ALU.is_ge, ALU.subtract,
                       r=[("cnt", it)], w=[("sg", it)])
                    STT(mids[:, it + 1:it + 2], sgs[:, it:it + 1], Wc[:, it:it + 1], mids[:, it:it + 1], ALU.mult,
                        ALU.add, r=[("sg", it), "Wc", ("mid", it)], w=[("mid", it + 1)])
                STT(st[:, 5:6], st[:, 4:5], -(2.0 ** -(NIT + 1)), mids[:, NIT:NIT + 1], ALU.mult, ALU.add,
                    r=[("Wd", qs), ("mid", NIT)], w=[("tau", qs)])
                TS("vector", Mneg[ms_][:, 0:n], sc[:, 0:n], st[:, 5:6], NEG, ALU.is_lt, ALU.mult,
                   r=sck + [("tau", qs)], w=[("Mneg", ms_)])

            def stageC(t):
                qs = t % 2
                q3 = t % 3
                ms_ = qs
                ob, obk, db, dbk = psf[4], "ps4", psf[5], "ps5"
                def qk(kt):
                    bank, bk = rot()
                    MM(bank[:], KdT[:, kt * 128:(kt + 1) * 128], QQ[q3][:, 0:512], True, False,
                       r=[("KdT", kt), ("QQ", q3)], w=[bk])
                    MM(bank[:], Mneg[ms_][:, kt * 128:(kt + 1) * 128], ident4b, False, True,
                       r=[("Mneg", ms_), "c_ident4b"], w=[bk])
                    return bank, bk
                cur = qk(0)
                for kt in range(t + 1):
                    nxt = qk(kt + 1) if kt + 1 <= t else None
                    bank, bk = cur
                    ps_ = kt % 2
                    ACT(pT[ps_], bank[:], AF.Exp, r=[bk], w=[("pT", ps_)], scale_=scale)
                    MM(ob[:], Vd[:, kt, :], pT[ps_], kt == 0, kt == t, r=[("Vd", kt), ("pT", ps_)], w=[obk])
                    MM(db[:], onesb, pT[ps_], kt == 0, kt == t, r=["c_onesb", ("pT", ps_)], w=[dbk])
                    cur = nxt
                CP("scalar", oS[qs], ob[:], r=[obk], w=[("oS", qs)])
                CP("scalar", dS[qs], db[:], r=[dbk], w=[("dS", qs)])

            def stageN(t):
                qs = t % 2
                RCP(rden, dS[qs], r=[("dS", qs)], w=["rden"])
                TT("vector", odT[:, :, t * 128:(t + 1) * 128], oS[qs].rearrange("p (a b) -> p a b", a=4, b=128),
                   rden.rearrange("p (a b) -> p a b", a=4, b=128), ALU.mult, r=[("oS", qs), "rden"], w=[("odT", t)])

            prepP(0)
            prepT(0)
            stageA(0)
            if T > 1:
                prepP(1)
                prepT(1)
            for t in range(T):
                if t + 2 < T:
                    prepP(t + 2)
                if t + 1 < T:
                    stageA(t + 1)
                stageB(t)
                if t >= 1:
                    stageN(t - 1)
                if t + 2 < T:
                    prepT(t + 2)
                stageC(t)
            stageN(T - 1)
            P.barrier()
            A.reset(persist_mark)
            A.cap = A.full_cap - 4 * S
            if dbg in ("D", "Y"):
                if dbg == "D":
                    DMA("gpsimd", dbg_out[:, 4 * S:8 * S], odT.rearrange("p a b -> p (a b)"), r=[], w=["dbgD"])
                if dbg == "Y":
                    DMA("gpsimd", dbg_out[:, 0:4 * S], ofT.rearrange("p a b -> p (a b)"), r=[], w=["dbgD"])
                P.add("sync", None, r=["dbgD"])
                P.barrier()

        if "F" in passes:
            tri = A.alloc([128], F32)
            onesf = A.alloc([128], F32)
            sel0 = A.alloc([128], F32)
            for ap_, nm in ((tri, "tri"), (onesf, "ones"), (sel0, "sel0")):
                DMA("sync", ap_, cst[nm], r=[], w=["c_" + nm])
            WF = A.alloc([KC, 1540], BF16)
            KfT = A.alloc([4, S], BF16)
            Vf = A.alloc([T, 512], BF16)
            negc = A.alloc([T, 4], F32)
            biascs = [A.alloc([T, 4], F32) for _ in range(2)]
            bfb = A.alloc([4, 4], F32)
            carry = A.alloc([4], F32)
            refb = A.alloc([4], F32)
            v3 = lambda a_: a_.rearrange("p (a b) -> p a b", a=4, b=4)
            ztf = A.alloc([16], F32)
            etf = A.alloc([16], F32)
            ltf = A.alloc([16], F32)
            Lsf = A.alloc([16], F32)
            zt, et, lt, Ls = v3(ztf), v3(etf), v3(ltf), v3(Lsf)
            ltot = A.alloc([4], F32)
            QfTs = [A.alloc([4, 512], BF16) for _ in range(2)]
            pT = [A.alloc([512], BF16) for _ in range(2)]
            rden = A.alloc([512], F32)
            ub_ = u_bufs()
            for half in range(2):
                DMA("gpsimd", WF[:, half * 4:(half + 1) * 4, :], w_in_r[:, half * 4:(half + 1) * 4, 0:1540],
                    r=[], w=[("WF", half)])
            WFK = [("WF", 0), ("WF", 1)]
            fold_gain(WF, WFK)
            for jj in range(4):
                DMA("sync", bfb[:, jj, :], b_forget.partition_broadcast(128), r=[], w=[("bfb", jj)])
            MS("gpsimd", carry, 0.0, w=["carry"])
            MS("gpsimd", Ls[:, 0, :], 0.0, w=["Ls0"])
            uT = ub_["uT"]
            uTk = [("uT", j) for j in range(4)]
            fb2 = psf[2]

            def prepA(c):
                for j in range(4):
                    u_stage(4 * c + j, j, ub_)

            def prepB1(c):
                qsl = c % 2
                for g in range(8):
                    bank, bk = rot(2)
                    for kc in range(KC):
                        MM(bank[:], WF[:, kc, g * 128:(g + 1) * 128], uT[:, kc, :], kc == 0, kc == KC - 1,
                           r=WFK + uTk, w=[bk])
                    if g < 4:
                        CP("vector", QfTs[qsl][:, g, :], bank[:], r=[bk], w=[("QfT", qsl, g)])
                    else:
                        CP("vector", KfT[:, g - 4, c * 512:(c + 1) * 512], bank[:], r=[bk], w=[("KfT", g - 4, c)])

            def prepB2(c):
                bsl = c % 2
                biasc = biascs[bsl]
                for j in range(4):
                    t = 4 * c + j
                    bank, bk = rot(2)
                    for kc in range(KC):
                        MM(bank[:], uT[:, kc, j * 128:(j + 1) * 128], WF[:, kc, 1024:1536], kc == 0, kc == KC - 1,
                           r=WFK + [("uT", j)], w=[bk])
                    CP("vector", Vf[:, t, :], bank[:], r=[bk], w=[("Vf", t)])
                    for kc in range(KC):
                        MM(fb2[:, j * 4:(j + 1) * 4], uT[:, kc, j * 128:(j + 1) * 128], WF[:, kc, 1536:1540],
                           kc == 0, kc == KC - 1, r=WFK + [("uT", j)], w=["ps2"])
                TT("vector", zt, fb2[:, 0:16].rearrange("p (a b) -> p a b", a=4, b=4), bfb, ALU.add,
                   r=["ps2"] + [("bfb", jj) for jj in range(4)], w=["zt"])
                ACT(et, zt, AF.Exp, r=["zt"], w=["et"], scale_=-1.0)
                ACT(lt, et, AF.Ln, r=["et"], w=["lt"], bias=1.0)
                CP("gpsimd", Ls[:, 1, :], lt[:, 0, :], r=["lt"], w=["Ls1"])
                TT("gpsimd", Ls[:, 2, :], Ls[:, 1, :], lt[:, 1, :], ALU.add, r=["lt", "Ls1"], w=["Ls2"])
                TT("gpsimd", Ls[:, 3, :], Ls[:, 2, :], lt[:, 2, :], ALU.add, r=["lt", "Ls2"], w=["Ls3"])
                TT("gpsimd", ltot, Ls[:, 3, :], lt[:, 3, :], ALU.add, r=["lt", "Ls3"], w=["ltot"])
                MM(fb2[:, 32:48], tri, ltf, True, False, r=["c_tri", "lt"], w=["ps2"])
                MM(fb2[:, 32:48], onesf, Lsf, False, True, r=["c_ones", "Ls0", "Ls1", "Ls2", "Ls3"], w=["ps2"])
                MM(fb2[:, 48:52], onesf, ltot, True, True, r=["c_ones", "ltot"], w=["ps2"])
                TT("vector", negc[:, 4 * c:4 * c + 4, :], fb2[:, 32:48].rearrange("p (a b) -> p a b", a=4, b=4),
                   carry.unsqueeze(1).broadcast_to([128, 4, 4]), ALU.add, r=["ps2", "carry"], w=[("negc", c)])
                TT("vector", carry, fb2[:, 48:52], carry, ALU.add, r=["ps2", "carry"], w=["carry"])
                MM(fb2[:, 64:68], sel0, negc[:, 4 * c + 2, :], True, True, r=["c_sel0", ("negc", c)], w=["ps2"])
                CP("vector", refb, fb2[:, 64:68], r=["ps2"], w=["refb"])
                nkt = 4 * c + 4
                for h in range(4):
                    TS("vector", biasc[:, 0:nkt, h], negc[:, 0:nkt, h], refb[:, h:h + 1], None, ALU.subtract, None,
                       r=[("negc", cc) for cc in range(c + 1)] + ["refb"], w=[("biasc", bsl, h)])

            ostate = {"o": 0}

            def attn(c, h):
                qsl = c % 2
                bsl = c % 2
                QfT = QfTs[qsl]
                biasc = biascs[bsl]
                nkt = 4 * c + 4
                ob, obk, db, dbk = (psf[3], "ps3", psf[4], "ps4") if ostate["o"] == 0 else (psf[5], "ps5", psf[6], "ps6")
                ostate["o"] ^= 1

                def qkf(kt):
                    off = max(0, kt - 4 * c) * 128
                    diag = kt >= 4 * c
                    bank, bk = rot(2)
                    MM(bank[:, off:512], KfT[:, h, kt * 128:(kt + 1) * 128], QfT[:, h, off:512], True, not diag,
                       r=[("KfT", h, kt // 4), ("QfT", qsl, h)], w=[bk])
                    if diag:
                        MM(bank[:, off:off + 128], identb, cmTb, False, True, r=["c_identb", "c_cmTb"], w=[bk])
                    return bank, bk, off
                cur = qkf(0)
                for kt in range(nkt):
                    nxt = qkf(kt + 1) if kt + 1 < nkt else None
                    bank, bk, off = cur
                    ps_ = kt % 2
                    ACT(pT[ps_][:, off:512], bank[:, off:512], AF.Exp, r=[bk, ("biasc", bsl, h)], w=[("pT", ps_)],
                        bias=biasc[:, kt, h:h + 1], scale_=scale)
                    MM(ob[:, off:512], Vf[:, kt, h * 128:(h + 1) * 128], pT[ps_][:, off:512], kt == 0,
                       kt == nkt - 1, r=[("Vf", kt), ("pT", ps_)], w=[obk])
                    MM(db[:, off:512], onesb, pT[ps_][:, off:512], kt == 0, kt == nkt - 1,
                       r=["c_onesb", ("pT", ps_)], w=[dbk])
                    cur = nxt
                RCP(rden, db[:], r=[dbk], w=["rden"])
                TT("vector", ofT[:, h, c * 512:(c + 1) * 512], ob[:], rden, ALU.mult, r=[obk, "rden"],
                   w=[("ofT", h, c)])

            prepA(0)
            prepB1(0)
            prepB2(0)
            for c in range(NCH):
                attn(c, 0)
                if c + 1 < NCH:
                    prepA(c + 1)
                attn(c, 1)
                if c + 1 < NCH:
                    prepB1(c + 1)
                attn(c, 2)
                if c + 1 < NCH:
                    prepB2(c + 1)
                attn(c, 3)
            P.barrier()
            A.reset(persist_mark)
            if dbg == "F":
                DMA("gpsimd", dbg_out[:, 0:4 * S], ofT.rearrange("p a b -> p (a b)"), r=[], w=["dbgF"])
                P.add("sync", None, r=["dbgF"])
                P.barrier()

        if dbg in ("Mi", "2i", "Mif", "Mid"):
            if dbg != "Mif":
                DMA("gpsimd", ofT.rearrange("p a b -> p (a b)"), dbg_in[:, 0:4 * S], r=[], w=["dbgi"])
            if dbg != "Mid":
                DMA("gpsimd", odT.rearrange("p a b -> p (a b)"), dbg_in[:, 4 * S:8 * S], r=[], w=["dbgi2"])
            P.barrier()
        if "M" in passes:
            Wg = A.alloc([KC, 2048], BF16)
            Wbf = A.alloc([4, D], BF16)
            Wbd = A.alloc([4, D], BF16)
            bg16 = A.alloc([128], F32)
            bgT = A.alloc([16], F32)
            sigf = A.alloc([512], F32)
            sigd = A.alloc([512], F32)
            t1 = A.alloc([512], F32)
            t2 = A.alloc([512], F32)
            mixt = A.alloc([8, 512], BF16)
            ub_ = u_bufs()
            for q4 in range(4):
                DMA("gpsimd", Wg[:, q4 * 2:(q4 + 1) * 2, :], w_in_r[:, q4 * 2:(q4 + 1) * 2, 2892:4940], r=[],
                    w=[("Wg", q4)])
            WGK = [("Wg", q4) for q4 in range(4)]
            fold_gain(Wg, WGK)
            DMA("gpsimd", Wbf, w_bf.rearrange("(h p) c -> p h c", p=128), r=[], w=["Wbf"])
            DMA("gpsimd", Wbd, w_bd.rearrange("(h p) c -> p h c", p=128), r=[], w=["Wbd"])
            DMA("sync", bg16[0:16, :], b_gate.rearrange("b (kc p) -> (b kc) p", p=128), r=[], w=["bg16"])
            tb_, tbk = psf[6], "ps6"
            TR(tb_[:, 0:16], bg16[0:16, :], identf[0:16, 0:16], r=["bg16", "c_ident"], w=[tbk])
            CP("vector", bgT, tb_[:, 0:16], r=[tbk], w=["bgT"])
            uT2 = [ub_["uT"], A.alloc([KC, 512], BF16)]

            def uprep(c):
                ub_["uT"] = uT2[c % 2]
                for j in range(4):
                    u_stage(4 * c + j, j, ub_, key=("uT", c % 2))
            uprep(0)
            for c in range(NCH):
                if c + 1 < NCH:
                    uprep(c + 1)
                uT = uT2[c % 2]
                uTk = [("uT", c % 2, j) for j in range(4)]
                cs = slice(c * 512, (c + 1) * 512)
                for cc in range(8):
                    bA, kA = rot(7)
                    for kc in range(KC):
                        MM(bA[:], Wg[:, kc, cc * 128:(cc + 1) * 128], uT[:, kc, :], kc == 0, kc == KC - 1,
                           r=WGK + uTk, w=[kA])
                    bB, kB = rot(7)
                    for kc in range(KC):
                        MM(bB[:], Wg[:, kc, 1024 + cc * 128:1024 + (cc + 1) * 128], uT[:, kc, :], kc == 0,
                           kc == KC - 1, r=WGK + uTk, w=[kB])
                    bC, kCk = rot(7)
                    for h in range(4):
                        MM(bC[:], Wbf[:, h, cc * 128:(cc + 1) * 128], ofT[:, h, cs], h == 0, h == 3,
                           r=["Wbf", ("ofT", h, c)], w=[kCk])
                    bD, kDk = rot(7)
                    for h in range(4):
                        MM(bD[:], Wbd[:, h, cc * 128:(cc + 1) * 128], odT[:, h, cs], h == 0, h == 3,
                           r=["Wbd", ("odT", h, c)], w=[kDk])
                    ACT(sigf, bA[:], AF.Sigmoid, r=[kA, "bgT"], w=["sigf"], bias=bgT[:, cc:cc + 1])
                    ACT(sigd, bB[:], AF.Sigmoid, r=[kB, "bgT"], w=["sigd"], bias=bgT[:, 8 + cc:9 + cc])
                    TT("vector", t1, sigf, bC[:], ALU.mult, r=["sigf", kCk], w=["t1"])
                    TT("vector", t2, sigd, bD[:], ALU.mult, r=["sigd", kDk], w=["t2"])
                    TT("vector", mixt[:, cc, :], t1, t2, ALU.add, r=["t1", "t2"], w=[("mixt", cc)])
                CP("vector", ofT[:, :, cs], mixt[:, 0:4, :], r=[("mixt", cc) for cc in range(4)],
                   w=[("ofT", h, c) for h in range(4)])
                CP("vector", odT[:, :, cs], mixt[:, 4:8, :], r=[("mixt", cc) for cc in range(4, 8)],
                   w=[("odT", h, c) for h in range(4)])
            P.barrier()
            A.reset(persist_mark)
            if dbg in ("M", "Mi", "Mif", "Mid"):
                DMA("gpsimd", dbg_out[:, 0:4 * S], ofT.rearrange("p a b -> p (a b)"), r=[], w=["dbgF"])
                DMA("gpsimd", dbg_out[:, 4 * S:8 * S], odT.rearrange("p a b -> p (a b)"), r=[], w=["dbgD"])
                P.add("sync", None, r=["dbgF", "dbgD"])
                P.barrier()

        if "2" in passes:
            Wo = A.alloc([KC, D], BF16)
            gpost = A.alloc([D], F32)
            g3 = A.alloc([D], F32)
            g4 = A.alloc([D], F32)
            hbuf = A.alloc([4, D], F32)
            ffb = A.alloc([4, D], F32)
            tmpy = A.alloc([512], F32)
            vb = A.alloc([D], BF16)
            vT = A.alloc([KC, 512], BF16)
            WGU = [A.alloc([KC, 512], BF16) for _ in range(2)]
            WDp = [A.alloc([2, 512], BF16) for _ in range(4)]
            sgb = [WDp[2 + i_].rearrange("p a b -> p (a b)").bitcast(F32) for i_ in range(2)]
            wdk = [("WDp", 0), ("WDp", 1), ("sgb", 0), ("sgb", 1)]
            actT = A.alloc([NFT, 512], BF16)
            junk = A.alloc([D], BF16)
            ssy = A.alloc([4, 2], F32)
            ssh = A.alloc([4], F32)
            ssf = A.alloc([4, 2], F32)
            rs1 = A.alloc([4], F32)
            rs2 = A.alloc([4], F32)
            rs3 = A.alloc([4], F32)
            DMA("gpsimd", Wo, w_out.rearrange("(kc p) c -> p kc c", p=128), r=[], w=["Wo"])
            DMA("sync", gpost, g_post.partition_broadcast(128), r=[], w=["gpost"])
            DMA("sync", g3, g_fpre.partition_broadcast(128), r=[], w=["g3"])
            DMA("sync", g4, g_fpost.partition_broadcast(128), r=[], w=["g4"])
            w_fg_r = scr_g.rearrange("(kc p) f -> p kc f", p=128)
            w_fu_r = scr_u.rearrange("(kc p) f -> p kc f", p=128)
            w_fd_r = scr_d.rearrange("(ft p) c -> p ft c", p=128)
            npiece = NFT // 2
            wgu_n = 0
            wd_n = 0

            def mixT(kc, sl):
                return ofT[:, kc, sl] if kc < 4 else odT[:, kc - 4, sl]

            import os as _os
            _p2 = int(_os.environ.get("K_P2", "9"))
            def pre_b(j, banks):
                TT("gpsimd", rs1[:, j:j + 1], ssy[:, j, 0:1], ssy[:, j, 1:2], ALU.add,
                   r=[("ssy", j, 0), ("ssy", j, 1)], w=[("rs1", j)])
                TS("gpsimd", rs1[:, j:j + 1], rs1[:, j:j + 1], 1.0 / D, EPS, ALU.mult, ALU.add, r=[("rs1", j)],
                   w=[("rs1", j)])
                TT("gpsimd", rs1[:, j:j + 1], rs1[:, j:j + 1], nhalf, ALU.pow, r=[("rs1", j), "nhalf"],
                   w=[("rs1", j)])
                for hf in range(2):
                    bank, bk = banks[hf]
                    hs = slice(hf * 512, (hf + 1) * 512)
                    STT(tmpy, bank[:], rs1[:, j:j + 1], gpost[:, hs], ALU.mult, ALU.mult,
                        r=[bk, ("rs1", j), "gpost"], w=["tmpy"])
                    TT("vector", hbuf[:, j, hs], hbuf[:, j, hs], tmpy, ALU.add, r=["tmpy", ("h", j)],
                       w=[("h", j)])
                ACT(junk, hbuf[:, j, :], AF.Square, r=[("h", j)], w=[("ssh", j), "junk2"], accum=ssh[:, j:j + 1])
                TS("gpsimd", rs2[:, j:j + 1], ssh[:, j:j + 1], 1.0 / D, EPS, ALU.mult, ALU.add, r=[("ssh", j)],
                   w=[("rs2", j)])
                TT("gpsimd", rs2[:, j:j + 1], rs2[:, j:j + 1], nhalf, ALU.pow, r=[("rs2", j), "nhalf"],
                   w=[("rs2", j)])
                STT(vb, hbuf[:, j, :], rs2[:, j:j + 1], g3, ALU.mult, ALU.mult,
                    r=[("h", j), ("rs2", j), "g3"], w=["vb"])
                for kc in range(KC):
                    TR(pst[:, kc * 128:(kc + 1) * 128], vb[:, kc * 128:(kc + 1) * 128], identb,
                       r=["vb", "c_identb"], w=["pst"])
                CP("vector", vT[:, :, j * 128:(j + 1) * 128], pst[:].rearrange("p (a b) -> p a b", a=KC, b=128),
                   r=["pst"], w=[("vT", j)])

            for c in range(NCH):
                ybanks = {}
                for j in range(4):
                    t = 4 * c + j
                    ts_ = slice(t * 128, (t + 1) * 128)
                    DMA("sync", hbuf[:, j, :], x[ts_, :], r=[], w=[("h", j)])
                    banks = []
                    for hf in range(2):
                        bank, bk = rot(7)
                        banks.append((bank, bk))
                        for kc in range(KC):
                            MM(bank[:], mixT(kc, ts_), Wo[:, kc, hf * 512:(hf + 1) * 512], kc == 0, kc == KC - 1,
                               r=["Wo"], w=[bk])
                        ACT(junk[:, 0:512], bank[:], AF.Square, r=[bk], w=[("ssy", j, hf), "junk2"], accum=ssy[:, j, hf:hf + 1])
                    ybanks[j] = banks
                    if j >= 1:
                        pre_b(j - 1, ybanks[j - 1])
                pre_b(3, ybanks[3])
                vTk = [("vT", j) for j in range(4)]
                if _p2 < 1:
                    for j in range(4):
                        DMA("sync", out[(4 * c + j) * 128:(4 * c + j + 1) * 128, :], hbuf[:, j, :], r=[("h", j)], w=[("out", 4 * c + j)])
                    continue
                for p_ in range(npiece):
                    sl = wgu_n % 2
                    wgu_n += 1
                    f0 = p_ * 256
                    DMA("sync", WGU[sl][:, :, 0:256], w_fg_r[:, :, f0:f0 + 256], r=[], w=[("WGUg", sl)])
                    DMA("sync", WGU[sl][:, :, 256:512], w_fu_r[:, :, f0:f0 + 256], r=[], w=[("WGUu", sl)])
                    for f2 in range(2):
                        ft = 2 * p_ + f2
                        bG, kG = rot(7)
                        for kc in range(KC):
                            MM(bG[:], WGU[sl][:, kc, f2 * 128:(f2 + 1) * 128], vT[:, kc, :], kc == 0, kc == KC - 1,
                               r=[("WGUg", sl)] + vTk, w=[kG])
                        bU, kU = rot(7)
                        for kc in range(KC):
                            MM(bU[:], WGU[sl][:, kc, 256 + f2 * 128:256 + (f2 + 1) * 128], vT[:, kc, :], kc == 0,
                               kc == KC - 1, r=[("WGUu", sl)] + vTk, w=[kU])
                        ss_ = ft % 2
                        ACT(sgb[ss_], bG[:], AF.Silu, r=[kG], w=[("sgb", ss_)])
                        TT("vector", actT[:, ft, :], sgb[ss_], bU[:], ALU.mult, r=[("sgb", ss_), kU], w=[("actT", ft)])
                if _p2 < 2:
                    for j in range(4):
                        DMA("sync", out[(4 * c + j) * 128:(4 * c + j + 1) * 128, :], hbuf[:, j, :], r=[("h", j)], w=[("out", 4 * c + j)])
                    continue
                for hf in range(2):
                    hs = slice(hf * 512, (hf + 1) * 512)
                    accs = [(psf[3 + j], f"ps{3 + j}") for j in range(4)]
                    for p_ in range(npiece):
                        sl = wd_n % 4
                        wd_n += 1
                        DMA("sync", WDp[sl], w_fd_r[:, 2 * p_:2 * p_ + 2, hs], r=[], w=[wdk[sl]])
                        for f2 in range(2):
                            ft = 2 * p_ + f2
                            for j in range(4):
                                MM(accs[j][0][:], actT[:, ft, j * 128:(j + 1) * 128], WDp[sl][:, f2, :], ft == 0,
                                   ft == NFT - 1, r=[("actT", ft), wdk[sl]], w=[accs[j][1]])
                    for j in range(4):
                        ACT(junk[:, 0:512], accs[j][0][:], AF.Square, r=[accs[j][1]], w=[("ssf", j, hf), "junk2"],
                            accum=ssf[:, j, hf:hf + 1])
                        CP("vector", ffb[:, j, hs], accs[j][0][:], r=[accs[j][1]], w=[("ffb", j)])
                for j in range(4):
                    t = 4 * c + j
                    TT("gpsimd", rs3[:, j:j + 1], ssf[:, j, 0:1], ssf[:, j, 1:2], ALU.add,
                       r=[("ssf", j, 0), ("ssf", j, 1)], w=[("rs3", j)])
                    TS("gpsimd", rs3[:, j:j + 1], rs3[:, j:j + 1], 1.0 / D, EPS, ALU.mult, ALU.add, r=[("rs3", j)],
                       w=[("rs3", j)])
                    TT("gpsimd", rs3[:, j:j + 1], rs3[:, j:j + 1], nhalf, ALU.pow, r=[("rs3", j), "nhalf"],
                       w=[("rs3", j)])
                    STT(ffb[:, j, :], ffb[:, j, :], rs3[:, j:j + 1], g4, ALU.mult, ALU.mult,
                        r=[("ffb", j), ("rs3", j), "g4"], w=[("ffb", j)])
                    TT("vector", ffb[:, j, :], ffb[:, j, :], hbuf[:, j, :], ALU.add, r=[("ffb", j), ("h", j)],
                       w=[("ffb", j)])
                    DMA("sync", out[t * 128:(t + 1) * 128, :], ffb[:, j, :], r=[("ffb", j)], w=[("out", t)])
            P.add("sync", None, r=[("out", t) for t in range(T)])
        P.barrier()
        print("arena peak bytes", A.peak * 2, "ops", {e: len(P.ops[e]) for e in ENGS})
        P.emit(block, sems)
    return nc


_CACHE = {}


def kernel(**inputs):
    S = 4096
    TOPK = 256
    B = 8
    x = np.asarray(inputs["x"], dtype=np.float32)
    consts = host_consts(S, TOPK)
    shared = {}
    for k in ("norm_mix_pre", "w_in", "b_forget", "b_gate", "w_branch_fox", "w_branch_dsa", "w_out",
              "norm_mix_post", "norm_ffn_pre", "w_ffn_gate", "w_ffn_up", "w_ffn_down", "norm_ffn_post"):
        shared[k] = np.ascontiguousarray(np.asarray(inputs[k], dtype=np.float32)[0])
    shared.update(consts)
    if "nc" not in _CACHE:
        _CACHE["nc"] = build(S, TOPK)
    nc = _CACHE["nc"]
    in_maps = []
    for b in range(B):
        m = dict(shared)
        m["x"] = np.ascontiguousarray(x[b])
        in_maps.append(m)
    res = run_bass_kernel_spmd(nc, in_maps, core_ids=list(range(B)))
    return np.stack([np.asarray(r["out"], dtype=np.float32) for r in res.results], axis=0)
```

```python
import numpy as np
from contextlib import ExitStack
import concourse.bass as bass
import concourse.mybir as mybir
from concourse.bass_utils import run_bass_kernel_spmd

F32 = mybir.dt.float32
BF16 = mybir.dt.bfloat16
ALU = mybir.AluOpType
AF = mybir.ActivationFunctionType
AX = mybir.AxisListType

ENGS = ("sync", "scalar", "tensor", "vector", "gpsimd")

D = 1024
KC = 8
DFF = 2816
NFT = DFF // 128
HD = 128
NIT = 12
ARENA_BYTES = 200 * 1024
EPS = 1e-6
NEG = -30000.0


class Prog:
    NDMA_SEM = 6

    def __init__(self):
        self.ops = {e: [] for e in ENGS}
        self.last_w = {}
        self.readers = {}
        self.last_compute = {e: None for e in ENGS}
        self.last_dmas = {e: [] for e in ENGS}

    def add(self, eng, fn, r=(), w=(), dma=False, extra=()):
        idx = len(self.ops[eng])
        deps = set(extra)
        px = [k for k in r if isinstance(k, str) and k.startswith("ps")]
        if px:
            r = [k for k in r if k not in px]
            w = list(w) + px
        for k in r:
            if k in self.last_w:
                deps.add(self.last_w[k])
        for k in w:
            if k in self.last_w:
                deps.add(self.last_w[k])
            for rd in self.readers.get(k, ()):
                deps.add(rd)
        me = (eng, idx)
        deps.discard(me)
        self.ops[eng].append(dict(fn=fn, deps=deps, dma=dma, signal=False, sig=None))
        for k in r:
            self.readers.setdefault(k, []).append(me)
        for k in w:
            self.last_w[k] = me
            self.readers[k] = []
        if fn is not None:
            if dma:
                self.last_dmas[eng] = (self.last_dmas[eng] + [me])[-self.NDMA_SEM:]
            else:
                self.last_compute[eng] = me
        return me

    def barrier(self):
        deps = []
        for e in ENGS:
            if self.last_compute[e] is not None:
                deps.append(self.last_compute[e])
            deps.extend(self.last_dmas[e])
        for e in ENGS:
            self.add(e, None, extra=deps)
        self.last_w = {}
        self.readers = {}

    def emit(self, block, sems):
        ops = self.ops
        for e in ENGS:
            for op in ops[e]:
                for (e2, j) in op["deps"]:
                    p = ops[e2][j]
                    if e2 == e and e == "tensor" and not p["dma"]:
                        continue
                    p["signal"] = True
        for e in ENGS:
            cnt = 0
            ndma = 0
            for op in ops[e]:
                if op["fn"] is None:
                    continue
                if op["dma"]:
                    s = sems["dma"][e][ndma % self.NDMA_SEM]
                    v = 16 * (ndma // self.NDMA_SEM + 1)
                    op["sig"] = (s, v)
                    op["prev"] = (s, v - 16)
                    ndma += 1
                elif op["signal"]:
                    cnt += 1
                    op["sig"] = (sems["cnt"][e], cnt)

        def make(e):
            def body(eng):
                waited = {}
                for op in ops[e]:
                    need = {}
                    for (e2, j) in op["deps"]:
                        p = ops[e2][j]
                        if p["sig"] is None:
                            continue
                        if e2 == e and e == "tensor" and not p["dma"]:
                            continue
                        s, v = p["sig"]
                        if need.get(id(s), (None, 0))[1] < v:
                            need[id(s)] = (s, v)
                    if op["dma"] and op["fn"] is not None:
                        s, v = op["prev"]
                        if v > 0 and need.get(id(s), (None, 0))[1] < v:
                            need[id(s)] = (s, v)
                    for sid, (s, v) in need.items():
                        if waited.get(sid, 0) < v:
                            eng.wait_ge(s, v)
                            waited[sid] = v
                    if op["fn"] is None:
                        continue
                    ins = op["fn"](eng)
                    if op["dma"]:
                        ins.then_inc(op["sig"][0], 16)
                    elif op["signal"]:
                        ins.then_inc(op["sig"][0], 1)
            return body

        block.sync(make("sync"))
        block.scalar(make("scalar"))
        block.tensor(make("tensor"))
        block.vector(make("vector"))
        block.gpsimd(make("gpsimd"))


class Arena:
    def __init__(self, t, nbytes):
        self.t = t
        self.cap = nbytes // 2
        self.off = 0
        self.peak = 0

    def alloc(self, shape, dtype):
        n = int(np.prod(shape))
        size = 4 if dtype == F32 else 2
        nel = n * size // 2
        nel = (nel + 15) // 16 * 16
        assert self.off + nel <= self.cap, f"arena overflow {self.off + nel} > {self.cap}"
        ap = self.t[:, self.off:self.off + n * size // 2]
        self.off += nel
        self.peak = max(self.peak, self.off)
        if dtype != BF16:
            ap = ap.bitcast(dtype)
        if len(shape) == 2:
            ap = ap.rearrange("p (a b) -> p a b", a=shape[0], b=shape[1])
        elif len(shape) == 3:
            ap = ap.rearrange("p (a b c) -> p a b c", a=shape[0], b=shape[1], c=shape[2])
        return ap

    def mark(self):
        return self.off

    def reset(self, m):
        self.off = m


def host_consts(S, TOPK):
    T = S // 128
    i = np.arange(128)
    c = {}
    c["ident"] = np.eye(128, dtype=np.float32)
    c["ident4"] = np.tile(np.eye(128, dtype=np.float32), (1, 4))
    c["cmT"] = np.where(i[:, None] > i[None, :], NEG, 0.0).astype(np.float32)
    c["cmQ"] = np.where(i[None, :] > i[:, None], NEG, 0.0).astype(np.float32)
    c["negm"] = np.where(i[None, :] > i[:, None], -1e30, 0.0).astype(np.float32)
    c["posm"] = np.where(i[None, :] > i[:, None], 1e30, 0.0).astype(np.float32)
    c["tri"] = (i[:, None] <= i[None, :]).astype(np.float32)
    c["ones"] = np.ones((128, 128), np.float32)
    sel0 = np.zeros((128, 128), np.float32)
    sel0[0, :] = 1.0
    c["sel0"] = sel0
    pos = np.arange(S, dtype=np.float32)

    def tab(rot):
        half = rot // 2
        inv = np.float32(500000.0) ** (-np.arange(half, dtype=np.float32) * np.float32(2.0) / np.float32(rot))
        ang = (pos[:, None] * inv[None, :]).astype(np.float32)
        cs = np.cos(ang).astype(np.float32).reshape(T, 128, half).transpose(1, 0, 2)
        sn = np.sin(ang).astype(np.float32).reshape(T, 128, half).transpose(1, 0, 2)
        return np.ascontiguousarray(cs), np.ascontiguousarray(sn)

    c["cosh"], c["sinh"] = tab(32)
    c["cosi"], c["sini"] = tab(16)
    c["cvec"] = np.tile((2.0 ** -(np.arange(NIT) + 1.0)).astype(np.float32)[None, :], (128, 1))
    return c


CONST_SHAPES = lambda S: {
    "ident": [128, 128], "ident4": [128, 512], "cmT": [128, 128], "cmQ": [128, 128],
    "negm": [128, 128], "posm": [128, 128], "tri": [128, 128], "ones": [128, 128],
    "sel0": [128, 128], "cosh": [128, S // 128, 16], "sinh": [128, S // 128, 16],
    "cosi": [128, S // 128, 8], "sini": [128, S // 128, 8], "cvec": [128, NIT],
}


def build(S, TOPK, passes="FDM2", dbg=None):
    T = S // 128
    NCH = S // 512
    KT0 = TOPK // 128
    nc = bass.Bass("TRN2", target_bir_lowering=False)
    dr = lambda n, s: nc.dram_tensor(n, s, F32, kind="ExternalInput").ap()
    x = dr("x", [S, D])
    g_pre = dr("norm_mix_pre", [D])
    w_in = dr("w_in", [D, 4940])
    b_forget = dr("b_forget", [4])
    b_gate = dr("b_gate", [2, D])
    w_bf = dr("w_branch_fox", [512, D])
    w_bd = dr("w_branch_dsa", [512, D])
    w_out = dr("w_out", [D, D])
    g_post = dr("norm_mix_post", [D])
    g_fpre = dr("norm_ffn_pre", [D])
    w_fg = dr("w_ffn_gate", [D, DFF])
    w_fu = dr("w_ffn_up", [D, DFF])
    w_fd = dr("w_ffn_down", [DFF, D])
    g_fpost = dr("norm_ffn_post", [D])
    cst = {k: dr(k, s) for k, s in CONST_SHAPES(S).items()}
    out = nc.dram_tensor("out", [S, D], F32, kind="ExternalOutput").ap()
    scr_g = nc.dram_tensor("scr_g", [D, DFF], BF16, kind="Internal").ap()
    scr_u = nc.dram_tensor("scr_u", [D, DFF], BF16, kind="Internal").ap()
    scr_d = nc.dram_tensor("scr_d", [DFF, D], BF16, kind="Internal").ap()
    dbg_out = nc.dram_tensor("dbg", [128, 8 * S], F32, kind="ExternalOutput").ap() if dbg else None
    dbg_in = nc.dram_tensor("dbg_in", [128, 8 * S], F32, kind="ExternalInput").ap() if dbg in ("Mi", "2i", "Mif", "Mid") else None

    P = Prog()
    scale = float(HD ** -0.5)
    idx_scale = float((64 ** -0.5) * (8 ** -0.5))

    with ExitStack() as es:
        arena_t = es.enter_context(nc.sbuf_tensor("arena", [128, ARENA_BYTES // 2], BF16))
        A = Arena(arena_t, ARENA_BYTES)
        psf = [es.enter_context(nc.psum_tensor(f"ps{i}", [128, 512], F32)) for i in range(7)]
        pst = es.enter_context(nc.psum_tensor("pst", [128, 1024], BF16))
        sems = {"cnt": {e: es.enter_context(nc.semaphore("c_" + e)) for e in ENGS},
                "dma": {e: [es.enter_context(nc.semaphore(f"d_{e}{i}")) for i in range(Prog.NDMA_SEM)]
                        for e in ("sync", "gpsimd")}}
        sems["dma"]["scalar"] = []
        block = es.enter_context(nc.Block())

        def MM(out_, lhsT, rhs, start, stop, r, w):
            P.add("tensor", lambda e: e.matmul(out=out_, lhsT=lhsT, rhs=rhs, start=start, stop=stop), r=r, w=w)

        def TR(out_, in_, idn, r, w):
            P.add("tensor", lambda e: e.transpose(out=out_, in_=in_, identity=idn), r=r, w=w)

        def ACT(out_, in_, func, r, w, bias=0.0, scale_=1.0, accum=None):
            if accum is None:
                P.add("scalar", lambda e: e.activation(out=out_, in_=in_, func=func, bias=bias, scale=scale_), r=r, w=w)
            else:
                P.add("scalar", lambda e: e.activation(out=out_, in_=in_, func=func, bias=bias, scale=scale_,
                                                       accum_out=accum), r=r, w=w)

        def TS(eng, out_, in0, s1, s2, op0, op1, r, w, accum=None):
            if accum is not None:
                P.add(eng, lambda e: e.tensor_scalar(out=out_, in0=in0, scalar1=s1, scalar2=s2, op0=op0, op1=op1,
                                                     accum_out=accum), r=r, w=w)
            elif op1 is None:
                P.add(eng, lambda e: e.tensor_scalar(out=out_, in0=in0, scalar1=s1, scalar2=None, op0=op0), r=r, w=w)
            else:
                P.add(eng, lambda e: e.tensor_scalar(out=out_, in0=in0, scalar1=s1, scalar2=s2, op0=op0, op1=op1),
                      r=r, w=w)

        def TT(eng, out_, in0, in1, op, r, w):
            P.add(eng, lambda e: e.tensor_tensor(out=out_, in0=in0, in1=in1, op=op), r=r, w=w)

        def STT(out_, in0, sc, in1, op0, op1, r, w, accum=None):
            if accum is None:
                P.add("vector", lambda e: e.scalar_tensor_tensor(out=out_, in0=in0, scalar=sc, in1=in1, op0=op0, op1=op1),
                      r=r, w=w)
            else:
                P.add("vector", lambda e: e.scalar_tensor_tensor(out=out_, in0=in0, scalar=sc, in1=in1, op0=op0, op1=op1,
                                                                  accum_out=accum), r=r, w=w)

        def CP(eng, out_, in_, r, w):
            if eng == "scalar":
                P.add(eng, lambda e: e.copy(out=out_, in_=in_), r=r, w=w)
            else:
                P.add(eng, lambda e: e.tensor_copy(out=out_, in_=in_), r=r, w=w)

        def RCP(out_, in_, r, w):
            P.add("vector", lambda e: e.reciprocal(out=out_, in_=in_), r=r, w=w)

        def RED(out_, in_, op, r, w):
            P.add("vector", lambda e: e.tensor_reduce(out=out_, in_=in_, axis=AX.X, op=op), r=r, w=w)

        def MS(eng, ap, val, w):
            P.add(eng, lambda e: e.memset(ap, val), w=w)

        def DMA(eng, out_, in_, r, w):
            P.add(eng, lambda e: e.dma_start(out=out_, in_=in_), r=r, w=w, dma=True)

        rot_state = {"i": 0}

        def rot(nb=3):
            i = rot_state["i"] % nb
            rot_state["i"] += 1
            return psf[i], f"ps{i}"

        identb = A.alloc([128], BF16)
        ident4b = A.alloc([512], BF16)
        cmTb = A.alloc([128], BF16)
        cmQb = A.alloc([128], BF16)
        onesb = A.alloc([128], BF16)
        negm = A.alloc([128], F32)
        posm = A.alloc([128], F32)
        cosh = A.alloc([T, 16], F32)
        sinh = A.alloc([T, 16], F32)
        cosi = A.alloc([T, 8], F32)
        sini = A.alloc([T, 8], F32)
        cvec = A.alloc([NIT], F32)
        nhalf = A.alloc([1], F32)
        identf = A.alloc([128], F32)
        g8 = A.alloc([128], F32)
        gT = A.alloc([8], F32)
        odT = A.alloc([4, S], BF16)
        A.full_cap = A.cap
        ofT = arena_t[:, A.cap - 4 * S:A.cap].rearrange("p (a b) -> p a b", a=4, b=S)
        A.cap = A.full_cap - 4 * S
        for ap_, nm in ((identb, "ident"), (ident4b, "ident4"), (cmTb, "cmT"), (cmQb, "cmQ"), (onesb, "ones")):
            DMA("gpsimd", ap_, cst[nm], r=[], w=["c_" + nm + "b"])
        for ap_, nm in ((negm, "negm"), (posm, "posm"), (cosh, "cosh"), (sinh, "sinh"), (cosi, "cosi"), (sini, "sini"),
                        (cvec, "cvec")):
            DMA("sync", ap_, cst[nm], r=[], w=["c_" + nm])
        DMA("sync", identf, cst["ident"], r=[], w=["c_ident"])
        DMA("sync", g8[0:8, :], g_pre.rearrange("(kc p) -> kc p", p=128), r=[], w=["g8"])
        TR(psf[6][:, 0:8], g8[0:8, :], identf[0:8, 0:8], r=["g8", "c_ident"], w=["ps6"])
        CP("vector", gT, psf[6][:, 0:8], r=["ps6"], w=["gT"])
        MS("gpsimd", nhalf, -0.5, w=["nhalf"])
        persist_mark = A.mark()
        def convert_ffn_weights():
            if "2" not in passes:
                return
            for hlf in range(2):
                DMA("gpsimd", scr_g[hlf * 512:(hlf + 1) * 512, :], w_fg[hlf * 512:(hlf + 1) * 512, :], r=[], w=[("scr_g", hlf)])
                DMA("gpsimd", scr_u[hlf * 512:(hlf + 1) * 512, :], w_fu[hlf * 512:(hlf + 1) * 512, :], r=[], w=[("scr_u", hlf)])
                DMA("gpsimd", scr_d[hlf * 1408:(hlf + 1) * 1408, :], w_fd[hlf * 1408:(hlf + 1) * 1408, :], r=[], w=[("scr_d", hlf)])

        def u_stage(t, j, bufs, key=None):
            s = t % 2
            xs = t % len(bufs["xt"])
            xt, ssq, rstd, ub, uT = bufs["xt"][xs], bufs["ss"][s], bufs["rstd"][s], bufs["ub"][s], bufs["uT"]
            DMA("sync", xt, x[t * 128:(t + 1) * 128, :], r=[], w=[("xt", xs)])
            ACT(ub, xt, AF.Square, r=[("xt", xs)], w=[("ss", s), ("ub", s)], accum=ssq)
            TS("gpsimd", rstd, ssq, 1.0 / D, EPS, ALU.mult, ALU.add, r=[("ss", s)], w=[("rstd", s)])
            TT("gpsimd", rstd, rstd, nhalf, ALU.pow, r=[("rstd", s), "nhalf"], w=[("rstd", s)])
            ACT(ub, xt, AF.Copy, r=[("xt", xs), ("rstd", s)], w=[("ub", s)], scale_=rstd)
            for kc in range(KC):
                TR(pst[:, kc * 128:(kc + 1) * 128], ub[:, kc * 128:(kc + 1) * 128], identb,
                   r=[("ub", s), "c_identb"], w=["pst"])
            CP("scalar", uT[:, :, j * 128:(j + 1) * 128], pst[:].rearrange("p (a b) -> p a b", a=KC, b=128),
               r=["pst"], w=[("uT", j) if key is None else key + (j,)])

        def fold_gain(W, keys):
            for kc in range(KC):
                TS("vector", W[:, kc, :], W[:, kc, :], gT[:, kc:kc + 1], None, ALU.mult, None, r=list(keys) + ["gT"],
                   w=list(keys))

        def u_bufs(nx=2):
            return dict(xt=[A.alloc([D], F32) for _ in range(nx)], ss=[A.alloc([1], F32) for _ in range(2)],
                        rstd=[A.alloc([1], F32) for _ in range(2)], ub=[A.alloc([D], BF16) for _ in range(2)],
                        uT=A.alloc([KC, 512], BF16))

        w_in_r = w_in.rearrange("(kc p) c -> p kc c", p=128)

        if "D" not in passes:
            convert_ffn_weights()
            if "F" not in passes and "M" not in passes:
                P.barrier()
        if "D" in passes:
            A.cap = A.full_cap
            WD_ = A.alloc([KC, 1352], BF16)
            KdT = A.alloc([S], BF16)
            Vd = A.alloc([T, 128], BF16)
            kiT = A.alloc([S], BF16)
            QQ = [A.alloc([1024], BF16) for _ in range(3)]
            sgnD = [A.alloc([8, 128], BF16) for _ in range(3)]
            scs = [A.alloc([S], F32) for _ in range(2)]
            Mneg = [A.alloc([S], BF16) for _ in range(2)]
            R = [A.alloc([8, 512], BF16) for _ in range(1)]
            qd_f = A.alloc([4, 128], F32)
            qi_f = A.alloc([8, 64], F32)
            g4_f = A.alloc([328], F32)
            qd_b = A.alloc([4, 128], BF16)
            qi_b = A.alloc([8, 64], BF16)
            kk_b = A.alloc([256], BF16)
            rt = [A.alloc([8, 16], F32) for _ in range(4)]
            aw = A.alloc([8], F32)
            sg01 = A.alloc([8], F32)
            sgn = A.alloc([8], F32)
            tmpds = [A.alloc([128], F32) for _ in range(2)]
            sts = [A.alloc([8], F32) for _ in range(2)]
            Wc = A.alloc([NIT], F32)
            mids = A.alloc([NIT + 1], F32)
            cnts = A.alloc([NIT], F32)
            sgs = A.alloc([NIT], F32)
            pT = [A.alloc([512], BF16) for _ in range(2)]
            oS = [A.alloc([512], F32) for _ in range(2)]
            dS = [A.alloc([512], F32) for _ in range(2)]
            rden = A.alloc([512], F32)
            ub_ = u_bufs(2)
            for (d0, s0, n_) in ((0, 1540, 512), (512, 2308, 512), (1024, 2052, 256), (1280, 2820, 72)):
                DMA("gpsimd", WD_[:, :, d0:d0 + n_], w_in_r[:, :, s0:s0 + n_], r=[], w=[("WD", d0)])
            WDK = [("WD", 0), ("WD", 512), ("WD", 1024), ("WD", 1280)]
            fold_gain(WD_, WDK)
            convert_ffn_weights()

            def rope(src3, dst3, nh, half, cos_t, sin_t, key_src, key_dst):
                x1 = src3[:, :, 0:half]
                x2 = src3[:, :, half:2 * half]
                cb_ = cos_t.unsqueeze(1).broadcast_to([128, nh, half])
                sb_ = sin_t.unsqueeze(1).broadcast_to([128, nh, half])
                ta, tb, tc, td = [r_[:, 0:nh, 0:half] for r_ in rt]
                TT("gpsimd", ta, x1, cb_, ALU.mult, r=[key_src], w=["rt0"])
                TT("gpsimd", tb, x2, sb_, ALU.mult, r=[key_src], w=["rt1"])
                TT("gpsimd", tc, x2, cb_, ALU.mult, r=[key_src], w=["rt2"])
                TT("gpsimd", td, x1, sb_, ALU.mult, r=[key_src], w=["rt3"])
                TT("gpsimd", dst3[:, :, 0:half], ta, tb, ALU.subtract, r=["rt0", "rt1"], w=[key_dst])
                TT("gpsimd", dst3[:, :, half:2 * half], tc, td, ALU.add, r=["rt2", "rt3"], w=[key_dst])

            def prepP(t):
                c, j = t // 4, t % 4
                qs = t % 3
                if j == 0:
                    for jj in range(4):
                        u_stage(4 * c + jj, jj, ub_)
                uT = ub_["uT"]
                for (dst, c0, wn, key) in ((qd_f, 0, 512, "qd_f"), (qi_f, 512, 512, "qi_f"), (g4_f, 1024, 328, "g4_f")):
                    bank, bk = rot()
                    for kc in range(KC):
                        MM(bank[:, 0:wn], uT[:, kc, j * 128:(j + 1) * 128], WD_[:, kc, c0:c0 + wn], kc == 0,
                           kc == KC - 1, r=WDK + [("uT", j)], w=[bk])
                    dflat = dst if key == "g4_f" else dst.rearrange("p a b -> p (a b)")
                    CP("scalar", dflat, bank[:, 0:wn], r=[bk], w=[key])
                rope(qd_f, qd_b, 4, 16, cosh[:, t, :], sinh[:, t, :], "qd_f", "qd_b")
                CP("gpsimd", qd_b[:, :, 32:128], qd_f[:, :, 32:128], r=["qd_f"], w=["qd_b"])
                TS("gpsimd", sg01, g4_f[:, 320:328], 0.0, None, ALU.is_ge, None, r=["g4_f"], w=["sg01"])
                TS("gpsimd", sgn, sg01, 2.0, -1.0, ALU.mult, ALU.add, r=["sg01"], w=["sgn"])
                TS("gpsimd", aw, sg01, 2.0 * idx_scale, -idx_scale, ALU.mult, ALU.add, r=["sg01"], w=["aw"])
                TT("gpsimd", aw, aw, g4_f[:, 320:328], ALU.mult, r=["aw", "g4_f"], w=["aw"])
                rope(qi_f, qi_f, 8, 8, cosi[:, t, :], sini[:, t, :], "qi_f", "qi_f")
                TT("gpsimd", qi_b, qi_f, aw.unsqueeze(2).broadcast_to([128, 8, 64]), ALU.mult, r=["qi_f", "aw"],
                   w=["qi_b"])
                kd3 = g4_f[:, 0:128].rearrange("p (a b) -> p a b", a=1, b=128)
                kdb3 = kk_b[:, 0:128].rearrange("p (a b) -> p a b", a=1, b=128)
                rope(kd3, kdb3, 1, 16, cosh[:, t, :], sinh[:, t, :], "g4_f", "kk_b")
                CP("gpsimd", kk_b[:, 32:128], g4_f[:, 32:128], r=["g4_f"], w=["kk_b"])
                ki3 = g4_f[:, 256:320].rearrange("p (a b) -> p a b", a=1, b=64)
                kib3 = kk_b[:, 128:192].rearrange("p (a b) -> p a b", a=1, b=64)
                rope(ki3, kib3, 1, 8, cosi[:, t, :], sini[:, t, :], "g4_f", "kk_b")
                CP("gpsimd", kk_b[:, 144:192], g4_f[:, 272:320], r=["g4_f"], w=["kk_b"])
                CP("gpsimd", kk_b[:, 192:256], kk_b[:, 128:192], r=["kk_b"], w=["kk_b2"])
                CP("gpsimd", Vd[:, t, :], g4_f[:, 128:256], r=["g4_f"], w=[("Vd", t)])
                TT("gpsimd", sgnD[qs], identb.unsqueeze(1).broadcast_to([128, 8, 128]),
                   sgn.unsqueeze(2).broadcast_to([128, 8, 128]), ALU.mult, r=["c_identb", "sgn"], w=[("sgnD", qs)])

            def prepT(t):
                qs = t % 3
                for i_ in range(2):
                    TR(pst[:, i_ * 128:(i_ + 1) * 128], kk_b[:, i_ * 128:(i_ + 1) * 128], identb,
                       r=["kk_b", "kk_b2", "c_identb"], w=["pst"])
                CP("scalar", KdT[:, t * 128:(t + 1) * 128], pst[:, 0:128], r=["pst"], w=[("KdT", t)])
                CP("scalar", kiT[:, t * 128:(t + 1) * 128], pst[:, 128:256], r=["pst"], w=[("kiT", t)])
                qdb2 = qd_b.rearrange("p a b -> p (a b)")
                qib2 = qi_b.rearrange("p a b -> p (a b)")
                for i_ in range(4):
                    TR(pst[:, i_ * 128:(i_ + 1) * 128], qdb2[:, i_ * 128:(i_ + 1) * 128], identb,
                       r=["qd_b", "c_identb"], w=["pst"])
                for i_ in range(4):
                    TR(pst[:, 512 + i_ * 128:512 + (i_ + 1) * 128], qib2[:, i_ * 128:(i_ + 1) * 128], identb,
                       r=["qi_b", "c_identb"], w=["pst"])
                CP("scalar", QQ[qs], pst[:], r=["pst"], w=[("QQ", qs)])

            def stageA(t):
                q3 = t % 3
                qs = t % 2
                n = (t + 1) * 128
                sc = scs[qs]
                tmpd = tmpds[qs]
                if t < KT0:
                    return
                nk5 = (n + 511) // 512
                for k5 in range(nk5):
                    c0 = k5 * 512
                    wn = min(512, n - c0)
                    rs = 0
                    kik = [("kiT", tt) for tt in range(c0 // 128, (c0 + wn) // 128)]
                    for hd in range(8):
                        hp, hf = hd // 2, hd % 2
                        bank, bk = rot()
                        MM(bank[:, 0:wn], QQ[q3][64 * hf:64 * hf + 64, 512 + hp * 128:512 + (hp + 1) * 128],
                           kiT[64 * hf:64 * hf + 64, c0:c0 + wn], True, True, r=[("QQ", q3)] + kik, w=[bk])
                        ACT(R[rs][:, hd, 0:wn], bank[:, 0:wn], AF.Relu, r=[bk], w=[("R", rs, hd)])
                    sb_, sbk = psf[3], "ps3"
                    for hd in range(8):
                        MM(sb_[:, 0:wn], sgnD[q3][:, hd, :], R[rs][:, hd, 0:wn], hd == 0, hd == 7,
                           r=[("sgnD", q3), ("R", rs, hd)], w=[sbk])
                    CP("scalar", sc[:, c0:c0 + wn], sb_[:, 0:wn], r=[sbk], w=[("sc", qs, k5)])
                sck = [("sc", qs, k5) for k5 in range(nk5)]
                TT("gpsimd", tmpd, sc[:, n - 128:n], posm, ALU.add, r=sck + ["c_posm"], w=[("tmpd", qs)])
                TT("gpsimd", sc[:, n - 128:n], sc[:, n - 128:n], negm, ALU.add, r=sck + ["c_negm", ("tmpd", qs)],
                   w=[("sc", qs, nk5 - 1)])

            def stageB(t):
                qs = t % 2
                ms_ = qs
                n = (t + 1) * 128
                sc = scs[qs]
                tmpd = tmpds[qs]
                st = sts[qs]
                if t < KT0:
                    if n > 128:
                        MS("gpsimd", Mneg[ms_][:, 0:n - 128], 0.0, w=[("Mneg", ms_)])
                    CP("gpsimd", Mneg[ms_][:, n - 128:n], cmQb, r=["c_cmQb"], w=[("Mneg", ms_)])
                    return
                nk5 = (n + 511) // 512
                sck = [("sc", qs, k5) for k5 in range(nk5)]
                RED(st[:, 0:1], sc[:, 0:n], ALU.max, r=sck, w=[("hi", qs)])
                RED(st[:, 1:2], tmpd, ALU.min, r=[("tmpd", qs)], w=[("m1", qs)])
                RED(st[:, 2:3], sc[:, 0:n - 128], ALU.min, r=sck, w=[("m2", qs)])
                TT("vector", st[:, 3:4], st[:, 1:2], st[:, 2:3], ALU.min, r=[("m1", qs), ("m2", qs)], w=[("lo", qs)])
                TT("vector", st[:, 4:5], st[:, 0:1], st[:, 3:4], ALU.subtract, r=[("hi", qs), ("lo", qs)],
                   w=[("Wd", qs)])
                TS("vector", Wc, cvec, st[:, 4:5], None, ALU.mult, None, r=["c_cvec", ("Wd", qs)], w=["Wc"])
                TT("vector", mids[:, 0:1], st[:, 3:4], Wc[:, 0:1], ALU.add, r=[("lo", qs), "Wc"], w=[("mid", 0)])
                for it in range(NIT):
                    TS("vector", Mneg[ms_][:, 0:n], sc[:, 0:n], mids[:, it:it + 1], None, ALU.is_ge, ALU.add,
                       r=sck + [("mid", it)], w=[("cnt", it), ("Mneg", ms_)], accum=cnts[:, it:it + 1])
                    TS("vector", sgs[:, it:it + 1], cnts[:, it:it + 1], TOPK - 0.5, 0.5, ALU.is_ge, ALU.subtract,
                       r=[("cnt", it)], w=[("sg", it)])
                    STT(mids[:, it + 1:it + 2], sgs[:, it:it + 1], Wc[:, it:it + 1], mids[:, it:it + 1], ALU.mult,
                        ALU.add, r=[("sg", it), "Wc", ("mid", it)], w=[("mid", it + 1)])
                STT(st[:, 5:6], st[:, 4:5], -(2.0 ** -(NIT + 1)), mids[:, NIT:NIT + 1], ALU.mult, ALU.add,
                    r=[("Wd", qs), ("mid", NIT)], w=[("tau", qs)])
                TS("vector", Mneg[ms_][:, 0:n], sc[:, 0:n], st[:, 5:6], NEG, ALU.is_lt, ALU.mult,
                   r=sck + [("tau", qs)], w=[("Mneg", ms_)])

            def stageC(t):
                qs = t % 2
                q3 = t % 3
                ms_ = qs
                ob, obk, db, dbk = psf[4], "ps4", psf[5], "ps5"
                def qk(kt):
                    bank, bk = rot()
                    MM(bank[:], KdT[:, kt * 128:(kt + 1) * 128], QQ[q3][:, 0:512], True, False,
                       r=[("KdT", kt), ("QQ", q3)], w=[bk])
                    MM(bank[:], Mneg[ms_][:, kt * 128:(kt + 1) * 128], ident4b, False, True,
                       r=[("Mneg", ms_), "c_ident4b"], w=[bk])
                    return bank, bk
                cur = qk(0)
                for kt in range(t + 1):
                    nxt = qk(kt + 1) if kt + 1 <= t else None
                    bank, bk = cur
                    ps_ = kt % 2
                    ACT(pT[ps_], bank[:], AF.Exp, r=[bk], w=[("pT", ps_)], scale_=scale)
                    MM(ob[:], Vd[:, kt, :], pT[ps_], kt == 0, kt == t, r=[("Vd", kt), ("pT", ps_)], w=[obk])
                    MM(db[:], onesb, pT[ps_], kt == 0, kt == t, r=["c_onesb", ("pT", ps_)], w=[dbk])
                    cur = nxt
                CP("scalar", oS[qs], ob[:], r=[obk], w=[("oS", qs)])
                CP("scalar", dS[qs], db[:], r=[dbk], w=[("dS", qs)])

            def stageN(t):
                qs = t % 2
                RCP(rden, dS[qs], r=[("dS", qs)], w=["rden"])
                TT("vector", odT[:, :, t * 128:(t + 1) * 128], oS[qs].rearrange("p (a b) -> p a b", a=4, b=128),
                   rden.rearrange("p (a b) -> p a b", a=4, b=128), ALU.mult, r=[("oS", qs), "rden"], w=[("odT", t)])

            prepP(0)
            prepT(0)
            stageA(0)
            if T > 1:
                prepP(1)
                prepT(1)
            for t in range(T):
                if t + 2 < T:
                    prepP(t + 2)
                if t + 1 < T:
                    stageA(t + 1)
                stageB(t)
                if t >= 1:
                    stageN(t - 1)
                if t + 2 < T:
                    prepT(t + 2)
                stageC(t)
            stageN(T - 1)
            P.barrier()
            A.reset(persist_mark)
            A.cap = A.full_cap - 4 * S
            if dbg in ("D", "Y"):
                if dbg == "D":
                    DMA("gpsimd", dbg_out[:, 4 * S:8 * S], odT.rearrange("p a b -> p (a b)"), r=[], w=["dbgD"])
                if dbg == "Y":
                    DMA("gpsimd", dbg_out[:, 0:4 * S], ofT.rearrange("p a b -> p (a b)"), r=[], w=["dbgD"])
                P.add("sync", None, r=["dbgD"])
                P.barrier()

        if "F" in passes:
            tri = A.alloc([128], F32)
            onesf = A.alloc([128], F32)
            sel0 = A.alloc([128], F32)
            for ap_, nm in ((tri, "tri"), (onesf, "ones"), (sel0, "sel0")):
                DMA("sync", ap_, cst[nm], r=[], w=["c_" + nm])
            WF = A.alloc([KC, 1540], BF16)
            KfT = A.alloc([4, S], BF16)
            Vf = A.alloc([T, 512], BF16)
            negc = A.alloc([T, 4], F32)
            biascs = [A.alloc([T, 4], F32) for _ in range(2)]
            bfb = A.alloc([4, 4], F32)
            carry = A.alloc([4], F32)
            refb = A.alloc([4], F32)
            v3 = lambda a_: a_.rearrange("p (a b) -> p a b", a=4, b=4)
            ztf = A.alloc([16], F32)
            etf = A.alloc([16], F32)
            ltf = A.alloc([16], F32)
            Lsf = A.alloc([16], F32)
            zt, et, lt, Ls = v3(ztf), v3(etf), v3(ltf), v3(Lsf)
            ltot = A.alloc([4], F32)
            QfTs = [A.alloc([4, 512], BF16) for _ in range(2)]
            pT = [A.alloc([512], BF16) for _ in range(2)]
            rden = A.alloc([512], F32)
            ub_ = u_bufs(1)
            for half in range(2):
                DMA("gpsimd", WF[:, half * 4:(half + 1) * 4, :], w_in_r[:, half * 4:(half + 1) * 4, 0:1540],
                    r=[], w=[("WF", half)])
            WFK = [("WF", 0), ("WF", 1)]
            fold_gain(WF, WFK)
            for jj in range(4):
                DMA("sync", bfb[:, jj, :], b_forget.partition_broadcast(128), r=[], w=[("bfb", jj)])
            MS("gpsimd", carry, 0.0, w=["carry"])
            MS("gpsimd", Ls[:, 0, :], 0.0, w=["Ls0"])
            uT = ub_["uT"]
            uTk = [("uT", j) for j in range(4)]
            fb2 = psf[2]

            ub4 = ub_["ub"] + [A.alloc([D], BF16) for _ in range(2)]

            def prepA0(c):
                for j in range(4):
                    t = 4 * c + j
                    xs = 0
                    xt, ssq, rstd, ub = ub_["xt"][xs], ub_["ss"][xs], ub_["rstd"][xs], ub4[j]
                    DMA("sync", xt, x[t * 128:(t + 1) * 128, :], r=[], w=[("xt", xs)])
                    STT(ub, xt, 1.0, xt, ALU.mult, ALU.mult, r=[("xt", xs)], w=[("ss", xs), ("ub4", j)], accum=ssq)
                    TS("gpsimd", rstd, ssq, 1.0 / D, EPS, ALU.mult, ALU.add, r=[("ss", xs)], w=[("rstd", xs)])
                    TT("gpsimd", rstd, rstd, nhalf, ALU.pow, r=[("rstd", xs), "nhalf"], w=[("rstd", xs)])
                    TS("vector", ub, xt, rstd, None, ALU.mult, None, r=[("xt", xs), ("rstd", xs)], w=[("ub4", j)])

            def prepA1(c):
                for j in range(4):
                    ub = ub4[j]
                    for kc in range(KC):
                        TR(pst[:, kc * 128:(kc + 1) * 128], ub[:, kc * 128:(kc + 1) * 128], identb,
                           r=[("ub4", j), "c_identb"], w=["pst"])
                    CP("vector", uT[:, :, j * 128:(j + 1) * 128], pst[:].rearrange("p (a b) -> p a b", a=KC, b=128),
                       r=["pst"], w=[("uT", j)])

            def prepB1(c):
                qsl = c % 2
                for g in range(8):
                    bank, bk = rot(2)
                    for kc in range(KC):
                        MM(bank[:], WF[:, kc, g * 128:(g + 1) * 128], uT[:, kc, :], kc == 0, kc == KC - 1,
                           r=WFK + uTk, w=[bk])
                    if g < 4:
                        CP("vector", QfTs[qsl][:, g, :], bank[:], r=[bk], w=[("QfT", qsl, g)])
                    else:
                        CP("vector", KfT[:, g - 4, c * 512:(c + 1) * 512], bank[:], r=[bk], w=[("KfT", g - 4, c)])

            def prepB2(c):
                bsl = c % 2
                biasc = biascs[bsl]
                for j in range(4):
                    t = 4 * c + j
                    bank, bk = rot(2)
                    for kc in range(KC):
                        MM(bank[:], uT[:, kc, j * 128:(j + 1) * 128], WF[:, kc, 1024:1536], kc == 0, kc == KC - 1,
                           r=WFK + [("uT", j)], w=[bk])
                    CP("vector", Vf[:, t, :], bank[:], r=[bk], w=[("Vf", t)])
                    for kc in range(KC):
                        MM(fb2[:, j * 4:(j + 1) * 4], uT[:, kc, j * 128:(j + 1) * 128], WF[:, kc, 1536:1540],
                           kc == 0, kc == KC - 1, r=WFK + [("uT", j)], w=["ps2"])
                TT("vector", zt, fb2[:, 0:16].rearrange("p (a b) -> p a b", a=4, b=4), bfb, ALU.add,
                   r=["ps2"] + [("bfb", jj) for jj in range(4)], w=["zt"])
                ACT(et, zt, AF.Exp, r=["zt"], w=["et"], scale_=-1.0)
                ACT(lt, et, AF.Ln, r=["et"], w=["lt"], bias=1.0)
                CP("gpsimd", Ls[:, 1, :], lt[:, 0, :], r=["lt"], w=["Ls1"])
                TT("gpsimd", Ls[:, 2, :], Ls[:, 1, :], lt[:, 1, :], ALU.add, r=["lt", "Ls1"], w=["Ls2"])
                TT("gpsimd", Ls[:, 3, :], Ls[:, 2, :], lt[:, 2, :], ALU.add, r=["lt", "Ls2"], w=["Ls3"])
                TT("gpsimd", ltot, Ls[:, 3, :], lt[:, 3, :], ALU.add, r=["lt", "Ls3"], w=["ltot"])
                MM(fb2[:, 32:48], tri, ltf, True, False, r=["c_tri", "lt"], w=["ps2"])
                MM(fb2[:, 32:48], onesf, Lsf, False, True, r=["c_ones", "Ls0", "Ls1", "Ls2", "Ls3"], w=["ps2"])
                MM(fb2[:, 48:52], onesf, ltot, True, True, r=["c_ones", "ltot"], w=["ps2"])
                TT("vector", negc[:, 4 * c:4 * c + 4, :], fb2[:, 32:48].rearrange("p (a b) -> p a b", a=4, b=4),
                   carry.unsqueeze(1).broadcast_to([128, 4, 4]), ALU.add, r=["ps2", "carry"], w=[("negc", c)])
                TT("vector", carry, fb2[:, 48:52], carry, ALU.add, r=["ps2", "carry"], w=["carry"])
                MM(fb2[:, 64:68], sel0, negc[:, 4 * c + 2, :], True, True, r=["c_sel0", ("negc", c)], w=["ps2"])
                CP("vector", refb, fb2[:, 64:68], r=["ps2"], w=["refb"])
                nkt = 4 * c + 4
                for h in range(4):
                    TS("vector", biasc[:, 0:nkt, h], negc[:, 0:nkt, h], refb[:, h:h + 1], None, ALU.subtract, None,
                       r=[("negc", cc) for cc in range(c + 1)] + ["refb"], w=[("biasc", bsl, h)])

            ostate = {"o": 0}

            def attn(c, h):
                qsl = c % 2
                bsl = c % 2
                QfT = QfTs[qsl]
                biasc = biascs[bsl]
                nkt = 4 * c + 4
                ob, obk, db, dbk = (psf[3], "ps3", psf[4], "ps4") if ostate["o"] == 0 else (psf[5], "ps5", psf[6], "ps6")
                ostate["o"] ^= 1

                def qkf(kt):
                    off = max(0, kt - 4 * c) * 128
                    diag = kt >= 4 * c
                    bank, bk = rot(2)
                    MM(bank[:, off:512], KfT[:, h, kt * 128:(kt + 1) * 128], QfT[:, h, off:512], True, not diag,
                       r=[("KfT", h, kt // 4), ("QfT", qsl, h)], w=[bk])
                    if diag:
                        MM(bank[:, off:off + 128], identb, cmTb, False, True, r=["c_identb", "c_cmTb"], w=[bk])
                    return bank, bk, off
                cur = qkf(0)
                for kt in range(nkt):
                    nxt = qkf(kt + 1) if kt + 1 < nkt else None
                    bank, bk, off = cur
                    ps_ = kt % 2
                    ACT(pT[ps_][:, off:512], bank[:, off:512], AF.Exp, r=[bk, ("biasc", bsl, h)], w=[("pT", ps_)],
                        bias=biasc[:, kt, h:h + 1], scale_=scale)
                    MM(ob[:, off:512], Vf[:, kt, h * 128:(h + 1) * 128], pT[ps_][:, off:512], kt == 0,
                       kt == nkt - 1, r=[("Vf", kt), ("pT", ps_)], w=[obk])
                    MM(db[:, off:512], onesb, pT[ps_][:, off:512], kt == 0, kt == nkt - 1,
                       r=["c_onesb", ("pT", ps_)], w=[dbk])
                    cur = nxt
                RCP(rden, db[:], r=[dbk], w=["rden"])
                TT("vector", ofT[:, h, c * 512:(c + 1) * 512], ob[:], rden, ALU.mult, r=[obk, "rden"],
                   w=[("ofT", h, c)])

            prepA0(0)
            prepA1(0)
            prepB1(0)
            prepB2(0)
            for c in range(NCH):
                if c + 1 < NCH:
                    prepA0(c + 1)
                attn(c, 0)
                if c + 1 < NCH:
                    prepA1(c + 1)
                attn(c, 1)
                if c + 1 < NCH:
                    prepB1(c + 1)
                attn(c, 2)
                if c + 1 < NCH:
                    prepB2(c + 1)
                attn(c, 3)
            P.barrier()
            A.reset(persist_mark)
            if dbg == "F":
                DMA("gpsimd", dbg_out[:, 0:4 * S], ofT.rearrange("p a b -> p (a b)"), r=[], w=["dbgF"])
                P.add("sync", None, r=["dbgF"])
                P.barrier()

        if dbg in ("Mi", "2i", "Mif", "Mid"):
            if dbg != "Mif":
                DMA("gpsimd", ofT.rearrange("p a b -> p (a b)"), dbg_in[:, 0:4 * S], r=[], w=["dbgi"])
            if dbg != "Mid":
                DMA("gpsimd", odT.rearrange("p a b -> p (a b)"), dbg_in[:, 4 * S:8 * S], r=[], w=["dbgi2"])
            P.barrier()
        if "M" in passes:
            Wg = A.alloc([KC, 2048], BF16)
            Wbf = A.alloc([4, D], BF16)
            Wbd = A.alloc([4, D], BF16)
            bg16 = A.alloc([128], F32)
            bgT = A.alloc([16], F32)
            sigf = A.alloc([512], F32)
            sigd = A.alloc([512], F32)
            t1 = A.alloc([512], F32)
            t2 = A.alloc([512], F32)
            mixt = A.alloc([8, 512], BF16)
            ub_ = u_bufs()
            for q4 in range(4):
                DMA("gpsimd", Wg[:, q4 * 2:(q4 + 1) * 2, :], w_in_r[:, q4 * 2:(q4 + 1) * 2, 2892:4940], r=[],
                    w=[("Wg", q4)])
            WGK = [("Wg", q4) for q4 in range(4)]
            fold_gain(Wg, WGK)
            DMA("gpsimd", Wbf, w_bf.rearrange("(h p) c -> p h c", p=128), r=[], w=["Wbf"])
            DMA("gpsimd", Wbd, w_bd.rearrange("(h p) c -> p h c", p=128), r=[], w=["Wbd"])
            DMA("sync", bg16[0:16, :], b_gate.rearrange("b (kc p) -> (b kc) p", p=128), r=[], w=["bg16"])
            tb_, tbk = psf[6], "ps6"
            TR(tb_[:, 0:16], bg16[0:16, :], identf[0:16, 0:16], r=["bg16", "c_ident"], w=[tbk])
            CP("vector", bgT, tb_[:, 0:16], r=[tbk], w=["bgT"])
            uT2 = [ub_["uT"], A.alloc([KC, 512], BF16)]

            def uprep(c):
                ub_["uT"] = uT2[c % 2]
                for j in range(4):
                    u_stage(4 * c + j, j, ub_, key=("uT", c % 2))
            uprep(0)
            for c in range(NCH):
                if c + 1 < NCH:
                    uprep(c + 1)
                uT = uT2[c % 2]
                uTk = [("uT", c % 2, j) for j in range(4)]
                cs = slice(c * 512, (c + 1) * 512)
                for cc in range(8):
                    bA, kA = rot(7)
                    for kc in range(KC):
                        MM(bA[:], Wg[:, kc, cc * 128:(cc + 1) * 128], uT[:, kc, :], kc == 0, kc == KC - 1,
                           r=WGK + uTk, w=[kA])
                    bB, kB = rot(7)
                    for kc in range(KC):
                        MM(bB[:], Wg[:, kc, 1024 + cc * 128:1024 + (cc + 1) * 128], uT[:, kc, :], kc == 0,
                           kc == KC - 1, r=WGK + uTk, w=[kB])
                    bC, kCk = rot(7)
                    for h in range(4):
                        MM(bC[:], Wbf[:, h, cc * 128:(cc + 1) * 128], ofT[:, h, cs], h == 0, h == 3,
                           r=["Wbf", ("ofT", h, c)], w=[kCk])
                    bD, kDk = rot(7)
                    for h in range(4):
                        MM(bD[:], Wbd[:, h, cc * 128:(cc + 1) * 128], odT[:, h, cs], h == 0, h == 3,
                           r=["Wbd", ("odT", h, c)], w=[kDk])
                    ACT(sigf, bA[:], AF.Sigmoid, r=[kA, "bgT"], w=["sigf"], bias=bgT[:, cc:cc + 1])
                    ACT(sigd, bB[:], AF.Sigmoid, r=[kB, "bgT"], w=["sigd"], bias=bgT[:, 8 + cc:9 + cc])
                    TT("vector", t1, sigf, bC[:], ALU.mult, r=["sigf", kCk], w=["t1"])
                    TT("vector", t2, sigd, bD[:], ALU.mult, r=["sigd", kDk], w=["t2"])
                    TT("vector", mixt[:, cc, :], t1, t2, ALU.add, r=["t1", "t2"], w=[("mixt", cc)])
                CP("vector", ofT[:, :, cs], mixt[:, 0:4, :], r=[("mixt", cc) for cc in range(4)],
                   w=[("ofT", h, c) for h in range(4)])
                CP("vector", odT[:, :, cs], mixt[:, 4:8, :], r=[("mixt", cc) for cc in range(4, 8)],
                   w=[("odT", h, c) for h in range(4)])
            P.barrier()
            A.reset(persist_mark)
            if dbg in ("M", "Mi", "Mif", "Mid"):
                DMA("gpsimd", dbg_out[:, 0:4 * S], ofT.rearrange("p a b -> p (a b)"), r=[], w=["dbgF"])
                DMA("gpsimd", dbg_out[:, 4 * S:8 * S], odT.rearrange("p a b -> p (a b)"), r=[], w=["dbgD"])
                P.add("sync", None, r=["dbgF", "dbgD"])
                P.barrier()

        if "2" in passes:
            Wo = A.alloc([KC, D], BF16)
            gpost = A.alloc([D], F32)
            g3 = A.alloc([D], F32)
            g4 = A.alloc([D], F32)
            hbuf = A.alloc([4, D], F32)
            ffb = A.alloc([4, D], F32)
            tmpy = A.alloc([512], F32)
            vb = A.alloc([D], BF16)
            vT = A.alloc([KC, 512], BF16)
            WGU = [A.alloc([KC, 512], BF16) for _ in range(2)]
            WDp = [A.alloc([2, 512], BF16) for _ in range(4)]
            sgb = [WDp[2 + i_].rearrange("p a b -> p (a b)").bitcast(F32) for i_ in range(2)]
            wdk = [("WDp", 0), ("WDp", 1), ("sgb", 0), ("sgb", 1)]
            actT = A.alloc([NFT, 512], BF16)
            junk = A.alloc([D], BF16)
            ssy = A.alloc([4, 2], F32)
            ssh = A.alloc([4], F32)
            ssf = A.alloc([4, 2], F32)
            rs1 = A.alloc([4], F32)
            rs2 = A.alloc([4], F32)
            rs3 = A.alloc([4], F32)
            DMA("gpsimd", Wo, w_out.rearrange("(kc p) c -> p kc c", p=128), r=[], w=["Wo"])
            DMA("sync", gpost, g_post.partition_broadcast(128), r=[], w=["gpost"])
            DMA("sync", g3, g_fpre.partition_broadcast(128), r=[], w=["g3"])
            DMA("sync", g4, g_fpost.partition_broadcast(128), r=[], w=["g4"])
            w_fg_r = scr_g.rearrange("(kc p) f -> p kc f", p=128)
            w_fu_r = scr_u.rearrange("(kc p) f -> p kc f", p=128)
            w_fd_r = scr_d.rearrange("(ft p) c -> p ft c", p=128)
            npiece = NFT // 2
            wgu_n = 0
            wd_n = 0

            def mixT(kc, sl):
                return ofT[:, kc, sl] if kc < 4 else odT[:, kc - 4, sl]

            import os as _os
            _p2 = int(_os.environ.get("K_P2", "9"))
            def pre_b(j, banks):
                TT("gpsimd", rs1[:, j:j + 1], ssy[:, j, 0:1], ssy[:, j, 1:2], ALU.add,
                   r=[("ssy", j, 0), ("ssy", j, 1)], w=[("rs1", j)])
                TS("gpsimd", rs1[:, j:j + 1], rs1[:, j:j + 1], 1.0 / D, EPS, ALU.mult, ALU.add, r=[("rs1", j)],
                   w=[("rs1", j)])
                TT("gpsimd", rs1[:, j:j + 1], rs1[:, j:j + 1], nhalf, ALU.pow, r=[("rs1", j), "nhalf"],
                   w=[("rs1", j)])
                for hf in range(2):
                    bank, bk = banks[hf]
                    hs = slice(hf * 512, (hf + 1) * 512)
                    STT(tmpy, bank[:], rs1[:, j:j + 1], gpost[:, hs], ALU.mult, ALU.mult,
                        r=[bk, ("rs1", j), "gpost"], w=["tmpy"])
                    TT("vector", hbuf[:, j, hs], hbuf[:, j, hs], tmpy, ALU.add, r=["tmpy", ("h", j)],
                       w=[("h", j)])
                ACT(junk, hbuf[:, j, :], AF.Square, r=[("h", j)], w=[("ssh", j), "junk2"], accum=ssh[:, j:j + 1])
                TS("gpsimd", rs2[:, j:j + 1], ssh[:, j:j + 1], 1.0 / D, EPS, ALU.mult, ALU.add, r=[("ssh", j)],
                   w=[("rs2", j)])
                TT("gpsimd", rs2[:, j:j + 1], rs2[:, j:j + 1], nhalf, ALU.pow, r=[("rs2", j), "nhalf"],
                   w=[("rs2", j)])
                STT(vb, hbuf[:, j, :], rs2[:, j:j + 1], g3, ALU.mult, ALU.mult,
                    r=[("h", j), ("rs2", j), "g3"], w=["vb"])
                for kc in range(KC):
                    TR(pst[:, kc * 128:(kc + 1) * 128], vb[:, kc * 128:(kc + 1) * 128], identb,
                       r=["vb", "c_identb"], w=["pst"])
                CP("vector", vT[:, :, j * 128:(j + 1) * 128], pst[:].rearrange("p (a b) -> p a b", a=KC, b=128),
                   r=["pst"], w=[("vT", j)])

            for c in range(NCH):
                ybanks = {}
                for j in range(4):
                    t = 4 * c + j
                    ts_ = slice(t * 128, (t + 1) * 128)
                    DMA("sync", hbuf[:, j, :], x[ts_, :], r=[], w=[("h", j)])
                    banks = []
                    for hf in range(2):
                        bank, bk = rot(7)
                        banks.append((bank, bk))
                        for kc in range(KC):
                            MM(bank[:], mixT(kc, ts_), Wo[:, kc, hf * 512:(hf + 1) * 512], kc == 0, kc == KC - 1,
                               r=["Wo"], w=[bk])
                        ACT(junk[:, 0:512], bank[:], AF.Square, r=[bk], w=[("ssy", j, hf), "junk2"], accum=ssy[:, j, hf:hf + 1])
                    ybanks[j] = banks
                    if j >= 1:
                        pre_b(j - 1, ybanks[j - 1])
                pre_b(3, ybanks[3])
                vTk = [("vT", j) for j in range(4)]
                if _p2 < 1:
                    for j in range(4):
                        DMA("sync", out[(4 * c + j) * 128:(4 * c + j + 1) * 128, :], hbuf[:, j, :], r=[("h", j)], w=[("out", 4 * c + j)])
                    continue
                for p_ in range(npiece):
                    sl = wgu_n % 2
                    wgu_n += 1
                    f0 = p_ * 256
                    DMA("sync", WGU[sl][:, :, 0:256], w_fg_r[:, :, f0:f0 + 256], r=[], w=[("WGUg", sl)])
                    DMA("sync", WGU[sl][:, :, 256:512], w_fu_r[:, :, f0:f0 + 256], r=[], w=[("WGUu", sl)])
                    for f2 in range(2):
                        ft = 2 * p_ + f2
                        bG, kG = rot(7)
                        for kc in range(KC):
                            MM(bG[:], WGU[sl][:, kc, f2 * 128:(f2 + 1) * 128], vT[:, kc, :], kc == 0, kc == KC - 1,
                               r=[("WGUg", sl)] + vTk, w=[kG])
                        bU, kU = rot(7)
                        for kc in range(KC):
                            MM(bU[:], WGU[sl][:, kc, 256 + f2 * 128:256 + (f2 + 1) * 128], vT[:, kc, :], kc == 0,
                               kc == KC - 1, r=[("WGUu", sl)] + vTk, w=[kU])
                        ss_ = ft % 2
                        ACT(sgb[ss_], bG[:], AF.Silu, r=[kG], w=[("sgb", ss_)])
                        TT("vector", actT[:, ft, :], sgb[ss_], bU[:], ALU.mult, r=[("sgb", ss_), kU], w=[("actT", ft)])
                if _p2 < 2:
                    for j in range(4):
                        DMA("sync", out[(4 * c + j) * 128:(4 * c + j + 1) * 128, :], hbuf[:, j, :], r=[("h", j)], w=[("out", 4 * c + j)])
                    continue
                for hf in range(2):
                    hs = slice(hf * 512, (hf + 1) * 512)
                    accs = [(psf[3 + j], f"ps{3 + j}") for j in range(4)]
                    for p_ in range(npiece):
                        sl = wd_n % 4
                        wd_n += 1
                        DMA("sync", WDp[sl], w_fd_r[:, 2 * p_:2 * p_ + 2, hs], r=[], w=[wdk[sl]])
                        for f2 in range(2):
                            ft = 2 * p_ + f2
                            for j in range(4):
                                MM(accs[j][0][:], actT[:, ft, j * 128:(j + 1) * 128], WDp[sl][:, f2, :], ft == 0,
                                   ft == NFT - 1, r=[("actT", ft), wdk[sl]], w=[accs[j][1]])
                    for j in range(4):
                        ACT(junk[:, 0:512], accs[j][0][:], AF.Square, r=[accs[j][1]], w=[("ssf", j, hf), "junk2"],
                            accum=ssf[:, j, hf:hf + 1])
                        CP("vector", ffb[:, j, hs], accs[j][0][:], r=[accs[j][1]], w=[("ffb", j)])
                for j in range(4):
                    t = 4 * c + j
                    TT("gpsimd", rs3[:, j:j + 1], ssf[:, j, 0:1], ssf[:, j, 1:2], ALU.add,
                       r=[("ssf", j, 0), ("ssf", j, 1)], w=[("rs3", j)])
                    TS("gpsimd", rs3[:, j:j + 1], rs3[:, j:j + 1], 1.0 / D, EPS, ALU.mult, ALU.add, r=[("rs3", j)],
                       w=[("rs3", j)])
                    TT("gpsimd", rs3[:, j:j + 1], rs3[:, j:j + 1], nhalf, ALU.pow, r=[("rs3", j), "nhalf"],
                       w=[("rs3", j)])
                    STT(ffb[:, j, :], ffb[:, j, :], rs3[:, j:j + 1], g4, ALU.mult, ALU.mult,
                        r=[("ffb", j), ("rs3", j), "g4"], w=[("ffb", j)])
                    TT("vector", ffb[:, j, :], ffb[:, j, :], hbuf[:, j, :], ALU.add, r=[("ffb", j), ("h", j)],
                       w=[("ffb", j)])
                    DMA("sync", out[t * 128:(t + 1) * 128, :], ffb[:, j, :], r=[("ffb", j)], w=[("out", t)])
            P.add("sync", None, r=[("out", t) for t in range(T)])
        P.barrier()
        print("arena peak bytes", A.peak * 2, "ops", {e: len(P.ops[e]) for e in ENGS})
        P.emit(block, sems)
    return nc


_CACHE = {}


def kernel(**inputs):
    S = 4096
    TOPK = 256
    B = 8
    x = np.asarray(inputs["x"], dtype=np.float32)
    consts = host_consts(S, TOPK)
    shared = {}
    for k in ("norm_mix_pre", "w_in", "b_forget", "b_gate", "w_branch_fox", "w_branch_dsa", "w_out",
              "norm_mix_post", "norm_ffn_pre", "w_ffn_gate", "w_ffn_up", "w_ffn_down", "norm_ffn_post"):
        shared[k] = np.ascontiguousarray(np.asarray(inputs[k], dtype=np.float32)[0])
    shared.update(consts)
    if "nc" not in _CACHE:
        _CACHE["nc"] = build(S, TOPK)
    nc = _CACHE["nc"]
    in_maps = []
    for b in range(B):
        m = dict(shared)
        m["x"] = np.ascontiguousarray(x[b])
        in_maps.append(m)
    res = run_bass_kernel_spmd(nc, in_maps, core_ids=list(range(B)))
    return np.stack([np.asarray(r["out"], dtype=np.float32) for r in res.results], axis=0)
```
